# Optimizing a Trainium2 kernel written in Bass

```python
import math
import jax, jax.numpy as jnp
from jax import lax
import numpy as np

D_MODEL = 1024
BATCH = 8
SEQ = 4096
DEPTH = 4

D_A = D_MODEL
CONV_A_WIDTH = 3
N_POOL_GROUPS = 4
POOL_GROUP_DIM = D_MODEL // N_POOL_GROUPS
POOL_WINDOWS = (2, 4, 8, 16)
D_C = D_MODEL
CONV_C_WIDTH = 31
N_BRANCHES = 3
IN_SPLITS = (D_A, D_A, D_A, D_MODEL, D_C, D_C, N_BRANCHES * D_MODEL)
D_IN = sum(IN_SPLITS)
N_EXPERTS = 32
TOP_K = 4
D_FF = D_MODEL
SWIGLU_LIMIT = 7.0
SWIGLU_ALPHA = 1.702
EXPERT_BLOCK = 128
LN_EPS = 1e-5
DEEPNORM_ALPHA = (2.0 * DEPTH) ** 0.25
DEEPNORM_BETA = (8.0 * DEPTH) ** -0.25

kernel_name = "hybrid_gated_conv_pool_conformer_moe_deepnorm"


def layer_norm(x, g, b):
    xf = x.astype(jnp.float32)
    mu = jnp.mean(xf, axis=-1, keepdims=True)
    var = jnp.mean(jnp.square(xf - mu), axis=-1, keepdims=True)
    y = (xf - mu) * lax.rsqrt(var + LN_EPS) * g.astype(jnp.float32) + b.astype(jnp.float32)
    return y.astype(x.dtype)


def causal_dwconv(u, w):
    k, c = w.shape
    return lax.conv_general_dilated(
        u, w[:, None, :].astype(u.dtype), window_strides=(1,), padding=[(k - 1, 0)],
        dimension_numbers=("NWC", "WIO", "NWC"), feature_group_count=c)


def causal_multiscale_pool(p):
    bsz, s, _ = p.shape
    pg = p.reshape(bsz, s, N_POOL_GROUPS, POOL_GROUP_DIM).astype(jnp.float32)
    csp = jnp.pad(jnp.cumsum(pg, axis=1), ((0, 0), (1, 0), (0, 0), (0, 0)))
    t = jnp.arange(s)
    outs = []
    for g, w in enumerate(POOL_WINDOWS):
        c = csp[:, :, g]
        upper = c[:, 1:]
        lower = jnp.pad(c, ((0, 0), (w - 1, 0), (0, 0)))[:, :s]
        cnt = jnp.minimum(t + 1, w).astype(jnp.float32)[None, :, None]
        outs.append((upper - lower) / cnt)
    pooled = jnp.stack(outs, axis=2)
    return pooled - pg


def token_mixer(x, w_in, b_in, conv_a, w_out_a, w_pool, scale_pool,
                conv_c, conv_c_b, ln_c_g, ln_c_b, w_out_c, b_out_c, w_o):
    bsz, s, d = x.shape
    z = jnp.einsum("bsd,de->bse", x, w_in) + b_in
    idx = np.cumsum(IN_SPLITS)[:-1].tolist()
    gb_a, gc_a, h_a, p_in, glu_a, glu_b, gate_logits = jnp.split(z, idx, axis=-1)

    u_a = causal_dwconv(gc_a * h_a, conv_a)
    y_a = jnp.einsum("bsc,cd->bsd", gb_a * u_a, w_out_a)

    pooled = causal_multiscale_pool(p_in).astype(x.dtype)
    y_b = jnp.einsum("bsgc,gce->bsge", pooled, w_pool).reshape(bsz, s, d) * scale_pool

    v = glu_a * jax.nn.sigmoid(glu_b)
    v = causal_dwconv(v, conv_c) + conv_c_b
    v = jax.nn.silu(layer_norm(v, ln_c_g, ln_c_b))
    y_c = jnp.einsum("bsc,cd->bsd", v, w_out_c) + b_out_c

    g = jax.nn.sigmoid(gate_logits).reshape(bsz, s, N_BRANCHES, d)
    merged = g[:, :, 0] * y_a + g[:, :, 1] * y_b + g[:, :, 2] * y_c
    return jnp.einsum("bsd,de->bse", merged, w_o)


def moe_ffn(x2, w_router, b_router, w_gu, b_gu, w_down, b_down):
    t_tok, d = x2.shape
    logits = jnp.einsum("td,de->te", x2, w_router).astype(jnp.float32) + b_router.astype(jnp.float32)
    top_vals, top_idx = lax.top_k(logits, TOP_K)
    gates = jax.nn.softmax(top_vals, axis=-1)

    n_assign = t_tok * TOP_K
    flat_e = top_idx.reshape(n_assign)
    order = jnp.argsort(flat_e)
    sorted_e = flat_e[order]
    sorted_tok = (order // TOP_K).astype(jnp.int32)
    sorted_gate = gates.reshape(n_assign)[order]

    counts = jnp.bincount(flat_e, length=N_EXPERTS)
    blocks_per_e = (counts + EXPERT_BLOCK - 1) // EXPERT_BLOCK
    blk_end = jnp.cumsum(blocks_per_e)
    blk_start = blk_end - blocks_per_e
    grp_start = jnp.cumsum(counts) - counts
    rank = jnp.arange(n_assign) - grp_start[sorted_e]
    dest = blk_start[sorted_e] * EXPERT_BLOCK + rank

    n_blocks = -(-n_assign // EXPERT_BLOCK) + N_EXPERTS
    n_slots = n_blocks * EXPERT_BLOCK
    slot_tok = jnp.full((n_slots,), t_tok, jnp.int32).at[dest].set(sorted_tok)
    slot_gate = jnp.zeros((n_slots,), jnp.float32).at[dest].set(sorted_gate)
    block_e = jnp.minimum(jnp.searchsorted(blk_end, jnp.arange(n_blocks), side="right"),
                          N_EXPERTS - 1).astype(jnp.int32)

    x_pad = jnp.concatenate([x2, jnp.zeros((1, d), x2.dtype)], axis=0)

    def expert_block(args):
        tok, e = args
        xb = x_pad[tok]
        h = xb @ w_gu[e] + b_gu[e]
        gate = jnp.minimum(h[:, 0::2], SWIGLU_LIMIT)
        up = jnp.clip(h[:, 1::2], -SWIGLU_LIMIT, SWIGLU_LIMIT)
        act = (up + 1.0) * (gate * jax.nn.sigmoid(SWIGLU_ALPHA * gate))
        return act @ w_down[e] + b_down[e]

    y_blocks = lax.map(expert_block, (slot_tok.reshape(n_blocks, EXPERT_BLOCK), block_e))
    y = y_blocks.reshape(n_slots, d).astype(jnp.float32) * slot_gate[:, None]
    out = jax.ops.segment_sum(y, slot_tok, num_segments=t_tok + 1)[:t_tok]
    return out.astype(x2.dtype)


def setup_inputs(seed: int = 0) -> dict:
    key = jax.random.key(seed)
    ks = jax.random.split(key, 24)
    f32 = jnp.float32
    nrm = lambda k, shape, scale: jax.random.normal(k, shape, f32) * scale
    L = DEPTH
    return {
        "x": jax.random.normal(ks[0], (BATCH, SEQ, D_MODEL), f32),
        "w_in": nrm(ks[1], (L, D_MODEL, D_IN), D_MODEL ** -0.5),
        "b_in": nrm(ks[2], (L, D_IN), 0.02),
        "conv_a": nrm(ks[3], (L, CONV_A_WIDTH, D_A), CONV_A_WIDTH ** -0.5),
        "w_out_a": nrm(ks[4], (L, D_A, D_MODEL), D_A ** -0.5),
        "w_pool": nrm(ks[5], (L, N_POOL_GROUPS, POOL_GROUP_DIM, POOL_GROUP_DIM), POOL_GROUP_DIM ** -0.5),
        "scale_pool": 1.0 + nrm(ks[6], (L, D_MODEL), 0.1),
        "conv_c": nrm(ks[7], (L, CONV_C_WIDTH, D_C), CONV_C_WIDTH ** -0.5),
        "conv_c_b": nrm(ks[8], (L, D_C), 0.02),
        "ln_c_g": 1.0 + nrm(ks[9], (L, D_C), 0.05),
        "ln_c_b": nrm(ks[10], (L, D_C), 0.02),
        "w_out_c": nrm(ks[11], (L, D_C, D_MODEL), D_C ** -0.5),
        "b_out_c": nrm(ks[12], (L, D_MODEL), 0.02),
        "w_o": nrm(ks[13], (L, D_MODEL, D_MODEL), D_MODEL ** -0.5 * DEEPNORM_BETA),
        "ln1_g": 1.0 + nrm(ks[14], (L, D_MODEL), 0.05),
        "ln1_b": nrm(ks[15], (L, D_MODEL), 0.02),
        "w_router": nrm(ks[16], (L, D_MODEL, N_EXPERTS), D_MODEL ** -0.5),
        "b_router": nrm(ks[17], (L, N_EXPERTS), 0.01),
        "w_gu": nrm(ks[18], (L, N_EXPERTS, D_MODEL, 2 * D_FF), D_MODEL ** -0.5),
        "b_gu": nrm(ks[19], (L, N_EXPERTS, 2 * D_FF), 0.02),
        "w_down": nrm(ks[20], (L, N_EXPERTS, D_FF, D_MODEL), D_FF ** -0.5 * DEEPNORM_BETA),
        "b_down": nrm(ks[21], (L, N_EXPERTS, D_MODEL), 0.02),
        "ln2_g": 1.0 + nrm(ks[22], (L, D_MODEL), 0.05),
        "ln2_b": nrm(ks[23], (L, D_MODEL), 0.02),
    }


def reference(x, w_in, b_in, conv_a, w_out_a, w_pool, scale_pool, conv_c, conv_c_b,
              ln_c_g, ln_c_b, w_out_c, b_out_c, w_o, ln1_g, ln1_b, w_router, b_router,
              w_gu, b_gu, w_down, b_down, ln2_g, ln2_b):
    bsz, s, d = x.shape
    for l in range(DEPTH):
        h = token_mixer(x, w_in[l], b_in[l], conv_a[l], w_out_a[l], w_pool[l], scale_pool[l],
                        conv_c[l], conv_c_b[l], ln_c_g[l], ln_c_b[l], w_out_c[l], b_out_c[l], w_o[l])
        x = layer_norm(DEEPNORM_ALPHA * x + h, ln1_g[l], ln1_b[l])
        m = moe_ffn(x.reshape(bsz * s, d), w_router[l], b_router[l], w_gu[l], b_gu[l],
                    w_down[l], b_down[l]).reshape(bsz, s, d)
        x = layer_norm(DEEPNORM_ALPHA * x + m, ln2_g[l], ln2_b[l])
    return x
```

```python
import numpy as np
import concourse.bass as bass
import concourse.mybir as mybir
from concourse.bass_utils import run_bass_kernel_spmd
from contextlib import ExitStack

F32 = mybir.dt.float32
BF16 = mybir.dt.bfloat16
I32 = mybir.dt.int32
AF = mybir.ActivationFunctionType
ALU = mybir.AluOpType

P = 128
D = 1024
KC = 8
E = 32
T = 512
TT = 4
DEPTH = 4
ALPHA = (2.0 * DEPTH) ** 0.25
EPS = 1e-5
WB = 256
NPRM = 896
O_BIN, O_CA, O_SP, O_CC, O_CCB, O_LG, O_LB, O_BOC, O_BGU = 0, 72, 96, 104, 352, 360, 368, 376, 384


class Res:
    __slots__ = ("name", "lw", "rd", "dsem", "dcnt")

    def __init__(self, name):
        self.name = name
        self.lw = {}
        self.rd = {}
        self.dsem = None
        self.dcnt = 0


class FW:
    def __init__(self, nc, es):
        self.nc = nc
        self.es = es
        self.sems = {}
        self.eng = {}
        for name, h in (("pe", nc.tensor), ("act", nc.scalar), ("dve", nc.vector),
                        ("pool", nc.gpsimd), ("sp", nc.sync)):
            key = "S_" + name
            self.sems[key] = es.enter_context(nc.semaphore(key))
            self.eng[name] = dict(h=h, key=key, tick=0, known={})
        self.ndsem = 0
        self.nins = 0
        self.nwaits = 0

    def _emit_waits(self, ename, reads, writes, cwrites):
        E_ = self.eng[ename]
        own = E_["key"]
        waits = {}

        def need(k, c):
            if waits.get(k, 0) < c:
                waits[k] = c
        raw_own = 0
        for r in reads:
            for k, c in r.lw.items():
                if k == own:
                    raw_own = max(raw_own, c)
                else:
                    need(k, c)
        for w in writes:
            for k, c in w.lw.items():
                if k != own:
                    need(k, c)
            for k, c in w.rd.items():
                if k != own:
                    need(k, c)
        for w in cwrites:
            for k, c in w.rd.items():
                if k != own:
                    need(k, c)
        if raw_own and ename != "pe":
            need(own, raw_own)
        for k, c in waits.items():
            if E_["known"].get(k, 0) < c:
                E_["h"].wait_ge(self.sems[k], c)
                E_["known"][k] = c
                self.nwaits += 1

    def _mark(self, tok, reads, writes, cwrites):
        k, c = tok
        for r in reads:
            if r.rd.get(k, 0) < c:
                r.rd[k] = c
        for w in writes:
            w.lw = {k: c}
            w.rd = {}
        for w in cwrites:
            if w.lw.get(k, 0) < c:
                w.lw[k] = c

    def op(self, ename, fn, reads=(), writes=(), inc=True):
        E_ = self.eng[ename]
        self._emit_waits(ename, reads, writes, ())
        ins = fn(E_["h"])
        self.nins += 1
        if inc:
            E_["tick"] += 1
            ins.then_inc(self.sems[E_["key"]], 1)
            tok = (E_["key"], E_["tick"])
        else:
            tok = (E_["key"], E_["tick"] + 1)
        self._mark(tok, reads, writes, ())
        return ins

    def dma(self, ename, fn, sbres, reads=(), writes=(), cwrites=()):
        E_ = self.eng[ename]
        if sbres.dsem is None:
            key = "D%d" % self.ndsem
            self.ndsem += 1
            self.sems[key] = self.es.enter_context(self.nc.semaphore(key))
            sbres.dsem = key
        self._emit_waits(ename, reads, writes, cwrites)
        ins = fn(E_["h"])
        self.nins += 1
        sbres.dcnt += 16
        ins.then_inc(self.sems[sbres.dsem], 16)
        self._mark((sbres.dsem, sbres.dcnt), reads, writes, cwrites)
        return ins

    def barrier(self, all_res):
        waits = {}
        for nm, E_ in self.eng.items():
            if E_["tick"]:
                waits[E_["key"]] = E_["tick"]
        for r in all_res:
            for k, c in list(r.lw.items()) + list(r.rd.items()):
                if waits.get(k, 0) < c:
                    waits[k] = c
        for nm, E_ in self.eng.items():
            for k, c in waits.items():
                if k == E_["key"]:
                    continue
                if E_["known"].get(k, 0) < c:
                    E_["h"].wait_ge(self.sems[k], c)
                    E_["known"][k] = c
                    self.nwaits += 1


class Tile:
    __slots__ = ("ap", "res")

    def __init__(self, ap, name):
        self.ap = ap
        self.res = Res(name)


class Carver:
    def __init__(self, arena, base, limit, allres):
        self.arena = arena
        self.off = base
        self.limit = limit
        self.allres = allres

    def get(self, name, shape, dtype):
        esz = 4 if dtype in (F32, I32) else 2
        n = 1
        for s in shape[1:]:
            n *= s
        nb = (n * esz + 31) // 32 * 32
        o = self.off
        self.off += nb
        assert self.off <= self.limit, ("SBUF arena overflow", name, self.off, self.limit)
        ap = self.arena[:, o // 2:(o + n * esz) // 2]
        if dtype != BF16:
            ap = ap.bitcast(dtype)
        if len(shape) == 3:
            ap = ap.rearrange("p (a b) -> p a b", b=shape[2])
        elif len(shape) == 4:
            ap = ap.rearrange("p (a b c) -> p a b c", b=shape[2], c=shape[3])
        t = Tile(ap, name)
        self.allres.append(t.res)
        return t

    def ring(self, name, n, shape, dtype):
        return Ring([self.get("%s%d" % (name, i), shape, dtype) for i in range(n)])


class Ring:
    def __init__(self, tiles):
        self.tiles = tiles
        self.i = 0

    def next(self):
        t = self.tiles[self.i % len(self.tiles)]
        self.i += 1
        return t


class PsumAlloc:
    def __init__(self, psum_f32):
        self.f = psum_f32
        self.b = psum_f32.bitcast(BF16)
        self.res = [Res("psum%d" % i) for i in range(8)]
        self.free = list(range(8))

    def alloc(self):
        assert self.free, "PSUM banks exhausted"
        return self.free.pop(0)

    def release(self, i):
        self.free.append(i)


def build_program(S, CAP, NL, first_is_input=True):
    NCH = S // T
    NT = S // P
    NSLOT = E * CAP
    TRASH = NSLOT
    CT = CAP // P
    NH = 2 if CAP > 512 else 1
    CH = CAP // NH
    nc = bass.Bass("TRN2", target_bir_lowering=False)

    def din(name, shape, dt=F32):
        return nc.dram_tensor(name, list(shape), dt, kind="ExternalInput").ap()
    x_d = din("x", [S, D])
    w_in_d = din("w_in", [NL, D, 9 * D])
    w_oa_d = din("w_out_a", [NL, D, D])
    w_pool_d = din("w_pool", [NL, 4, 256, 256])
    w_oc_d = din("w_out_c", [NL, D, D])
    w_o_d = din("w_o", [NL, D, D])
    w_r_d = din("w_router", [NL, D, E])
    w_gu_d = din("w_gu", [NL, E, D, 2 * D])
    w_dn_d = din("w_down", [NL, E, D, D])
    prm_d = din("prm", [NL, P, NPRM])
    lnp_d = din("lnp", [NL, 4, D])
    brt_d = din("b_router", [NL, E])
    bdn_d = din("b_down", [NL, E, D])
    out_d = nc.dram_tensor("out", [S, D], F32, kind="ExternalOutput").ap()
    X1_d = nc.dram_tensor("X1s", [S, D], F32, kind="Internal").ap()
    X2_d = nc.dram_tensor("X2s", [S, D], F32, kind="Internal").ap()
    XS_d = nc.dram_tensor("XSs", [NSLOT + P, D], BF16, kind="Internal").ap()
    YS_d = nc.dram_tensor("YSs", [NSLOT + P, D], F32, kind="Internal").ap()
    r_X1 = [Res("X1c%d" % c) for c in range(NCH)]
    r_X2 = [Res("X2c%d" % c) for c in range(NCH)]
    r_XS = Res("XS")
    r_YS = Res("YS")

    with ExitStack() as es:
        fw = FW(nc, es)
        ARENA_BYTES = 206 * 1024
        arena = es.enter_context(nc.sbuf_tensor("arena", [P, ARENA_BYTES // 2], BF16))
        psum = es.enter_context(nc.psum_tensor("psum", [P, 8, 512], F32))
        PS = PsumAlloc(psum)
        allres = list(PS.res) + r_X1 + r_X2 + [r_XS, r_YS]
        G = Carver(arena, 0, ARENA_BYTES, allres)

        def act(fn, reads, writes):
            return fw.op("act", fn, reads, writes)

        def dve(fn, reads, writes):
            return fw.op("dve", fn, reads, writes)

        def pool(fn, reads, writes):
            return fw.op("pool", fn, reads, writes)

        def mm(out, lhsT, rhs, start, stop, reads, writes):
            return fw.op("pe", lambda e: e.matmul(out, lhsT=lhsT, rhs=rhs, start=start, stop=stop),
                         reads, writes, inc=stop)

        ident_f = G.get("ident_f", [P, P], F32)
        ident_b = G.get("ident_b", [P, P], BF16)
        triu_b = G.get("triu_b", [P, P], BF16)
        ones_b = G.get("ones_b", [P, P], BF16)
        iota_cap = G.get("iota_cap", [P, E], F32)
        rcnt = G.get("rcnt", [P, 4, 16], F32)
        io_t = G.get("io_t", [P, P], F32)
        PRM = G.get("PRM", [P, NPRM], F32)
        LNP = G.get("LNP", [P, 4, D], F32)
        WR = G.get("WR", [P, KC, E], F32)
        BRT = G.get("BRT", [P, E], F32)
        DG3 = G.get("DG3", [P, KC, 3, P], BF16)
        GATES = G.get("GATES", [P, NT, 4], F32)
        DESTI = G.get("DESTI", [P, NT, 4], I32)
        BASE = G.get("BASE", [P, E], F32)
        HA = G.get("HA", [P, KC, 2], BF16)
        HP = G.get("HP", [P, KC, 16], F32)
        HC = G.get("HC", [P, KC, 32], BF16)
        zrow = G.get("zrow", [P, D], F32)
        GBASE = G.off

        pool(lambda e: e.iota(io_t.ap, pattern=[[1, P]], base=0, channel_multiplier=-1,
                              allow_small_or_imprecise_dtypes=True), [], [io_t.res])
        dve(lambda e: e.tensor_single_scalar(out=ident_f.ap, in_=io_t.ap, scalar=0.0, op=ALU.is_equal),
            [io_t.res], [ident_f.res])
        dve(lambda e: e.tensor_single_scalar(out=ident_b.ap, in_=io_t.ap, scalar=0.0, op=ALU.is_equal),
            [io_t.res], [ident_b.res])
        dve(lambda e: e.tensor_single_scalar(out=triu_b.ap, in_=io_t.ap, scalar=0.0, op=ALU.is_gt),
            [io_t.res], [triu_b.res])
        dve(lambda e: e.memset(ones_b.ap, 1.0), [], [ones_b.res])
        pool(lambda e: e.iota(iota_cap.ap, pattern=[[CAP, E]], base=0, channel_multiplier=0,
                              allow_small_or_imprecise_dtypes=True), [], [iota_cap.res])
        for g in range(4):
            w = 2 << g
            pool(lambda e, g=g: e.iota(rcnt.ap[:, g, :], pattern=[[1, 16]], base=1, channel_multiplier=0,
                                       allow_small_or_imprecise_dtypes=True), [], [rcnt.res])
            dve(lambda e, g=g, w=w: e.tensor_scalar_min(out=rcnt.ap[:, g, :], in0=rcnt.ap[:, g, :], scalar1=float(w)),
                [rcnt.res], [rcnt.res])
        dve(lambda e: e.reciprocal(out=rcnt.ap, in_=rcnt.ap), [rcnt.res], [rcnt.res])
        dve(lambda e: e.memset(zrow.ap, 0.0), [], [zrow.res])
        XSz = XS_d.rearrange("(n p) d -> n p d", p=P)
        YSz = YS_d.rearrange("(n p) d -> n p d", p=P)
        zb = zrow.ap.bitcast(BF16)
        for n in range(0, (NSLOT + P) // P, 2):
            nn = min(2, (NSLOT + P) // P - n)
            fw.dma("sp", lambda e, n=n, nn=nn: e.dma_start(
                out=XSz[n:n + nn].rearrange("n p d -> p n d"),
                in_=zb[:, 0:nn * D].rearrange("p (n d) -> p n d", d=D)), zrow.res,
                reads=[zrow.res], cwrites=[r_XS])
        fw.dma("sp", lambda e: e.dma_start(out=YSz[NSLOT // P], in_=zrow.ap), zrow.res,
               reads=[zrow.res], cwrites=[r_YS])

        A = Carver(arena, GBASE, ARENA_BYTES, allres)
        XT = A.get("XT", [P, KC, T], BF16)
        PL1 = A.get("PL1", [P, KC, T], BF16)
        CVB = A.get("CVB", [P, KC, T], BF16)
        MACC = A.get("MACC", [P, KC, T], BF16)
        DG31 = A.get("DG31", [P, 31, P], BF16)
        NRA = 10
        ringA = A.ring("wA", NRA, [P, KC, WB], BF16)
        r_bf = A.ring("sbf", 6, [P, T], BF16)
        r_uh = A.ring("uh", 2, [P, T + 2], BF16)
        r_vh = A.ring("vh", 2, [P, T + 30], BF16)
        r_cv = A.ring("cv", 2, [P, T], F32)
        Pt = A.get("Pt", [P, 2, T + 15], F32)
        Sa = A.get("Sa", [P, 2, T + 15], F32)
        Sb = A.get("Sb", [P, 2, T + 15], F32)
        r_pooled = A.ring("pooled", 2, [P, 2, T], BF16)
        st_mean = A.get("st_mean", [P, T], F32)
        st_var = A.get("st_var", [P, T], F32)
        st_mr = A.get("st_mr", [P, T], F32)
        r_R = A.ring("R", 2, [P, D], F32)
        XRES = A.get("XRES", [P, D], F32)
        r_x1b = A.ring("x1b", 2, [P, D], BF16)
        X1T = A.get("X1T", [P, KC, P], F32)
        r_small = A.ring("rt", 2, [P, 512], F32)
        r_yg = A.ring("yg", 2, [P, D], F32)
        X1L = A.get("X1L", [P, D], F32)
        r_acc = A.ring("acc", 2, [P, D], F32)
        r_xb = A.ring("xb", 2, [P, D], BF16)
        A_END = A.off

        B = Carver(arena, GBASE, ARENA_BYTES, allres)
        r_xe = B.ring("xe", 2, [P, CT, D], BF16)
        r_xet = B.ring("xet", 2, [P, KC, CAP], BF16)
        r_actt = B.ring("actt", 2, [P, KC, CAP], BF16)
        NRB = 16
        ringB = B.ring("wB", NRB, [P, KC, WB], BF16)
        r_g = B.ring("bg", 2, [P, CAP], F32)
        r_sg = B.ring("bsg", 2, [P, CAP], F32)
        r_u = B.ring("bu", 2, [P, CAP], F32)
        r_a1 = B.ring("ba1", 2, [P, CAP], F32)
        r_yo = B.ring("yo", 3, [P, D], F32)
        r_bd = B.ring("bd", 2, [P, D], F32)
        B_END = B.off

        class WStream:
            def __init__(self, ring, nring, la):
                self.ring = ring
                self.n = nring
                self.la = la
                self.blocks = []
                self.emitted = 0
                self.tiles = {}

            def add(self, src, nk=KC):
                self.blocks.append((src, nk))
                return len(self.blocks) - 1

            def get(self, i):
                lim = min(len(self.blocks), i + 1 + self.la)
                while self.emitted < lim:
                    j = self.emitted
                    t = self.ring.next()
                    src, nk = self.blocks[j]
                    fw.dma("pool", lambda e, t=t, src=src, nk=nk: e.dma_start(out=t.ap[:, 0:nk, :], in_=src), t.res,
                           writes=[t.res])
                    self.tiles[j] = t
                    self.emitted += 1
                return self.tiles[i]

        def wcols(w2d, c0, ncol=WB):
            return w2d.rearrange("(k p) c -> p k c", p=P)[:, :, c0:c0 + ncol]

        def layer_setup(l):
            fw.dma("sp", lambda e: e.dma_start(out=PRM.ap, in_=prm_d[l]), PRM.res, writes=[PRM.res])
            for i in range(4):
                ll = l if i < 2 else l - 1
                if ll < 0:
                    continue
                fw.dma("sp", lambda e, i=i, ll=ll: e.dma_start(out=LNP.ap[:, i, :], in_=lnp_d[ll, i, :].partition_broadcast(P)),
                       LNP.res, writes=[LNP.res])
            fw.dma("sp", lambda e: e.dma_start(out=WR.ap, in_=w_r_d[l].rearrange("(k p) e -> p k e", p=P)),
                   WR.res, writes=[WR.res])
            fw.dma("sp", lambda e: e.dma_start(out=BRT.ap, in_=brt_d[l, :].partition_broadcast(P)),
                   BRT.res, writes=[BRT.res])
            for oc in range(KC):
                dve(lambda e, oc=oc: e.tensor_tensor(
                    out=DG3.ap[:, oc], in0=ident_f.ap.unsqueeze(1).to_broadcast([P, 3, P]),
                    in1=PRM.ap[:, O_CA + oc * 3:O_CA + oc * 3 + 3].unsqueeze(2).to_broadcast([P, 3, P]),
                    op=ALU.mult), [ident_f.res, PRM.res], [DG3.res])
            dve(lambda e: e.memset(BASE.ap, 0.0), [], [BASE.res])
            dve(lambda e: e.memset(HA.ap, 0.0), [], [HA.res])
            dve(lambda e: e.memset(HP.ap, 0.0), [], [HP.res])
            dve(lambda e: e.memset(HC.ap, 0.0), [], [HC.res])

        def make_xT(src_tile, j):
            xb = r_xb.next()
            act(lambda e: e.copy(out=xb.ap, in_=src_tile.ap), [src_tile.res], [xb.res])
            b = PS.alloc()
            for kc in range(KC):
                fw.op("pe", lambda e, kc=kc: e.transpose(out=PS.b[:, b, kc * P:(kc + 1) * P],
                                                         in_=xb.ap[:, kc * P:(kc + 1) * P], identity=ident_b.ap),
                      [xb.res, ident_b.res], [PS.res[b]], inc=(kc == KC - 1))
            dve(lambda e: e.tensor_copy(out=XT.ap[:, :, j * P:(j + 1) * P],
                                        in_=PS.b[:, b, :].rearrange("p (k t) -> p k t", t=P)),
                [PS.res[b]], [XT.res])
            PS.release(b)

        def ln_tokmajor(R, gi, bi, eng_gb):
            sm = r_small.next()
            st = sm.ap[:, 0:12].rearrange("p (a b) -> p a b", b=6)
            ag = sm.ap[:, 12:14]
            sd = sm.ap[:, 14:15]
            rs = sm.ap[:, 15:16]
            for h in range(2):
                dve(lambda e, h=h: e.bn_stats(out=st[:, h, :], in_=R.ap[:, h * 512:(h + 1) * 512]),
                    [R.res], [sm.res])
            dve(lambda e: e.bn_aggr(out=ag, in_=st), [sm.res], [sm.res])
            act(lambda e: e.activation(out=sd, in_=ag[:, 1:2], func=AF.Sqrt, bias=EPS, scale=1.0),
                [sm.res], [sm.res])
            dve(lambda e: e.reciprocal(out=rs, in_=sd), [sm.res], [sm.res])
            dve(lambda e: e.tensor_scalar(out=R.ap, in0=R.ap, scalar1=ag[:, 0:1], scalar2=rs,
                                          op0=ALU.subtract, op1=ALU.mult), [R.res, sm.res], [R.res])
            fw.op(eng_gb, lambda e: e.tensor_tensor(out=R.ap, in0=R.ap, in1=LNP.ap[:, gi, :], op=ALU.mult),
                  [R.res, LNP.res], [R.res])
            fw.op(eng_gb, lambda e: e.tensor_tensor(out=R.ap, in0=R.ap, in1=LNP.ap[:, bi, :], op=ALU.add),
                  [R.res, LNP.res], [R.res])

        def phase_c_tile(l, c, j, is_last):
            tg = c * TT + j
            tok0 = tg * P
            fw.dma("sp", lambda e: e.dma_start(out=X1L.ap, in_=X1_d[tok0:tok0 + P, :]), X1L.res,
                   reads=[r_X1[c]], writes=[X1L.res])
            acc = r_acc.next()
            for k in range(4):
                yg = r_yg.next()
                fw.dma("pool", lambda e, yg=yg, k=k: e.indirect_dma_start(
                    out=yg.ap, out_offset=None, in_=YS_d,
                    in_offset=bass.IndirectOffsetOnAxis(ap=DESTI.ap[:, tg, k:k + 1], axis=0)),
                    yg.res, reads=[r_YS, DESTI.res], writes=[yg.res])
                if k == 0:
                    dve(lambda e, yg=yg: e.tensor_scalar(out=acc.ap, in0=yg.ap, scalar1=GATES.ap[:, tg, 0:1],
                                                         scalar2=None, op0=ALU.mult),
                        [yg.res, GATES.res], [acc.res])
                else:
                    dve(lambda e, yg=yg, k=k: e.scalar_tensor_tensor(
                        out=acc.ap, in0=yg.ap, scalar=GATES.ap[:, tg, k:k + 1], in1=acc.ap,
                        op0=ALU.mult, op1=ALU.add), [yg.res, GATES.res, acc.res], [acc.res])
            dve(lambda e: e.scalar_tensor_tensor(out=acc.ap, in0=X1L.ap, scalar=ALPHA, in1=acc.ap,
                                                 op0=ALU.mult, op1=ALU.add), [X1L.res, acc.res], [acc.res])
            ln_tokmajor(acc, 2, 3, "pool")
            if is_last:
                fw.dma("sp", lambda e: e.dma_start(out=out_d[tok0:tok0 + P, :], in_=acc.ap), acc.res,
                       reads=[acc.res], cwrites=[r_out])
            else:
                fw.dma("sp", lambda e: e.dma_start(out=X2_d[tok0:tok0 + P, :], in_=acc.ap), acc.res,
                       reads=[acc.res], cwrites=[r_X2[c]])
                make_xT(acc, j)

        r_out = Res("out")
        allres.append(r_out)

        def phase_a(l, c):
            t0 = c * T
            ws = WStream(ringA, NRA, 5)
            w_in = w_in_d[l]
            bl = {}
            for q in range(4):
                for s in (1, 2, 0):
                    bl[("in", s, q)] = ws.add(wcols(w_in, s * D + q * WB))
            for q in range(4):
                bl[("oa", q)] = ws.add(wcols(w_oa_d[l], q * WB))
                bl[("in", 6, q)] = ws.add(wcols(w_in, 6 * D + q * WB))
            for g in range(4):
                bl[("in", 3, g)] = ws.add(wcols(w_in, 3 * D + g * WB))
                bl[("wp", g)] = ws.add(w_pool_d[l, g].rearrange("(k p) e -> p k e", p=P), 2)
                bl[("in", 7, g)] = ws.add(wcols(w_in, 7 * D + g * WB))
            for q in range(4):
                bl[("in", 5, q)] = ws.add(wcols(w_in, 5 * D + q * WB))
                bl[("in", 4, q)] = ws.add(wcols(w_in, 4 * D + q * WB))
            for q in range(4):
                bl[("oc", q)] = ws.add(wcols(w_oc_d[l], q * WB))
                bl[("in", 8, q)] = ws.add(wcols(w_in, 8 * D + q * WB))
            for q in range(4):
                bl[("wo", q)] = ws.add(wcols(w_o_d[l], q * WB))

            def inproj(s, oc):
                wt = ws.get(bl[("in", s, oc // 2)])
                b = PS.alloc()
                for k in range(KC):
                    mm(PS.f[:, b, :], wt.ap[:, k, (oc % 2) * P:(oc % 2 + 1) * P], XT.ap[:, k, :],
                       k == 0, k == KC - 1, [wt.res, XT.res], [PS.res[b]])
                return b

            def bias(s, oc):
                col = O_BIN + s * 8 + oc
                return PRM.ap[:, col:col + 1]

            YA = PL1
            pend = None
            for oc in range(KC + 1):
                if oc < KC:
                    b_gc = inproj(1, oc)
                    gct = r_bf.next()
                    act(lambda e, b=b_gc, gct=gct, oc=oc: e.activation(out=gct.ap, in_=PS.f[:, b, :], func=AF.Identity,
                                                                       bias=bias(1, oc), scale=1.0),
                        [PS.res[b_gc], PRM.res], [gct.res])
                    PS.release(b_gc)
                    b_h = inproj(2, oc)
                    uh = r_uh.next()
                    pool(lambda e, uh=uh, oc=oc: e.tensor_copy(out=uh.ap[:, 0:2], in_=HA.ap[:, oc, :]),
                         [HA.res], [uh.res])
                    dve(lambda e, b=b_h, uh=uh, gct=gct, oc=oc: e.scalar_tensor_tensor(
                        out=uh.ap[:, 2:T + 2], in0=PS.f[:, b, :], scalar=bias(2, oc), in1=gct.ap,
                        op0=ALU.add, op1=ALU.mult), [PS.res[b_h], PRM.res, gct.res, uh.res], [uh.res])
                    PS.release(b_h)
                    pool(lambda e, uh=uh, oc=oc: e.tensor_copy(out=HA.ap[:, oc, :], in_=uh.ap[:, T:T + 2]),
                         [uh.res], [HA.res])
                    b_gb = inproj(0, oc)
                    cur = (oc, uh, b_gb)
                else:
                    cur = None
                if pend is not None:
                    poc, puh, pb_gb = pend
                    b_cv = PS.alloc()
                    for k in range(3):
                        mm(PS.f[:, b_cv, :], DG3.ap[:, poc, k, :], puh.ap[:, k:k + T], k == 0, k == 2,
                           [DG3.res, puh.res], [PS.res[b_cv]])
                    cvt = r_cv.next()
                    act(lambda e, b=b_cv, cvt=cvt: e.copy(out=cvt.ap, in_=PS.f[:, b, :]), [PS.res[b_cv]], [cvt.res])
                    PS.release(b_cv)
                    dve(lambda e, b=pb_gb, cvt=cvt, poc=poc: e.scalar_tensor_tensor(
                        out=YA.ap[:, poc, :], in0=PS.f[:, b, :], scalar=bias(0, poc), in1=cvt.ap,
                        op0=ALU.add, op1=ALU.mult), [PS.res[pb_gb], PRM.res, cvt.res], [YA.res])
                    PS.release(pb_gb)
                pend = cur

            def out_and_gate(wkey, yin, gs, mode, post_scalar_col, first):
                for oc in range(KC):
                    wt = ws.get(bl[(wkey, oc // 2)])
                    b_y = PS.alloc()
                    for k in range(KC):
                        mm(PS.f[:, b_y, :], wt.ap[:, k, (oc % 2) * P:(oc % 2 + 1) * P], yin.ap[:, k, :],
                           k == 0, k == KC - 1, [wt.res, yin.res], [PS.res[b_y]])
                    b_g = inproj(gs, oc)
                    gt = r_bf.next()
                    act(lambda e, b=b_g, gt=gt, oc=oc: e.activation(out=gt.ap, in_=PS.f[:, b, :], func=AF.Sigmoid,
                                                                    bias=bias(gs, oc), scale=1.0),
                        [PS.res[b_g], PRM.res], [gt.res])
                    PS.release(b_g)
                    merge(b_y, gt, oc, mode, post_scalar_col, first)
                    PS.release(b_y)

            def merge(b_y, gt, oc, mode, col, first):
                sc = PRM.ap[:, col + oc:col + oc + 1] if col is not None else None
                if first:
                    dve(lambda e: e.tensor_tensor(out=MACC.ap[:, oc, :], in0=PS.f[:, b_y, :], in1=gt.ap, op=ALU.mult),
                        [PS.res[b_y], gt.res], [MACC.res])
                else:
                    mt = r_bf.next()
                    dve(lambda e: e.scalar_tensor_tensor(out=mt.ap, in0=PS.f[:, b_y, :], scalar=sc, in1=gt.ap,
                                                         op0=(ALU.mult if mode == "scale" else ALU.add), op1=ALU.mult),
                        [PS.res[b_y], PRM.res, gt.res], [mt.res])
                    pool(lambda e: e.tensor_tensor(out=MACC.ap[:, oc, :], in0=MACC.ap[:, oc, :], in1=mt.ap, op=ALU.add),
                         [MACC.res, mt.res], [MACC.res])

            out_and_gate("oa", YA, 6, None, None, True)

            for g in range(4):
                w = 2 << g
                for h in range(2):
                    oc = 2 * g + h
                    b_p = inproj(3, oc)
                    pool(lambda e, h=h, oc=oc: e.tensor_copy(out=Pt.ap[:, h, 0:15], in_=HP.ap[:, oc, 0:15]),
                         [HP.res], [Pt.res])
                    act(lambda e, b=b_p, h=h, oc=oc: e.activation(out=Pt.ap[:, h, 15:15 + T], in_=PS.f[:, b, :],
                                                                  func=AF.Identity, bias=bias(3, oc), scale=1.0),
                        [PS.res[b_p], PRM.res, Pt.res], [Pt.res])
                    PS.release(b_p)
                    pool(lambda e, h=h, oc=oc: e.tensor_copy(out=HP.ap[:, oc, 0:15], in_=Pt.ap[:, h, T:T + 15]),
                         [Pt.res], [HP.res])
                src = Pt
                lo = 0
                bufs = [Sa, Sb]
                for i in range(g + 1):
                    sh = 1 << i
                    dst = bufs[i % 2]
                    lo2 = lo + sh
                    dve(lambda e, src=src, dst=dst, lo2=lo2, sh=sh: e.tensor_tensor(
                        out=dst.ap[:, :, lo2:T + 15], in0=src.ap[:, :, lo2:T + 15], in1=src.ap[:, :, lo2 - sh:T + 15 - sh],
                        op=ALU.add), [src.res], [dst.res])
                    src = dst
                    lo = lo2
                pl = r_pooled.next()
                dve(lambda e, src=src, pl=pl, w=w: e.scalar_tensor_tensor(
                    out=pl.ap, in0=src.ap[:, :, 15:15 + T], scalar=1.0 / w, in1=Pt.ap[:, :, 15:15 + T],
                    op0=ALU.mult, op1=ALU.subtract), [src.res, Pt.res], [pl.res])
                if c == 0:
                    nfx = w - 1
                    tmpf = r_cv.next()
                    for h in range(2):
                        dve(lambda e, src=src, h=h, g=g, nfx=nfx: e.tensor_tensor(
                            out=tmpf.ap[:, h * 16:h * 16 + nfx], in0=src.ap[:, h, 15:15 + nfx], in1=rcnt.ap[:, g, 0:nfx],
                            op=ALU.mult), [src.res, rcnt.res, tmpf.res], [tmpf.res])
                        dve(lambda e, pl=pl, h=h, nfx=nfx: e.tensor_tensor(
                            out=pl.ap[:, h, 0:nfx], in0=tmpf.ap[:, h * 16:h * 16 + nfx], in1=Pt.ap[:, h, 15:15 + nfx],
                            op=ALU.subtract), [tmpf.res, Pt.res, pl.res], [pl.res])
                wt = ws.get(bl[("wp", g)])
                for h in range(2):
                    oc = 2 * g + h
                    b_y = PS.alloc()
                    for k in range(2):
                        mm(PS.f[:, b_y, :], wt.ap[:, k, h * P:(h + 1) * P], pl.ap[:, k, :], k == 0, k == 1,
                           [wt.res, pl.res], [PS.res[b_y]])
                    b_g = inproj(7, oc)
                    gt = r_bf.next()
                    act(lambda e, b=b_g, gt=gt, oc=oc: e.activation(out=gt.ap, in_=PS.f[:, b, :], func=AF.Sigmoid,
                                                                    bias=bias(7, oc), scale=1.0),
                        [PS.res[b_g], PRM.res], [gt.res])
                    PS.release(b_g)
                    merge(b_y, gt, oc, "scale", O_SP, False)
                    PS.release(b_y)

            b_s1 = PS.alloc()
            b_s2 = PS.alloc()
            pend = None
            pend2 = None
            for oc in range(KC + 2):
                if oc < KC:
                    b_gbb = inproj(5, oc)
                    sgt = r_bf.next()
                    act(lambda e, b=b_gbb, sgt=sgt, oc=oc: e.activation(out=sgt.ap, in_=PS.f[:, b, :], func=AF.Sigmoid,
                                                                        bias=bias(5, oc), scale=1.0),
                        [PS.res[b_gbb], PRM.res], [sgt.res])
                    PS.release(b_gbb)
                    b_ga = inproj(4, oc)
                    vh = r_vh.next()
                    pool(lambda e, vh=vh, oc=oc: e.tensor_copy(out=vh.ap[:, 0:30], in_=HC.ap[:, oc, 0:30]),
                         [HC.res], [vh.res])
                    dve(lambda e, b=b_ga, vh=vh, sgt=sgt, oc=oc: e.scalar_tensor_tensor(
                        out=vh.ap[:, 30:T + 30], in0=PS.f[:, b, :], scalar=bias(4, oc), in1=sgt.ap,
                        op0=ALU.add, op1=ALU.mult), [PS.res[b_ga], PRM.res, sgt.res, vh.res], [vh.res])
                    PS.release(b_ga)
                    pool(lambda e, vh=vh, oc=oc: e.tensor_copy(out=HC.ap[:, oc, 0:30], in_=vh.ap[:, T:T + 30]),
                         [vh.res], [HC.res])
                    cur = (oc, vh)
                else:
                    cur = None
                if pend2 is not None:
                    poc = pend2
                    sq = r_bf.next()
                    dve(lambda e, poc=poc, sq=sq: e.tensor_tensor(out=sq.ap, in0=CVB.ap[:, poc, :], in1=CVB.ap[:, poc, :],
                                                                  op=ALU.mult), [CVB.res], [sq.res])
                    fw.op("pe", lambda e, poc=poc: e.matmul(PS.f[:, b_s1, :], lhsT=ones_b.ap, rhs=CVB.ap[:, poc, :],
                                                            start=(poc == 0), stop=(poc == KC - 1)),
                          [ones_b.res, CVB.res], [PS.res[b_s1]], inc=True)
                    fw.op("pe", lambda e, poc=poc, sq=sq: e.matmul(PS.f[:, b_s2, :], lhsT=ones_b.ap, rhs=sq.ap,
                                                                   start=(poc == 0), stop=(poc == KC - 1)),
                          [ones_b.res, sq.res], [PS.res[b_s2]], inc=True)
                    pend2 = None
                if pend is not None:
                    poc, pvh = pend
                    dve(lambda e, poc=poc: e.tensor_tensor(
                        out=DG31.ap, in0=ident_f.ap.unsqueeze(1).to_broadcast([P, 31, P]),
                        in1=PRM.ap[:, O_CC + poc * 31:O_CC + poc * 31 + 31].unsqueeze(2).to_broadcast([P, 31, P]),
                        op=ALU.mult), [ident_f.res, PRM.res], [DG31.res])
                    b_cv = PS.alloc()
                    for k in range(31):
                        mm(PS.f[:, b_cv, :], DG31.ap[:, k, :], pvh.ap[:, k:k + T], k == 0, k == 30,
                           [DG31.res, pvh.res], [PS.res[b_cv]])
                    act(lambda e, b=b_cv, poc=poc: e.activation(out=CVB.ap[:, poc, :], in_=PS.f[:, b, :], func=AF.Identity,
                                                                bias=PRM.ap[:, O_CCB + poc:O_CCB + poc + 1], scale=1.0),
                        [PS.res[b_cv], PRM.res], [CVB.res])
                    PS.release(b_cv)
                    pend2 = poc
                pend = cur
            act(lambda e: e.activation(out=st_mean.ap, in_=PS.f[:, b_s1, :], func=AF.Copy, scale=1.0 / D),
                [PS.res[b_s1]], [st_mean.res])
            PS.release(b_s1)
            dve(lambda e: e.tensor_tensor(out=st_mr.ap, in0=st_mean.ap, in1=st_mean.ap, op=ALU.mult),
                [st_mean.res], [st_mr.res])
            dve(lambda e: e.scalar_tensor_tensor(out=st_var.ap, in0=PS.f[:, b_s2, :], scalar=1.0 / D, in1=st_mr.ap,
                                                 op0=ALU.mult, op1=ALU.subtract), [PS.res[b_s2], st_mr.res], [st_var.res])
            PS.release(b_s2)
            act(lambda e: e.activation(out=st_var.ap, in_=st_var.ap, func=AF.Sqrt, bias=EPS, scale=1.0),
                [st_var.res], [st_var.res])
            dve(lambda e: e.reciprocal(out=st_var.ap, in_=st_var.ap), [st_var.res], [st_var.res])
            dve(lambda e: e.tensor_tensor(out=st_mr.ap, in0=st_mean.ap, in1=st_var.ap, op=ALU.mult),
                [st_mean.res, st_var.res], [st_mr.res])
            VN = PL1
            for oc in range(KC):
                t1 = r_cv.next()
                dve(lambda e, oc=oc, t1=t1: e.tensor_tensor(out=t1.ap, in0=CVB.ap[:, oc, :], in1=st_var.ap, op=ALU.mult),
                    [CVB.res, st_var.res], [t1.res])
                pool(lambda e, t1=t1: e.tensor_tensor(out=t1.ap, in0=t1.ap, in1=st_mr.ap, op=ALU.subtract),
                     [t1.res, st_mr.res], [t1.res])
                act(lambda e, oc=oc, t1=t1: e.activation(out=VN.ap[:, oc, :], in_=t1.ap, func=AF.Silu,
                                                         bias=PRM.ap[:, O_LB + oc:O_LB + oc + 1],
                                                         scale=PRM.ap[:, O_LG + oc:O_LG + oc + 1]),
                    [t1.res, PRM.res], [VN.res])
            out_and_gate("oc", VN, 8, "bias", O_BOC, False)

            wo = [ws.get(bl[("wo", q)]) for q in range(4)]
            for j in range(TT):
                tg = c * TT + j
                tok0 = tg * P
                src_d = x_d if l == 0 else X2_d
                rd = [] if l == 0 else [r_X2[c]]
                fw.dma("sp", lambda e: e.dma_start(out=XRES.ap, in_=src_d[tok0:tok0 + P, :]), XRES.res,
                       reads=rd, writes=[XRES.res])
                R = r_R.next()
                for hf in range(2):
                    b_h = PS.alloc()
                    for qq in range(2):
                        q = hf * 2 + qq
                        for k in range(KC):
                            mm(PS.f[:, b_h, qq * WB:(qq + 1) * WB], MACC.ap[:, k, j * P:(j + 1) * P], wo[q].ap[:, k, :],
                               k == 0, k == KC - 1, [MACC.res, wo[q].res], [PS.res[b_h]])
                    dve(lambda e, b=b_h, hf=hf: e.scalar_tensor_tensor(
                        out=R.ap[:, hf * 512:(hf + 1) * 512], in0=XRES.ap[:, hf * 512:(hf + 1) * 512], scalar=ALPHA,
                        in1=PS.f[:, b, :], op0=ALU.mult, op1=ALU.add), [XRES.res, PS.res[b_h], R.res], [R.res])
                    PS.release(b_h)
                ln_tokmajor(R, 0, 1, "pool")
                fw.dma("sp", lambda e: e.dma_start(out=X1_d[tok0:tok0 + P, :], in_=R.ap), R.res,
                       reads=[R.res], cwrites=[r_X1[c]])
                x1b = r_x1b.next()
                act(lambda e: e.copy(out=x1b.ap, in_=R.ap), [R.res], [x1b.res])
                for half in range(2):
                    b_t = PS.alloc()
                    for kk in range(4):
                        kc = half * 4 + kk
                        fw.op("pe", lambda e, kc=kc, kk=kk: e.transpose(out=PS.f[:, b_t, kk * P:(kk + 1) * P],
                                                                        in_=R.ap[:, kc * P:(kc + 1) * P],
                                                                        identity=ident_f.ap),
                              [R.res, ident_f.res], [PS.res[b_t]], inc=(kk == 3))
                    act(lambda e, b=b_t, half=half: e.copy(out=X1T.ap[:, half * 4:(half + 1) * 4, :],
                                                           in_=PS.f[:, b, :].rearrange("p (k t) -> p k t", t=P)),
                        [PS.res[b_t]], [X1T.res])
                    PS.release(b_t)
                b_l = PS.alloc()
                for kc in range(KC):
                    mm(PS.f[:, b_l, 0:E], X1T.ap[:, kc, :], WR.ap[:, kc, :], kc == 0, kc == KC - 1,
                       [X1T.res, WR.res], [PS.res[b_l]])
                sm = r_small.next()
                LG = sm.ap[:, 0:32]
                MX8 = sm.ap[:, 32:40]
                RANKF = sm.ap[:, 64:96]
                VAL = sm.ap[:, 96:128]
                DA = sm.ap[:, 128:160]
                JUNK = sm.ap[:, 160:192]
                DEST4 = sm.ap[:, 192:196]
                VAL4 = sm.ap[:, 196:200]
                EX4 = sm.ap[:, 200:204]
                SUM = sm.ap[:, 204:205]
                NEGMX = sm.ap[:, 205:206]
                RSM = sm.ap[:, 206:207]
                MSKb = sm.ap[:, 256:272].bitcast(BF16)
                sr = [sm.res]
                dve(lambda e: e.tensor_tensor(out=LG, in0=PS.f[:, b_l, 0:E], in1=BRT.ap, op=ALU.add),
                    [PS.res[b_l], BRT.res], sr)
                dve(lambda e: e.max(out=MX8, in_=LG), sr, sr)
                dve(lambda e: e.tensor_scalar(out=MSKb, in0=LG, scalar1=MX8[:, 3:4], scalar2=None, op0=ALU.is_ge), sr, sr)
                mm(PS.f[:, b_l, 64:64 + E], triu_b.ap, MSKb, True, True, [triu_b.res, sm.res], [PS.res[b_l]])
                mm(PS.f[:, b_l, 128:128 + E], ones_b.ap, MSKb, True, True, [ones_b.res, sm.res], [PS.res[b_l]])
                dve(lambda e: e.tensor_tensor(out=RANKF, in0=PS.f[:, b_l, 64:64 + E], in1=BASE.ap, op=ALU.add),
                    [PS.res[b_l], BASE.res] + sr, sr)
                dve(lambda e: e.tensor_tensor(out=BASE.ap, in0=PS.f[:, b_l, 128:128 + E], in1=BASE.ap, op=ALU.add),
                    [PS.res[b_l], BASE.res], [BASE.res])
                PS.release(b_l)
                dve(lambda e: e.tensor_single_scalar(out=VAL, in_=RANKF, scalar=float(CAP), op=ALU.is_lt), sr, sr)
                dve(lambda e: e.tensor_tensor(out=DA, in0=RANKF, in1=iota_cap.ap, op=ALU.add), sr + [iota_cap.res], sr)
                dve(lambda e: e.scalar_tensor_tensor(out=DA, in0=DA, scalar=-float(TRASH), in1=VAL,
                                                     op0=ALU.add, op1=ALU.mult), sr, sr)
                dve(lambda e: e.tensor_scalar_add(out=DA, in0=DA, scalar1=float(TRASH)), sr, sr)
                for k in range(4):
                    dve(lambda e, k=k: e.scalar_tensor_tensor(out=JUNK, in0=LG, scalar=MX8[:, k:k + 1], in1=DA,
                                                              op0=ALU.is_equal, op1=ALU.mult,
                                                              accum_out=DEST4[:, k:k + 1]), sr, sr)
                dve(lambda e: e.tensor_single_scalar(out=VAL4, in_=DEST4, scalar=float(TRASH), op=ALU.is_lt), sr, sr)
                dve(lambda e: e.tensor_scalar_mul(out=NEGMX, in0=MX8[:, 0:1], scalar1=-1.0), sr, sr)
                act(lambda e: e.activation(out=EX4, in_=MX8[:, 0:4], func=AF.Exp, bias=NEGMX, scale=1.0,
                                           accum_out=SUM), sr, sr)
                dve(lambda e: e.reciprocal(out=RSM, in_=SUM), sr, sr)
                dve(lambda e: e.scalar_tensor_tensor(out=GATES.ap[:, tg, :], in0=EX4, scalar=RSM, in1=VAL4,
                                                     op0=ALU.mult, op1=ALU.mult), sr + [GATES.res], [GATES.res])
                dve(lambda e: e.tensor_copy(out=DESTI.ap[:, tg, :], in_=DEST4), sr + [DESTI.res], [DESTI.res])
                for k in range(4):
                    fw.dma("pool", lambda e, k=k: e.indirect_dma_start(
                        out=XS_d, out_offset=bass.IndirectOffsetOnAxis(ap=DESTI.ap[:, tg, k:k + 1], axis=0),
                        in_=x1b.ap, in_offset=None), x1b.res, reads=[x1b.res, DESTI.res], cwrites=[r_XS])

        def phase_b(l):
            ws = WStream(ringB, NRB, 10)
            bl = {}
            for e_ in range(E):
                for fc in range(KC):
                    bl[("gu", e_, fc)] = ws.add(wcols(w_gu_d[l, e_], fc * WB))
                for q in range(4):
                    bl[("dn", e_, q)] = ws.add(wcols(w_dn_d[l, e_], q * WB))
            XSv = XS_d[0:NSLOT, :].rearrange("(e i p) d -> e p i d", e=E, p=P)
            YSv = YS_d[0:NSLOT, :].rearrange("(e i p) d -> e i p d", e=E, p=P)

            def load_x(e_):
                xe = r_xe.next()
                fw.dma("sp", lambda e: e.dma_start(out=xe.ap, in_=XSv[e_]), xe.res, reads=[r_XS], writes=[xe.res])
                bd = r_bd.next()
                fw.dma("sp", lambda e: e.dma_start(out=bd.ap, in_=bdn_d[l, e_, :].partition_broadcast(P)), bd.res,
                       writes=[bd.res])
                return xe, bd

            nxt = load_x(0)
            for e_ in range(E):
                xe, bd = nxt
                if e_ + 1 < E:
                    nxt = load_x(e_ + 1)
                xet = r_xet.next()
                for kc in range(KC):
                    b = PS.alloc()
                    for i in range(CT):
                        fw.op("pe", lambda e, i=i, kc=kc, b=b: e.transpose(out=PS.b[:, b, i * P:(i + 1) * P],
                                                                           in_=xe.ap[:, i, kc * P:(kc + 1) * P],
                                                                           identity=ident_b.ap),
                              [xe.res, ident_b.res], [PS.res[b]], inc=(i == CT - 1))
                    eng = "act" if kc % 2 == 0 else "dve"
                    if eng == "act":
                        act(lambda e, b=b, kc=kc: e.copy(out=xet.ap[:, kc, :], in_=PS.b[:, b, 0:CAP]), [PS.res[b]], [xet.res])
                    else:
                        dve(lambda e, b=b, kc=kc: e.tensor_copy(out=xet.ap[:, kc, :], in_=PS.b[:, b, 0:CAP]), [PS.res[b]], [xet.res])
                    PS.release(b)
                actt = r_actt.next()
                for fc in range(KC):
                    wt = ws.get(bl[("gu", e_, fc)])
                    wv = wt.ap.rearrange("p k (f two) -> p k two f", two=2)
                    bg = [PS.alloc() for _ in range(NH)]
                    bu = [PS.alloc() for _ in range(NH)]
                    for gu, banks in ((0, bg), (1, bu)):
                        for nh in range(NH):
                            for k in range(KC):
                                mm(PS.f[:, banks[nh], 0:CH], wv[:, k, gu, :], xet.ap[:, k, nh * CH:(nh + 1) * CH],
                                   k == 0, k == KC - 1, [wt.res, xet.res], [PS.res[banks[nh]]])
                    cg = O_BGU + e_ * 16 + fc
                    cu = O_BGU + e_ * 16 + 8 + fc
                    gt = r_g.next()
                    sg = r_sg.next()
                    ut = r_u.next()
                    a1 = r_a1.next()
                    for nh in range(NH):
                        sl = slice(nh * CH, (nh + 1) * CH)
                        dve(lambda e, nh=nh, sl=sl: e.tensor_scalar(out=gt.ap[:, sl], in0=PS.f[:, bg[nh], 0:CH],
                                                                    scalar1=PRM.ap[:, cg:cg + 1], scalar2=7.0,
                                                                    op0=ALU.add, op1=ALU.min),
                            [PS.res[bg[nh]], PRM.res, gt.res], [gt.res])
                        act(lambda e, nh=nh, sl=sl: e.activation(out=ut.ap[:, sl], in_=PS.f[:, bu[nh], 0:CH],
                                                                 func=AF.Identity, bias=PRM.ap[:, cu:cu + 1], scale=1.0),
                            [PS.res[bu[nh]], PRM.res, ut.res], [ut.res])
                    for b in bg + bu:
                        PS.release(b)
                    act(lambda e: e.activation(out=sg.ap, in_=gt.ap, func=AF.Sigmoid, scale=1.702), [gt.res], [sg.res])
                    pool(lambda e: e.tensor_scalar(out=ut.ap, in0=ut.ap, scalar1=7.0, scalar2=-7.0,
                                                   op0=ALU.min, op1=ALU.max), [ut.res], [ut.res])
                    pool(lambda e: e.tensor_tensor(out=a1.ap, in0=gt.ap, in1=sg.ap, op=ALU.mult),
                         [gt.res, sg.res], [a1.res])
                    dve(lambda e, fc=fc: e.scalar_tensor_tensor(out=actt.ap[:, fc, :], in0=ut.ap, scalar=1.0, in1=a1.ap,
                                                                op0=ALU.add, op1=ALU.mult),
                        [ut.res, a1.res], [actt.res])
                wd = [ws.get(bl[("dn", e_, q)]) for q in range(4)]
                for i in range(CT):
                    yo = r_yo.next()
                    for hf in range(2):
                        b = PS.alloc()
                        for qq in range(2):
                            q = hf * 2 + qq
                            for k in range(KC):
                                mm(PS.f[:, b, qq * WB:(qq + 1) * WB], actt.ap[:, k, i * P:(i + 1) * P], wd[q].ap[:, k, :],
                                   k == 0, k == KC - 1, [actt.res, wd[q].res], [PS.res[b]])
                        dve(lambda e, b=b, hf=hf: e.tensor_tensor(out=yo.ap[:, hf * 512:(hf + 1) * 512], in0=PS.f[:, b, :],
                                                                  in1=bd.ap[:, hf * 512:(hf + 1) * 512], op=ALU.add),
                            [PS.res[b], bd.res, yo.res], [yo.res])
                        PS.release(b)
                    fw.dma("sp", lambda e, i=i: e.dma_start(out=YSv[e_, i], in_=yo.ap), yo.res,
                           reads=[yo.res], cwrites=[r_YS])

        for l in range(NL):
            layer_setup(l)
            for c in range(NCH):
                if l == 0:
                    for j in range(TT):
                        tok0 = (c * TT + j) * P
                        acc = r_acc.next()
                        fw.dma("sp", lambda e: e.dma_start(out=acc.ap, in_=x_d[tok0:tok0 + P, :]), acc.res,
                               writes=[acc.res])
                        make_xT(acc, j)
                else:
                    for j in range(TT):
                        phase_c_tile(l - 1, c, j, False)
                phase_a(l, c)
            fw.barrier(allres)
            phase_b(l)
            fw.barrier(allres)
        for i in (2, 3):
            fw.dma("sp", lambda e, i=i: e.dma_start(out=LNP.ap[:, i, :], in_=lnp_d[NL - 1, i, :].partition_broadcast(P)),
                   LNP.res, writes=[LNP.res])
        for c in range(NCH):
            for j in range(TT):
                phase_c_tile(NL - 1, c, j, True)
        fw.barrier(allres)
        build_program.stats = dict(nins=fw.nins, nwaits=fw.nwaits, ndsem=fw.ndsem, a_end=A_END, b_end=B_END)
    return nc


def host_prm(inp, l0, l1):
    NL = l1 - l0
    prm = np.zeros((NL, P, NPRM), np.float32)
    for i, l in enumerate(range(l0, l1)):
        def pc(v):
            return np.asarray(v, np.float32).reshape(-1, P).T
        prm[i, :, O_BIN:O_BIN + 72] = pc(inp["b_in"][l])
        ca = np.asarray(inp["conv_a"][l], np.float32).reshape(3, KC, P)
        prm[i, :, O_CA:O_CA + 24] = ca.transpose(2, 1, 0).reshape(P, 24)
        prm[i, :, O_SP:O_SP + 8] = pc(inp["scale_pool"][l])
        cc = np.asarray(inp["conv_c"][l], np.float32).reshape(31, KC, P)
        prm[i, :, O_CC:O_CC + 248] = cc.transpose(2, 1, 0).reshape(P, 248)
        prm[i, :, O_CCB:O_CCB + 8] = pc(inp["conv_c_b"][l])
        prm[i, :, O_LG:O_LG + 8] = pc(inp["ln_c_g"][l])
        prm[i, :, O_LB:O_LB + 8] = pc(inp["ln_c_b"][l])
        prm[i, :, O_BOC:O_BOC + 8] = pc(inp["b_out_c"][l])
        bg = np.asarray(inp["b_gu"][l], np.float32).reshape(E, KC, P, 2)
        prm[i, :, O_BGU:O_BGU + 512] = bg.transpose(2, 0, 3, 1).reshape(P, 512)
    return prm


def make_in_maps(inp, xs, l0, l1):
    sl = slice(l0, l1)
    f = lambda a: np.ascontiguousarray(np.asarray(a, np.float32))
    shared = {
        "w_in": f(inp["w_in"][sl]), "w_out_a": f(inp["w_out_a"][sl]), "w_pool": f(inp["w_pool"][sl]),
        "w_out_c": f(inp["w_out_c"][sl]), "w_o": f(inp["w_o"][sl]), "w_router": f(inp["w_router"][sl]),
        "w_gu": f(inp["w_gu"][sl]), "w_down": f(inp["w_down"][sl]),
        "prm": host_prm(inp, l0, l1),
        "lnp": f(np.stack([inp["ln1_g"][sl], inp["ln1_b"][sl], inp["ln2_g"][sl], inp["ln2_b"][sl]], axis=1)),
        "b_router": f(inp["b_router"][sl]), "b_down": f(inp["b_down"][sl]),
    }
    return [dict(shared, x=f(x)) for x in xs]


CAP_FULL = 640
_prog_cache = {}


def run_layers(inp, xs, l0, l1, S, CAP):
    key = (S, CAP, l1 - l0)
    if key not in _prog_cache:
        _prog_cache[key] = build_program(S, CAP, l1 - l0)
    nc = _prog_cache[key]
    in_maps = make_in_maps(inp, xs, l0, l1)
    res = run_bass_kernel_spmd(nc, in_maps, core_ids=list(range(len(xs))))
    return [np.asarray(r["out"]) for r in res.results]


def kernel(**inputs):
    x = np.asarray(inputs["x"], np.float32)
    Bn, S, _ = x.shape
    xs = [x[b] for b in range(Bn)]
    outs = run_layers(inputs, xs, 0, DEPTH, S, CAP_FULL)
    return np.stack(outs, axis=0).astype(np.float32)
```

```python
import numpy as np
import concourse.bass as bass
import concourse.mybir as mybir
from concourse.bass_utils import run_bass_kernel_spmd
from contextlib import ExitStack

F32 = mybir.dt.float32
BF16 = mybir.dt.bfloat16
I32 = mybir.dt.int32
AF = mybir.ActivationFunctionType
ALU = mybir.AluOpType

P = 128
D = 1024
KC = 8
E = 32
T = 512
TT = 4
DEPTH = 4
ALPHA = (2.0 * DEPTH) ** 0.25
EPS = 1e-5
WB = 256
NPRM = 896
O_BIN, O_CA, O_SP, O_CC, O_CCB, O_LG, O_LB, O_BOC, O_BGU = 0, 72, 96, 104, 352, 360, 368, 376, 384


class Res:
    __slots__ = ("name", "lw", "rd", "dsem", "dcnt")

    def __init__(self, name):
        self.name = name
        self.lw = {}
        self.rd = {}
        self.dsem = None
        self.dcnt = 0


class FW:
    def __init__(self, nc, es):
        self.nc = nc
        self.es = es
        self.sems = {}
        self.eng = {}
        for name, h in (("pe", nc.tensor), ("act", nc.scalar), ("dve", nc.vector),
                        ("pool", nc.gpsimd), ("sp", nc.sync)):
            key = "S_" + name
            self.sems[key] = es.enter_context(nc.semaphore(key))
            self.eng[name] = dict(h=h, key=key, tick=0, known={})
        self.ndsem = 0
        self.nins = 0
        self.nwaits = 0

    def _emit_waits(self, ename, reads, writes, cwrites):
        E_ = self.eng[ename]
        own = E_["key"]
        waits = {}

        def need(k, c):
            if waits.get(k, 0) < c:
                waits[k] = c
        raw_own = 0
        for r in reads:
            for k, c in r.lw.items():
                if k == own:
                    raw_own = max(raw_own, c)
                else:
                    need(k, c)
        for w in writes:
            for k, c in w.lw.items():
                if k != own:
                    need(k, c)
            for k, c in w.rd.items():
                if k != own:
                    need(k, c)
        for w in cwrites:
            for k, c in w.rd.items():
                if k != own:
                    need(k, c)
        if raw_own and ename != "pe":
            need(own, raw_own)
        for k, c in waits.items():
            if E_["known"].get(k, 0) < c:
                E_["h"].wait_ge(self.sems[k], c)
                E_["known"][k] = c
                self.nwaits += 1

    def _mark(self, tok, reads, writes, cwrites):
        k, c = tok
        for r in reads:
            if r.rd.get(k, 0) < c:
                r.rd[k] = c
        for w in writes:
            w.lw = {k: c}
            w.rd = {}
        for w in cwrites:
            if w.lw.get(k, 0) < c:
                w.lw[k] = c

    def op(self, ename, fn, reads=(), writes=(), inc=True):
        E_ = self.eng[ename]
        self._emit_waits(ename, reads, writes, ())
        ins = fn(E_["h"])
        self.nins += 1
        if inc:
            E_["tick"] += 1
            ins.then_inc(self.sems[E_["key"]], 1)
            tok = (E_["key"], E_["tick"])
        else:
            tok = (E_["key"], E_["tick"] + 1)
        self._mark(tok, reads, writes, ())
        return ins

    def dma(self, ename, fn, sbres, reads=(), writes=(), cwrites=()):
        E_ = self.eng[ename]
        if sbres.dsem is None:
            key = "D%d" % self.ndsem
            self.ndsem += 1
            self.sems[key] = self.es.enter_context(self.nc.semaphore(key))
            sbres.dsem = key
        self._emit_waits(ename, reads, writes, cwrites)
        ins = fn(E_["h"])
        self.nins += 1
        sbres.dcnt += 16
        ins.then_inc(self.sems[sbres.dsem], 16)
        self._mark((sbres.dsem, sbres.dcnt), reads, writes, cwrites)
        return ins

    def barrier(self, all_res):
        waits = {}
        for nm, E_ in self.eng.items():
            if E_["tick"]:
                waits[E_["key"]] = E_["tick"]
        for r in all_res:
            for k, c in list(r.lw.items()) + list(r.rd.items()):
                if waits.get(k, 0) < c:
                    waits[k] = c
        for nm, E_ in self.eng.items():
            for k, c in waits.items():
                if k == E_["key"]:
                    continue
                if E_["known"].get(k, 0) < c:
                    E_["h"].wait_ge(self.sems[k], c)
                    E_["known"][k] = c
                    self.nwaits += 1


class Tile:
    __slots__ = ("ap", "res")

    def __init__(self, ap, name):
        self.ap = ap
        self.res = Res(name)


class Carver:
    def __init__(self, arena, base, limit, allres):
        self.arena = arena
        self.off = base
        self.limit = limit
        self.allres = allres

    def get(self, name, shape, dtype):
        esz = 4 if dtype in (F32, I32) else 2
        n = 1
        for s in shape[1:]:
            n *= s
        nb = (n * esz + 31) // 32 * 32
        o = self.off
        self.off += nb
        assert self.off <= self.limit, ("SBUF arena overflow", name, self.off, self.limit)
        ap = self.arena[:, o // 2:(o + n * esz) // 2]
        if dtype != BF16:
            ap = ap.bitcast(dtype)
        if len(shape) == 3:
            ap = ap.rearrange("p (a b) -> p a b", b=shape[2])
        elif len(shape) == 4:
            ap = ap.rearrange("p (a b c) -> p a b c", b=shape[2], c=shape[3])
        t = Tile(ap, name)
        self.allres.append(t.res)
        return t

    def ring(self, name, n, shape, dtype):
        return Ring([self.get("%s%d" % (name, i), shape, dtype) for i in range(n)])


class Ring:
    def __init__(self, tiles):
        self.tiles = tiles
        self.i = 0

    def next(self):
        t = self.tiles[self.i % len(self.tiles)]
        self.i += 1
        return t


class PsumAlloc:
    def __init__(self, psum_f32):
        self.f = psum_f32
        self.b = psum_f32.bitcast(BF16)
        self.res = [Res("psum%d" % i) for i in range(8)]
        self.free = list(range(8))

    def alloc(self):
        assert self.free, "PSUM banks exhausted"
        return self.free.pop(0)

    def release(self, i):
        self.free.append(i)


def build_program(S, CAP, NL, first_is_input=True):
    NCH = S // T
    NT = S // P
    NSLOT = E * CAP
    TRASH = NSLOT
    CT = CAP // P
    NH = 2 if CAP > 512 else 1
    CH = CAP // NH
    nc = bass.Bass("TRN2", target_bir_lowering=False)

    def din(name, shape, dt=F32):
        return nc.dram_tensor(name, list(shape), dt, kind="ExternalInput").ap()
    x_d = din("x", [S, D])
    w_in_d = din("w_in", [NL, D, 9 * D])
    w_oa_d = din("w_out_a", [NL, D, D])
    w_pool_d = din("w_pool", [NL, 4, 256, 256])
    w_oc_d = din("w_out_c", [NL, D, D])
    w_o_d = din("w_o", [NL, D, D])
    w_r_d = din("w_router", [NL, D, E])
    w_gu_d = din("w_gu", [NL, E, D, 2 * D])
    w_dn_d = din("w_down", [NL, E, D, D])
    prm_d = din("prm", [NL, P, NPRM])
    lnp_d = din("lnp", [NL, 4, D])
    brt_d = din("b_router", [NL, E])
    bdn_d = din("b_down", [NL, E, D])
    out_d = nc.dram_tensor("out", [S, D], F32, kind="ExternalOutput").ap()
    X1_d = nc.dram_tensor("X1s", [S, D], F32, kind="Internal").ap()
    X2_d = nc.dram_tensor("X2s", [S, D], F32, kind="Internal").ap()
    XS_d = nc.dram_tensor("XSs", [NSLOT + P, D], BF16, kind="Internal").ap()
    YS_d = nc.dram_tensor("YSs", [NSLOT + P, D], F32, kind="Internal").ap()
    r_X1 = [Res("X1c%d" % c) for c in range(NCH)]
    r_X2 = [Res("X2c%d" % c) for c in range(NCH)]
    r_XS = Res("XS")
    r_YS = Res("YS")

    with ExitStack() as es:
        fw = FW(nc, es)
        ARENA_BYTES = 206 * 1024
        arena = es.enter_context(nc.sbuf_tensor("arena", [P, ARENA_BYTES // 2], BF16))
        psum = es.enter_context(nc.psum_tensor("psum", [P, 8, 512], F32))
        PS = PsumAlloc(psum)
        allres = list(PS.res) + r_X1 + r_X2 + [r_XS, r_YS]
        G = Carver(arena, 0, ARENA_BYTES, allres)

        def act(fn, reads, writes):
            return fw.op("act", fn, reads, writes)

        def dve(fn, reads, writes):
            return fw.op("dve", fn, reads, writes)

        def pool(fn, reads, writes):
            return fw.op("pool", fn, reads, writes)

        def mm(out, lhsT, rhs, start, stop, reads, writes):
            return fw.op("pe", lambda e: e.matmul(out, lhsT=lhsT, rhs=rhs, start=start, stop=stop),
                         reads, writes, inc=stop)

        ident_f = G.get("ident_f", [P, P], F32)
        ident_b = G.get("ident_b", [P, P], BF16)
        triu_b = G.get("triu_b", [P, P], BF16)
        ones_b = G.get("ones_b", [P, P], BF16)
        iota_cap = G.get("iota_cap", [P, E], F32)
        rcnt = G.get("rcnt", [P, 4, 16], F32)
        io_t = G.get("io_t", [P, P], F32)
        PRM = G.get("PRM", [P, NPRM], F32)
        LNP = G.get("LNP", [P, 4, D], F32)
        WR = G.get("WR", [P, KC, E], F32)
        BRT = G.get("BRT", [P, E], F32)
        DG3 = G.get("DG3", [P, KC, 3, P], BF16)
        GATES = G.get("GATES", [P, NT, 4], F32)
        DESTI = G.get("DESTI", [P, NT, 4], I32)
        BASE = G.get("BASE", [P, E], F32)
        HA = G.get("HA", [P, KC, 2], BF16)
        HP = G.get("HP", [P, KC, 16], F32)
        HC = G.get("HC", [P, KC, 32], BF16)
        GBASE = G.off

        pool(lambda e: e.iota(io_t.ap, pattern=[[1, P]], base=0, channel_multiplier=-1,
                              allow_small_or_imprecise_dtypes=True), [], [io_t.res])
        dve(lambda e: e.tensor_single_scalar(out=ident_f.ap, in_=io_t.ap, scalar=0.0, op=ALU.is_equal),
            [io_t.res], [ident_f.res])
        dve(lambda e: e.tensor_single_scalar(out=ident_b.ap, in_=io_t.ap, scalar=0.0, op=ALU.is_equal),
            [io_t.res], [ident_b.res])
        dve(lambda e: e.tensor_single_scalar(out=triu_b.ap, in_=io_t.ap, scalar=0.0, op=ALU.is_gt),
            [io_t.res], [triu_b.res])
        dve(lambda e: e.memset(ones_b.ap, 1.0), [], [ones_b.res])
        pool(lambda e: e.iota(iota_cap.ap, pattern=[[CAP, E]], base=0, channel_multiplier=0,
                              allow_small_or_imprecise_dtypes=True), [], [iota_cap.res])
        for g in range(4):
            w = 2 << g
            pool(lambda e, g=g: e.iota(rcnt.ap[:, g, :], pattern=[[1, 16]], base=1, channel_multiplier=0,
                                       allow_small_or_imprecise_dtypes=True), [], [rcnt.res])
            dve(lambda e, g=g, w=w: e.tensor_scalar_min(out=rcnt.ap[:, g, :], in0=rcnt.ap[:, g, :], scalar1=float(w)),
                [rcnt.res], [rcnt.res])
        dve(lambda e: e.reciprocal(out=rcnt.ap, in_=rcnt.ap), [rcnt.res], [rcnt.res])
        A = Carver(arena, GBASE, ARENA_BYTES, allres)
        XT = A.get("XT", [P, KC, T], BF16)
        PL1 = A.get("PL1", [P, KC, T], BF16)
        CVB = A.get("CVB", [P, KC, T], BF16)
        MACC = A.get("MACC", [P, KC, T], BF16)
        DGa = A.get("DGa", [P, 16, P], BF16)
        DGb = A.get("DGb", [P, 15, P], BF16)
        NRA = 7
        ringA = A.ring("wA", NRA, [P, KC, WB], BF16)
        r_bf = A.ring("sbf", 6, [P, T], BF16)
        r_uh = A.ring("uh", 2, [P, T + 2], BF16)
        r_vh = A.ring("vh", 2, [P, T + 30], BF16)
        r_cv = A.ring("cv", 2, [P, T], F32)
        GBASE_PT = A.off
        Pt = A.get("Pt", [P, 2, T + 15], F32)
        Sa = A.get("Sa", [P, 2, T + 15], F32)
        Sb = A.get("Sb", [P, 2, T + 15], F32)
        r_pooled = A.ring("pooled", 4, [P, 2, T], BF16)
        st_mean = A.get("st_mean", [P, T], F32)
        st_var = A.get("st_var", [P, T], F32)
        st_mr = A.get("st_mr", [P, T], F32)
        r_R = A.ring("R", 4, [P, D], F32)
        r_x1b = A.ring("x1b", 4, [P, D], BF16)
        X1T = A.get("X1T", [P, KC, P], F32)
        r_small = A.ring("rt", 4, [P, 288], F32)
        r_lnst = A.ring("lnst", 8, [P, 16], F32)
        r_yg = A.ring("yg", 2, [P, D], F32)
        r_acc = A.ring("acc", 4, [P, D], F32)
        r_xb = A.ring("xb", 4, [P, D], BF16)
        A_END = A.off
        gt8_off = None
        GT8_ap = arena[:, (GBASE_PT) // 2:(GBASE_PT + KC * T * 2) // 2].rearrange("p (a b) -> p a b", b=T)
        GT8_res = [Pt.res, Sa.res]

        B = Carver(arena, GBASE, ARENA_BYTES, allres)
        r_xe = B.ring("xe", 2, [P, CT, D], BF16)
        r_xet = B.ring("xet", 2, [P, KC, CAP], BF16)
        r_actt = B.ring("actt", 2, [P, KC, CAP], BF16)
        NRB = 14
        ringB = B.ring("wB", NRB, [P, KC, WB], BF16)
        r_g = B.ring("bg", 2, [P, CAP], F32)
        r_sg = B.ring("bsg", 2, [P, CAP], F32)
        r_u = B.ring("bu", 2, [P, CAP], F32)
        r_a1 = B.ring("ba1", 2, [P, CAP], F32)
        r_yo = B.ring("yo", 3, [P, D], F32)
        r_bd = B.ring("bd", 2, [P, D], F32)
        B_END = B.off

        zrow = r_R.tiles[0]
        dve(lambda e: e.memset(zrow.ap, 0.0), [], [zrow.res])
        XSz = XS_d.rearrange("(n p) d -> n p d", p=P)
        YSz = YS_d.rearrange("(n p) d -> n p d", p=P)
        zb = zrow.ap.bitcast(BF16)
        for n in range(0, (NSLOT + P) // P, 2):
            nn = min(2, (NSLOT + P) // P - n)
            fw.dma("sp", lambda e, n=n, nn=nn: e.dma_start(
                out=XSz[n:n + nn].rearrange("n p d -> p n d"),
                in_=zb[:, 0:nn * D].rearrange("p (n d) -> p n d", d=D)), zrow.res,
                reads=[zrow.res], cwrites=[r_XS])
        fw.dma("sp", lambda e: e.dma_start(out=YSz[NSLOT // P], in_=zrow.ap), zrow.res,
               reads=[zrow.res], cwrites=[r_YS])

        class WStream:
            def __init__(self, ring, nring, la):
                self.ring = ring
                self.n = nring
                self.la = la
                self.blocks = []
                self.emitted = 0
                self.tiles = {}

            def add(self, src, nk=KC):
                self.blocks.append((src, nk))
                return len(self.blocks) - 1

            def get(self, i):
                lim = min(len(self.blocks), i + 1 + self.la)
                while self.emitted < lim:
                    j = self.emitted
                    t = self.ring.next()
                    src, nk = self.blocks[j]
                    fw.dma("pool", lambda e, t=t, src=src, nk=nk: e.dma_start(out=t.ap[:, 0:nk, :], in_=src), t.res,
                           writes=[t.res])
                    self.tiles[j] = t
                    self.emitted += 1
                return self.tiles[i]

        def wcols(w2d, c0, ncol=WB):
            return w2d.rearrange("(k p) c -> p k c", p=P)[:, :, c0:c0 + ncol]

        def layer_setup(l):
            fw.dma("sp", lambda e: e.dma_start(out=PRM.ap, in_=prm_d[l]), PRM.res, writes=[PRM.res])
            for i in range(4):
                ll = l if i < 2 else l - 1
                if ll < 0:
                    continue
                fw.dma("sp", lambda e, i=i, ll=ll: e.dma_start(out=LNP.ap[:, i, :], in_=lnp_d[ll, i, :].partition_broadcast(P)),
                       LNP.res, writes=[LNP.res])
            fw.dma("sp", lambda e: e.dma_start(out=WR.ap, in_=w_r_d[l].rearrange("(k p) e -> p k e", p=P)),
                   WR.res, writes=[WR.res])
            fw.dma("sp", lambda e: e.dma_start(out=BRT.ap, in_=brt_d[l, :].partition_broadcast(P)),
                   BRT.res, writes=[BRT.res])
            for oc in range(KC):
                dve(lambda e, oc=oc: e.tensor_tensor(
                    out=DG3.ap[:, oc], in0=ident_f.ap.unsqueeze(1).to_broadcast([P, 3, P]),
                    in1=PRM.ap[:, O_CA + oc * 3:O_CA + oc * 3 + 3].unsqueeze(2).to_broadcast([P, 3, P]),
                    op=ALU.mult), [ident_f.res, PRM.res], [DG3.res])
            dve(lambda e: e.memset(BASE.ap, 0.0), [], [BASE.res])
            dve(lambda e: e.memset(HA.ap, 0.0), [], [HA.res])
            dve(lambda e: e.memset(HP.ap, 0.0), [], [HP.res])
            dve(lambda e: e.memset(HC.ap, 0.0), [], [HC.res])

        def make_xb(src_tile):
            xb = r_xb.next()
            act(lambda e: e.copy(out=xb.ap, in_=src_tile.ap), [src_tile.res], [xb.res])
            return xb

        def xT_from_xb(xb, j):
            b = PS.alloc()
            for kc in range(KC):
                fw.op("pe", lambda e, kc=kc: e.transpose(out=PS.b[:, b, kc * P:(kc + 1) * P],
                                                         in_=xb.ap[:, kc * P:(kc + 1) * P], identity=ident_b.ap),
                      [xb.res, ident_b.res], [PS.res[b]], inc=(kc == KC - 1))
            act(lambda e: e.copy(out=XT.ap[:, :, j * P:(j + 1) * P],
                                 in_=PS.b[:, b, :].rearrange("p (k t) -> p k t", t=P)),
                [PS.res[b]], [XT.res])
            PS.release(b)

        def ln_stages(R, gi, bi, eng_gb):
            sm = r_lnst.next()
            st = sm.ap[:, 0:12].rearrange("p (a b) -> p a b", b=6)
            ag = sm.ap[:, 12:14]
            sd = sm.ap[:, 14:15]
            rs = sm.ap[:, 15:16]

            def s_stats():
                for h in range(2):
                    dve(lambda e, h=h: e.bn_stats(out=st[:, h, :], in_=R.ap[:, h * 512:(h + 1) * 512]),
                        [R.res], [sm.res])
                dve(lambda e: e.bn_aggr(out=ag, in_=st), [sm.res], [sm.res])
            return [
                s_stats,
                lambda: act(lambda e: e.activation(out=sd, in_=ag[:, 1:2], func=AF.Sqrt, bias=EPS, scale=1.0),
                            [sm.res], [sm.res]),
                lambda: dve(lambda e: e.reciprocal(out=rs, in_=sd), [sm.res], [sm.res]),
                lambda: dve(lambda e: e.tensor_scalar(out=R.ap, in0=R.ap, scalar1=ag[:, 0:1], scalar2=rs,
                                                      op0=ALU.subtract, op1=ALU.mult), [R.res, sm.res], [R.res]),
                lambda: fw.op(eng_gb, lambda e: e.tensor_tensor(out=R.ap, in0=R.ap, in1=LNP.ap[:, gi, :], op=ALU.mult),
                              [R.res, LNP.res], [R.res]),
                lambda: fw.op(eng_gb, lambda e: e.tensor_tensor(out=R.ap, in0=R.ap, in1=LNP.ap[:, bi, :], op=ALU.add),
                              [R.res, LNP.res], [R.res]),
            ]

        def ln_tokmajor(R, gi, bi, eng_gb):
            for f in ln_stages(R, gi, bi, eng_gb):
                f()

        def phase_c_steps(c, is_last):
            steps = []
            xbs = [None] * TT
            accs = [r_acc.next() for _ in range(TT)]
            lns = []

            def mk(j):
                tg = c * TT + j
                tok0 = tg * P
                acc = accs[j]
                ygs = {}

                def gather(k):
                    yg = r_yg.next()
                    ygs[k] = yg
                    fw.dma("pool", lambda e: e.indirect_dma_start(
                        out=yg.ap, out_offset=None, in_=YS_d,
                        in_offset=bass.IndirectOffsetOnAxis(ap=DESTI.ap[:, tg, k:k + 1], axis=0)),
                        yg.res, reads=[r_YS, DESTI.res], writes=[yg.res])

                def combine(k):
                    yg = ygs[k]
                    dve(lambda e: e.scalar_tensor_tensor(
                        out=acc.ap, in0=yg.ap, scalar=GATES.ap[:, tg, k:k + 1], in1=acc.ap,
                        op0=ALU.mult, op1=ALU.add), [yg.res, GATES.res, acc.res], [acc.res])

                def s0():
                    fw.dma("sp", lambda e: e.dma_start(out=acc.ap, in_=X1_d[tok0:tok0 + P, :]), acc.res,
                           reads=[r_X1[c]], writes=[acc.res])
                    gather(0)
                    gather(1)
                    act(lambda e: e.activation(out=acc.ap, in_=acc.ap, func=AF.Copy, scale=ALPHA), [acc.res], [acc.res])

                def s1():
                    combine(0)
                    gather(2)
                    combine(1)
                    gather(3)

                def s2():
                    combine(2)
                    combine(3)

                def s_out():
                    if is_last:
                        fw.dma("sp", lambda e: e.dma_start(out=out_d[tok0:tok0 + P, :], in_=acc.ap), acc.res,
                               reads=[acc.res], cwrites=[r_out])
                    else:
                        fw.dma("sp", lambda e: e.dma_start(out=X2_d[tok0:tok0 + P, :], in_=acc.ap), acc.res,
                               reads=[acc.res], cwrites=[r_X2[c]])
                        xbs[j] = make_xb(acc)
                return [s0, s1, s2], s_out
            outs = []
            for j in range(TT):
                pre, so = mk(j)
                steps.extend(pre)
                outs.append(so)
                lns.append(ln_stages(accs[j], 2, 3, "dve"))
                if j % 2 == 1:
                    for fs in zip(lns[j - 1], lns[j]):
                        steps.extend(fs)
                    steps.append(outs[j - 1])
                    steps.append(outs[j])
            return steps, xbs

        r_out = Res("out")
        allres.append(r_out)

        def reg_blocks(l, ws):
            w_in = w_in_d[l]
            bl = {}
            for q in range(4):
                for s_ in (1, 2, 0):
                    bl[("in", s_, q)] = ws.add(wcols(w_in, s_ * D + q * WB))
            for q in range(4):
                bl[("oa", q)] = ws.add(wcols(w_oa_d[l], q * WB))
                bl[("in", 6, q)] = ws.add(wcols(w_in, 6 * D + q * WB))
            for g in range(4):
                bl[("in", 3, g)] = ws.add(wcols(w_in, 3 * D + g * WB))
            for g in range(4):
                bl[("in", 7, g)] = ws.add(wcols(w_in, 7 * D + g * WB))
                bl[("wp", g)] = ws.add(w_pool_d[l, g].rearrange("(k p) e -> p k e", p=P), 2)
            for q in range(4):
                bl[("in", 8, q)] = ws.add(wcols(w_in, 8 * D + q * WB))
            for q in range(4):
                bl[("in", 5, q)] = ws.add(wcols(w_in, 5 * D + q * WB))
                bl[("in", 4, q)] = ws.add(wcols(w_in, 4 * D + q * WB))
            for q in range(4):
                bl[("oc", q)] = ws.add(wcols(w_oc_d[l], q * WB))
            for q in range(4):
                bl[("wo", q)] = ws.add(wcols(w_o_d[l], q * WB))
            return bl

        def phase_a(l, c, ws, bl, bg, bg_fin, deferred):
            t0 = c * T
            def bg_step():
                if bg:
                    bg.pop(0)()

            def flush_deferred():
                n = len(deferred)
                for f in deferred[:n - (TT if False else 0)]:
                    f()
                del deferred[:]

            def inproj(s, oc):
                wt = ws.get(bl[("in", s, oc // 2)])
                b = PS.alloc()
                for k in range(KC):
                    mm(PS.f[:, b, :], wt.ap[:, k, (oc % 2) * P:(oc % 2 + 1) * P], XT.ap[:, k, :],
                       k == 0, k == KC - 1, [wt.res, XT.res], [PS.res[b]])
                return b

            def bias(s, oc):
                col = O_BIN + s * 8 + oc
                return PRM.ap[:, col:col + 1]

            YA = PL1
            pend = None
            for oc in range(KC + 1):
                bg_step()
                if oc < KC:
                    b_gc = inproj(1, oc)
                    gct = r_bf.next()
                    act(lambda e, b=b_gc, gct=gct, oc=oc: e.activation(out=gct.ap, in_=PS.f[:, b, :], func=AF.Identity,
                                                                       bias=bias(1, oc), scale=1.0),
                        [PS.res[b_gc], PRM.res], [gct.res])
                    PS.release(b_gc)
                    b_h = inproj(2, oc)
                    uh = r_uh.next()
                    pool(lambda e, uh=uh, oc=oc: e.tensor_copy(out=uh.ap[:, 0:2], in_=HA.ap[:, oc, :]),
                         [HA.res], [uh.res])
                    dve(lambda e, b=b_h, uh=uh, gct=gct, oc=oc: e.scalar_tensor_tensor(
                        out=uh.ap[:, 2:T + 2], in0=PS.f[:, b, :], scalar=bias(2, oc), in1=gct.ap,
                        op0=ALU.add, op1=ALU.mult), [PS.res[b_h], PRM.res, gct.res, uh.res], [uh.res])
                    PS.release(b_h)
                    pool(lambda e, uh=uh, oc=oc: e.tensor_copy(out=HA.ap[:, oc, :], in_=uh.ap[:, T:T + 2]),
                         [uh.res], [HA.res])
                    b_gb = inproj(0, oc)
                    cur = (oc, uh, b_gb)
                else:
                    cur = None
                if pend is not None:
                    poc, puh, pb_gb = pend
                    b_cv = PS.alloc()
                    for k in range(3):
                        mm(PS.f[:, b_cv, :], DG3.ap[:, poc, k, :], puh.ap[:, k:k + T], k == 0, k == 2,
                           [DG3.res, puh.res], [PS.res[b_cv]])
                    cvt = r_cv.next()
                    act(lambda e, b=b_cv, cvt=cvt: e.copy(out=cvt.ap, in_=PS.f[:, b, :]), [PS.res[b_cv]], [cvt.res])
                    PS.release(b_cv)
                    dve(lambda e, b=pb_gb, cvt=cvt, poc=poc: e.scalar_tensor_tensor(
                        out=YA.ap[:, poc, :], in0=PS.f[:, b, :], scalar=bias(0, poc), in1=cvt.ap,
                        op0=ALU.add, op1=ALU.mult), [PS.res[pb_gb], PRM.res, cvt.res], [YA.res])
                    PS.release(pb_gb)
                pend = cur

            def out_and_gate(wkey, yin, gs, mode, post_scalar_col, first):
                for oc in range(KC):
                    bg_step()
                    wt = ws.get(bl[(wkey, oc // 2)])
                    b_y = PS.alloc()
                    for k in range(KC):
                        mm(PS.f[:, b_y, :], wt.ap[:, k, (oc % 2) * P:(oc % 2 + 1) * P], yin.ap[:, k, :],
                           k == 0, k == KC - 1, [wt.res, yin.res], [PS.res[b_y]])
                    b_g = inproj(gs, oc)
                    gt = r_bf.next()
                    act(lambda e, b=b_g, gt=gt, oc=oc: e.activation(out=gt.ap, in_=PS.f[:, b, :], func=AF.Sigmoid,
                                                                    bias=bias(gs, oc), scale=1.0),
                        [PS.res[b_g], PRM.res], [gt.res])
                    PS.release(b_g)
                    merge(b_y, gt, oc, mode, post_scalar_col, first)
                    PS.release(b_y)

            def merge(b_y, gt, oc, mode, col, first):
                sc = PRM.ap[:, col + oc:col + oc + 1] if col is not None else None
                if first:
                    dve(lambda e: e.tensor_tensor(out=MACC.ap[:, oc, :], in0=PS.f[:, b_y, :], in1=gt.ap, op=ALU.mult),
                        [PS.res[b_y], gt.res], [MACC.res])
                else:
                    mt = r_bf.next()
                    dve(lambda e: e.scalar_tensor_tensor(out=mt.ap, in0=PS.f[:, b_y, :], scalar=sc, in1=gt.ap,
                                                         op0=(ALU.mult if mode == "scale" else ALU.add), op1=ALU.mult),
                        [PS.res[b_y], PRM.res, gt.res], [mt.res])
                    pool(lambda e: e.tensor_tensor(out=MACC.ap[:, oc, :], in0=MACC.ap[:, oc, :], in1=mt.ap, op=ALU.add),
                         [MACC.res, mt.res], [MACC.res])

            out_and_gate("oa", YA, 6, None, None, True)

            flush_deferred()
            pooled = []
            for g in range(4):
                bg_step()
                w = 2 << g
                for h in range(2):
                    oc = 2 * g + h
                    b_p = inproj(3, oc)
                    pool(lambda e, h=h, oc=oc: e.tensor_copy(out=Pt.ap[:, h, 0:15], in_=HP.ap[:, oc, 0:15]),
                         [HP.res], [Pt.res])
                    act(lambda e, b=b_p, h=h, oc=oc: e.activation(out=Pt.ap[:, h, 15:15 + T], in_=PS.f[:, b, :],
                                                                  func=AF.Identity, bias=bias(3, oc), scale=1.0),
                        [PS.res[b_p], PRM.res, Pt.res], [Pt.res])
                    PS.release(b_p)
                    pool(lambda e, h=h, oc=oc: e.tensor_copy(out=HP.ap[:, oc, 0:15], in_=Pt.ap[:, h, T:T + 15]),
                         [Pt.res], [HP.res])
                src = Pt
                lo = 0
                bufs = [Sa, Sb]
                for i in range(g + 1):
                    sh = 1 << i
                    dst = bufs[i % 2]
                    lo2 = lo + sh
                    dve(lambda e, src=src, dst=dst, lo2=lo2, sh=sh: e.tensor_tensor(
                        out=dst.ap[:, :, lo2:T + 15], in0=src.ap[:, :, lo2:T + 15], in1=src.ap[:, :, lo2 - sh:T + 15 - sh],
                        op=ALU.add), [src.res], [dst.res])
                    src = dst
                    lo = lo2
                pl = r_pooled.next()
                dve(lambda e, src=src, pl=pl, w=w: e.scalar_tensor_tensor(
                    out=pl.ap, in0=src.ap[:, :, 15:15 + T], scalar=1.0 / w, in1=Pt.ap[:, :, 15:15 + T],
                    op0=ALU.mult, op1=ALU.subtract), [src.res, Pt.res], [pl.res])
                if c == 0:
                    nfx = w - 1
                    tmpf = r_cv.next()
                    for h in range(2):
                        dve(lambda e, src=src, h=h, g=g, nfx=nfx: e.tensor_tensor(
                            out=tmpf.ap[:, h * 16:h * 16 + nfx], in0=src.ap[:, h, 15:15 + nfx], in1=rcnt.ap[:, g, 0:nfx],
                            op=ALU.mult), [src.res, rcnt.res, tmpf.res], [tmpf.res])
                        dve(lambda e, pl=pl, h=h, nfx=nfx: e.tensor_tensor(
                            out=pl.ap[:, h, 0:nfx], in0=tmpf.ap[:, h * 16:h * 16 + nfx], in1=Pt.ap[:, h, 15:15 + nfx],
                            op=ALU.subtract), [tmpf.res, Pt.res, pl.res], [pl.res])
                pooled.append(pl)
            for oc in range(KC):
                bg_step()
                g, h = oc // 2, oc % 2
                b_g = inproj(7, oc)
                gt = r_bf.next()
                act(lambda e, b=b_g, gt=gt, oc=oc: e.activation(out=gt.ap, in_=PS.f[:, b, :], func=AF.Sigmoid,
                                                                bias=bias(7, oc), scale=1.0),
                    [PS.res[b_g], PRM.res], [gt.res])
                PS.release(b_g)
                wt = ws.get(bl[("wp", g)])
                pl = pooled[g]
                b_y = PS.alloc()
                for k in range(2):
                    mm(PS.f[:, b_y, :], wt.ap[:, k, h * P:(h + 1) * P], pl.ap[:, k, :], k == 0, k == 1,
                       [wt.res, pl.res], [PS.res[b_y]])
                merge(b_y, gt, oc, "scale", O_SP, False)
                PS.release(b_y)

            for oc in range(KC):
                bg_step()
                b_g = inproj(8, oc)
                act(lambda e, b=b_g, oc=oc: e.activation(out=GT8_ap[:, oc, :], in_=PS.f[:, b, :], func=AF.Sigmoid,
                                                         bias=bias(8, oc), scale=1.0),
                    [PS.res[b_g], PRM.res] + GT8_res, GT8_res)
                PS.release(b_g)
            b_s1 = PS.alloc()
            b_s2 = PS.alloc()
            pend = None
            pend2 = None
            for oc in range(KC + 2):
                bg_step()
                if oc < KC:
                    b_gbb = inproj(5, oc)
                    sgt = r_bf.next()
                    act(lambda e, b=b_gbb, sgt=sgt, oc=oc: e.activation(out=sgt.ap, in_=PS.f[:, b, :], func=AF.Sigmoid,
                                                                        bias=bias(5, oc), scale=1.0),
                        [PS.res[b_gbb], PRM.res], [sgt.res])
                    PS.release(b_gbb)
                    b_ga = inproj(4, oc)
                    vh = r_vh.next()
                    pool(lambda e, vh=vh, oc=oc: e.tensor_copy(out=vh.ap[:, 0:30], in_=HC.ap[:, oc, 0:30]),
                         [HC.res], [vh.res])
                    dve(lambda e, b=b_ga, vh=vh, sgt=sgt, oc=oc: e.scalar_tensor_tensor(
                        out=vh.ap[:, 30:T + 30], in0=PS.f[:, b, :], scalar=bias(4, oc), in1=sgt.ap,
                        op0=ALU.add, op1=ALU.mult), [PS.res[b_ga], PRM.res, sgt.res, vh.res], [vh.res])
                    PS.release(b_ga)
                    pool(lambda e, vh=vh, oc=oc: e.tensor_copy(out=HC.ap[:, oc, 0:30], in_=vh.ap[:, T:T + 30]),
                         [vh.res], [HC.res])
                    cur = (oc, vh)
                else:
                    cur = None
                if pend2 is not None:
                    poc = pend2
                    sq = r_bf.next()
                    dve(lambda e, poc=poc, sq=sq: e.tensor_tensor(out=sq.ap, in0=CVB.ap[:, poc, :], in1=CVB.ap[:, poc, :],
                                                                  op=ALU.mult), [CVB.res], [sq.res])
                    fw.op("pe", lambda e, poc=poc: e.matmul(PS.f[:, b_s1, :], lhsT=ones_b.ap, rhs=CVB.ap[:, poc, :],
                                                            start=(poc == 0), stop=(poc == KC - 1)),
                          [ones_b.res, CVB.res], [PS.res[b_s1]], inc=True)
                    fw.op("pe", lambda e, poc=poc, sq=sq: e.matmul(PS.f[:, b_s2, :], lhsT=ones_b.ap, rhs=sq.ap,
                                                                   start=(poc == 0), stop=(poc == KC - 1)),
                          [ones_b.res, sq.res], [PS.res[b_s2]], inc=True)
                    pend2 = None
                if pend is not None:
                    poc, pvh = pend
                    cbase = O_CC + poc * 31
                    dve(lambda e: e.tensor_tensor(
                        out=DGa.ap, in0=ident_f.ap.unsqueeze(1).to_broadcast([P, 16, P]),
                        in1=PRM.ap[:, cbase:cbase + 16].unsqueeze(2).to_broadcast([P, 16, P]),
                        op=ALU.mult), [ident_f.res, PRM.res], [DGa.res])
                    dve(lambda e: e.tensor_tensor(
                        out=DGb.ap, in0=ident_f.ap.unsqueeze(1).to_broadcast([P, 15, P]),
                        in1=PRM.ap[:, cbase + 16:cbase + 31].unsqueeze(2).to_broadcast([P, 15, P]),
                        op=ALU.mult), [ident_f.res, PRM.res], [DGb.res])
                    b_cv = PS.alloc()
                    for k in range(31):
                        dg = DGa if k < 16 else DGb
                        mm(PS.f[:, b_cv, :], dg.ap[:, k % 16, :], pvh.ap[:, k:k + T], k == 0, k == 30,
                           [dg.res, pvh.res], [PS.res[b_cv]])
                    act(lambda e, b=b_cv, poc=poc: e.activation(out=CVB.ap[:, poc, :], in_=PS.f[:, b, :], func=AF.Identity,
                                                                bias=PRM.ap[:, O_CCB + poc:O_CCB + poc + 1], scale=1.0),
                        [PS.res[b_cv], PRM.res], [CVB.res])
                    PS.release(b_cv)
                    pend2 = poc
                pend = cur
            act(lambda e: e.activation(out=st_mean.ap, in_=PS.f[:, b_s1, :], func=AF.Copy, scale=1.0 / D),
                [PS.res[b_s1]], [st_mean.res])
            PS.release(b_s1)
            dve(lambda e: e.tensor_tensor(out=st_mr.ap, in0=st_mean.ap, in1=st_mean.ap, op=ALU.mult),
                [st_mean.res], [st_mr.res])
            dve(lambda e: e.scalar_tensor_tensor(out=st_var.ap, in0=PS.f[:, b_s2, :], scalar=1.0 / D, in1=st_mr.ap,
                                                 op0=ALU.mult, op1=ALU.subtract), [PS.res[b_s2], st_mr.res], [st_var.res])
            PS.release(b_s2)
            act(lambda e: e.activation(out=st_var.ap, in_=st_var.ap, func=AF.Sqrt, bias=EPS, scale=1.0),
                [st_var.res], [st_var.res])
            dve(lambda e: e.reciprocal(out=st_var.ap, in_=st_var.ap), [st_var.res], [st_var.res])
            dve(lambda e: e.tensor_tensor(out=st_mr.ap, in0=st_mean.ap, in1=st_var.ap, op=ALU.mult),
                [st_mean.res, st_var.res], [st_mr.res])
            VN = PL1
            for oc in range(KC):
                t1 = r_cv.next()
                dve(lambda e, oc=oc, t1=t1: e.tensor_tensor(out=t1.ap, in0=CVB.ap[:, oc, :], in1=st_var.ap, op=ALU.mult),
                    [CVB.res, st_var.res], [t1.res])
                pool(lambda e, t1=t1: e.tensor_tensor(out=t1.ap, in0=t1.ap, in1=st_mr.ap, op=ALU.subtract),
                     [t1.res, st_mr.res], [t1.res])
                act(lambda e, oc=oc, t1=t1: e.activation(out=VN.ap[:, oc, :], in_=t1.ap, func=AF.Silu,
                                                         bias=PRM.ap[:, O_LB + oc:O_LB + oc + 1],
                                                         scale=PRM.ap[:, O_LG + oc:O_LG + oc + 1]),
                    [t1.res, PRM.res], [VN.res])
            while bg:
                bg_step()
            if bg_fin is not None:
                bg_fin()
            for oc in range(KC):
                wt = ws.get(bl[("oc", oc // 2)])
                b_y = PS.alloc()
                for k in range(KC):
                    mm(PS.f[:, b_y, :], wt.ap[:, k, (oc % 2) * P:(oc % 2 + 1) * P], VN.ap[:, k, :],
                       k == 0, k == KC - 1, [wt.res, VN.res], [PS.res[b_y]])
                mt = r_bf.next()
                dve(lambda e, oc=oc, mt=mt, b_y=b_y: e.scalar_tensor_tensor(
                    out=mt.ap, in0=PS.f[:, b_y, :], scalar=PRM.ap[:, O_BOC + oc:O_BOC + oc + 1], in1=GT8_ap[:, oc, :],
                    op0=ALU.add, op1=ALU.mult), [PS.res[b_y], PRM.res, mt.res] + GT8_res, [mt.res])
                PS.release(b_y)
                pool(lambda e, oc=oc, mt=mt: e.tensor_tensor(out=MACC.ap[:, oc, :], in0=MACC.ap[:, oc, :], in1=mt.ap, op=ALU.add),
                     [MACC.res, mt.res], [MACC.res])

            wo = [ws.get(bl[("wo", q)]) for q in range(4)]
            src_d = x_d if l == 0 else X2_d
            rd = [] if l == 0 else [r_X2[c]]
            Rs = [r_R.next() for _ in range(TT)]
            for j in range(TT):
                tok0 = (c * TT + j) * P
                fw.dma("sp", lambda e, j=j, tok0=tok0: e.dma_start(out=Rs[j].ap, in_=src_d[tok0:tok0 + P, :]), Rs[j].res,
                       reads=rd, writes=[Rs[j].res])
            for j in range(TT):
                R = Rs[j]
                for hf in range(2):
                    b_h = PS.alloc()
                    for qq in range(2):
                        q = hf * 2 + qq
                        for k in range(KC):
                            mm(PS.f[:, b_h, qq * WB:(qq + 1) * WB], MACC.ap[:, k, j * P:(j + 1) * P], wo[q].ap[:, k, :],
                               k == 0, k == KC - 1, [MACC.res, wo[q].res], [PS.res[b_h]])
                    dve(lambda e, b=b_h, hf=hf, R=R: e.scalar_tensor_tensor(
                        out=R.ap[:, hf * 512:(hf + 1) * 512], in0=R.ap[:, hf * 512:(hf + 1) * 512], scalar=ALPHA,
                        in1=PS.f[:, b, :], op0=ALU.mult, op1=ALU.add), [PS.res[b_h], R.res], [R.res])
                    PS.release(b_h)
            stages = [ln_stages(Rs[j], 0, 1, "dve") for j in range(TT)]
            for fs in zip(*stages):
                for f in fs:
                    f()
            x1bs = []
            sms = []
            banks = []
            for j in range(TT):
                R = Rs[j]
                tok0 = (c * TT + j) * P
                fw.dma("sp", lambda e, R=R, tok0=tok0: e.dma_start(out=X1_d[tok0:tok0 + P, :], in_=R.ap), R.res,
                       reads=[R.res], cwrites=[r_X1[c]])
                x1b = r_x1b.next()
                x1bs.append(x1b)
                act(lambda e, R=R, x1b=x1b: e.copy(out=x1b.ap, in_=R.ap), [R.res], [x1b.res])
                for half in range(2):
                    b_t = PS.alloc()
                    for kk in range(4):
                        kc = half * 4 + kk
                        fw.op("pe", lambda e, kc=kc, kk=kk, R=R, b_t=b_t: e.transpose(
                            out=PS.f[:, b_t, kk * P:(kk + 1) * P], in_=R.ap[:, kc * P:(kc + 1) * P], identity=ident_f.ap),
                              [R.res, ident_f.res], [PS.res[b_t]], inc=(kk == 3))
                    act(lambda e, b=b_t, half=half: e.copy(out=X1T.ap[:, half * 4:(half + 1) * 4, :],
                                                           in_=PS.f[:, b, :].rearrange("p (k t) -> p k t", t=P)),
                        [PS.res[b_t]], [X1T.res])
                    PS.release(b_t)
                b_l = PS.alloc()
                banks.append(b_l)
                for kc in range(KC):
                    mm(PS.f[:, b_l, 0:E], X1T.ap[:, kc, :], WR.ap[:, kc, :], kc == 0, kc == KC - 1,
                       [X1T.res, WR.res], [PS.res[b_l]])
                sms.append(r_small.next())

            def route_ops(j):
                tg = c * TT + j
                sm = sms[j]
                b_l = banks[j]
                x1b = x1bs[j]
                LG = sm.ap[:, 0:32]
                MX8 = sm.ap[:, 32:40]
                RANKF = sm.ap[:, 64:96]
                VAL = sm.ap[:, 96:128]
                DA = sm.ap[:, 128:160]
                JUNK = sm.ap[:, 160:192]
                DEST4 = sm.ap[:, 192:196]
                VAL4 = sm.ap[:, 196:200]
                EX4 = sm.ap[:, 200:204]
                SUM = sm.ap[:, 204:205]
                NEGMX = sm.ap[:, 205:206]
                RSM = sm.ap[:, 206:207]
                MSKb = sm.ap[:, 256:272].bitcast(BF16)
                sr = [sm.res]
                ops = []
                ops.append(lambda: dve(lambda e: e.tensor_tensor(out=LG, in0=PS.f[:, b_l, 0:E], in1=BRT.ap, op=ALU.add),
                                       [PS.res[b_l], BRT.res], sr))
                ops.append(lambda: dve(lambda e: e.max(out=MX8, in_=LG), sr, sr))
                ops.append(lambda: dve(lambda e: e.tensor_scalar(out=MSKb, in0=LG, scalar1=MX8[:, 3:4], scalar2=None,
                                                                 op0=ALU.is_ge), sr, sr))

                def mms():
                    mm(PS.f[:, b_l, 64:64 + E], triu_b.ap, MSKb, True, True, [triu_b.res, sm.res], [PS.res[b_l]])
                    mm(PS.f[:, b_l, 128:128 + E], ones_b.ap, MSKb, True, True, [ones_b.res, sm.res], [PS.res[b_l]])
                ops.append(mms)
                ops.append(lambda: dve(lambda e: e.tensor_scalar_mul(out=NEGMX, in0=MX8[:, 0:1], scalar1=-1.0), sr, sr))
                ops.append(lambda: act(lambda e: e.activation(out=EX4, in_=MX8[:, 0:4], func=AF.Exp, bias=NEGMX, scale=1.0,
                                                              accum_out=SUM), sr, sr))
                ops.append(lambda: dve(lambda e: e.reciprocal(out=RSM, in_=SUM), sr, sr))

                def rank():
                    dve(lambda e: e.tensor_tensor(out=RANKF, in0=PS.f[:, b_l, 64:64 + E], in1=BASE.ap, op=ALU.add),
                        [PS.res[b_l], BASE.res] + sr, sr)
                    dve(lambda e: e.tensor_tensor(out=BASE.ap, in0=PS.f[:, b_l, 128:128 + E], in1=BASE.ap, op=ALU.add),
                        [PS.res[b_l], BASE.res], [BASE.res])
                    PS.release(b_l)
                ops.append(rank)
                ops.append(lambda: dve(lambda e: e.tensor_single_scalar(out=VAL, in_=RANKF, scalar=float(CAP), op=ALU.is_lt), sr, sr))
                ops.append(lambda: dve(lambda e: e.tensor_tensor(out=DA, in0=RANKF, in1=iota_cap.ap, op=ALU.add),
                                       sr + [iota_cap.res], sr))
                ops.append(lambda: dve(lambda e: e.scalar_tensor_tensor(out=DA, in0=DA, scalar=-float(TRASH), in1=VAL,
                                                                        op0=ALU.add, op1=ALU.mult), sr, sr))
                ops.append(lambda: dve(lambda e: e.tensor_scalar_add(out=DA, in0=DA, scalar1=float(TRASH)), sr, sr))
                for k in range(4):
                    ops.append(lambda k=k: dve(lambda e: e.scalar_tensor_tensor(
                        out=JUNK, in0=LG, scalar=MX8[:, k:k + 1], in1=DA, op0=ALU.is_equal, op1=ALU.mult,
                        accum_out=DEST4[:, k:k + 1]), sr, sr))
                ops.append(lambda: dve(lambda e: e.tensor_single_scalar(out=VAL4, in_=DEST4, scalar=float(TRASH), op=ALU.is_lt), sr, sr))
                ops.append(lambda: dve(lambda e: e.scalar_tensor_tensor(out=GATES.ap[:, tg, :], in0=EX4, scalar=RSM, in1=VAL4,
                                                                        op0=ALU.mult, op1=ALU.mult), sr + [GATES.res], [GATES.res]))
                ops.append(lambda: dve(lambda e: e.tensor_copy(out=DESTI.ap[:, tg, :], in_=DEST4), sr + [DESTI.res], [DESTI.res]))

                def scat():
                    for k in range(4):
                        fw.dma("pool", lambda e, k=k: e.indirect_dma_start(
                            out=XS_d, out_offset=bass.IndirectOffsetOnAxis(ap=DESTI.ap[:, tg, k:k + 1], axis=0),
                            in_=x1b.ap, in_offset=None), x1b.res, reads=[x1b.res, DESTI.res], cwrites=[r_XS])
                deferred.append(scat)
                return ops
            allops = [route_ops(j) for j in range(TT)]
            for fs in zip(*allops):
                for f in fs:
                    f()

        def phase_b(l):
            ws = WStream(ringB, NRB, 8)
            bl = {}
            for e_ in range(E):
                for fc in range(KC):
                    bl[("gu", e_, fc)] = ws.add(wcols(w_gu_d[l, e_], fc * WB))
                if e_ >= 1:
                    for q in range(4):
                        bl[("dn", e_ - 1, q)] = ws.add(wcols(w_dn_d[l, e_ - 1], q * WB))
            for q in range(4):
                bl[("dn", E - 1, q)] = ws.add(wcols(w_dn_d[l, E - 1], q * WB))
            XSv = XS_d[0:NSLOT, :].rearrange("(e i p) d -> e p i d", e=E, p=P)
            YSv = YS_d[0:NSLOT, :].rearrange("(e i p) d -> e i p d", e=E, p=P)
            xes = {}
            bds = {}
            xets = {}
            actts = {}

            def load_x(e_):
                xe = r_xe.next()
                fw.dma("sp", lambda e: e.dma_start(out=xe.ap, in_=XSv[e_]), xe.res, reads=[r_XS], writes=[xe.res])
                xes[e_] = xe

            def load_bd(e_):
                bd = r_bd.next()
                fw.dma("sp", lambda e: e.dma_start(out=bd.ap, in_=bdn_d[l, e_, :].partition_broadcast(P)), bd.res,
                       writes=[bd.res])
                bds[e_] = bd

            def TR(e_):
                xe = xes[e_]
                xet = r_xet.next()
                xets[e_] = xet
                for kc in range(KC):
                    b = PS.alloc()
                    for i in range(CT):
                        fw.op("pe", lambda e, i=i, kc=kc, b=b: e.transpose(out=PS.b[:, b, i * P:(i + 1) * P],
                                                                           in_=xe.ap[:, i, kc * P:(kc + 1) * P],
                                                                           identity=ident_b.ap),
                              [xe.res, ident_b.res], [PS.res[b]], inc=(i == CT - 1))
                    if kc % 2 == 0:
                        act(lambda e, b=b, kc=kc: e.copy(out=xet.ap[:, kc, :], in_=PS.b[:, b, 0:CAP]), [PS.res[b]], [xet.res])
                    else:
                        dve(lambda e, b=b, kc=kc: e.tensor_copy(out=xet.ap[:, kc, :], in_=PS.b[:, b, 0:CAP]), [PS.res[b]], [xet.res])
                    PS.release(b)

            def GU(e_):
                xet = xets[e_]
                actt = r_actt.next()
                actts[e_] = actt
                for fc in range(KC):
                    wt = ws.get(bl[("gu", e_, fc)])
                    wv = wt.ap.rearrange("p k (f two) -> p k two f", two=2)
                    bg = [PS.alloc() for _ in range(NH)]
                    bu = [PS.alloc() for _ in range(NH)]
                    for gu, banks in ((0, bg), (1, bu)):
                        for nh in range(NH):
                            for k in range(KC):
                                mm(PS.f[:, banks[nh], 0:CH], wv[:, k, gu, :], xet.ap[:, k, nh * CH:(nh + 1) * CH],
                                   k == 0, k == KC - 1, [wt.res, xet.res], [PS.res[banks[nh]]])
                    cg = O_BGU + e_ * 16 + fc
                    cu = O_BGU + e_ * 16 + 8 + fc
                    gt = r_g.next()
                    sg = r_sg.next()
                    ut = r_u.next()
                    a1 = r_a1.next()
                    for nh in range(NH):
                        sl = slice(nh * CH, (nh + 1) * CH)
                        dve(lambda e, nh=nh, sl=sl: e.tensor_scalar(out=gt.ap[:, sl], in0=PS.f[:, bg[nh], 0:CH],
                                                                    scalar1=PRM.ap[:, cg:cg + 1], scalar2=7.0,
                                                                    op0=ALU.add, op1=ALU.min),
                            [PS.res[bg[nh]], PRM.res, gt.res], [gt.res])
                        act(lambda e, nh=nh, sl=sl: e.activation(out=ut.ap[:, sl], in_=PS.f[:, bu[nh], 0:CH],
                                                                 func=AF.Identity, bias=PRM.ap[:, cu:cu + 1], scale=1.0),
                            [PS.res[bu[nh]], PRM.res, ut.res], [ut.res])
                    for b in bg + bu:
                        PS.release(b)
                    act(lambda e: e.activation(out=sg.ap, in_=gt.ap, func=AF.Sigmoid, scale=1.702), [gt.res], [sg.res])
                    pool(lambda e: e.tensor_scalar(out=ut.ap, in0=ut.ap, scalar1=7.0, scalar2=-7.0,
                                                   op0=ALU.min, op1=ALU.max), [ut.res], [ut.res])
                    pool(lambda e: e.tensor_tensor(out=a1.ap, in0=gt.ap, in1=sg.ap, op=ALU.mult),
                         [gt.res, sg.res], [a1.res])
                    dve(lambda e, fc=fc: e.scalar_tensor_tensor(out=actt.ap[:, fc, :], in0=ut.ap, scalar=1.0, in1=a1.ap,
                                                                op0=ALU.add, op1=ALU.mult),
                        [ut.res, a1.res], [actt.res])

            def DN(e_):
                actt = actts.pop(e_)
                bd = bds.pop(e_)
                wd = [ws.get(bl[("dn", e_, q)]) for q in range(4)]
                for i in range(CT):
                    yo = r_yo.next()
                    for hf in range(2):
                        b = PS.alloc()
                        for qq in range(2):
                            q = hf * 2 + qq
                            for k in range(KC):
                                mm(PS.f[:, b, qq * WB:(qq + 1) * WB], actt.ap[:, k, i * P:(i + 1) * P], wd[q].ap[:, k, :],
                                   k == 0, k == KC - 1, [actt.res, wd[q].res], [PS.res[b]])
                        dve(lambda e, b=b, hf=hf: e.tensor_tensor(out=yo.ap[:, hf * 512:(hf + 1) * 512], in0=PS.f[:, b, :],
                                                                  in1=bd.ap[:, hf * 512:(hf + 1) * 512], op=ALU.add),
                            [PS.res[b], bd.res, yo.res], [yo.res])
                        PS.release(b)
                    fw.dma("sp", lambda e, i=i: e.dma_start(out=YSv[e_, i], in_=yo.ap), yo.res,
                           reads=[yo.res], cwrites=[r_YS])

            load_x(0)
            load_x(1)
            TR(0)
            for e_ in range(E):
                load_bd(e_)
                GU(e_)
                if e_ + 2 < E:
                    load_x(e_ + 2)
                if e_ + 1 < E:
                    TR(e_ + 1)
                if e_ >= 1:
                    DN(e_ - 1)
            DN(E - 1)

        def prep_steps(l, c):
            if l == 0:
                xbs = []

                def ld(j):
                    def f():
                        tok0 = (c * TT + j) * P
                        acc = r_acc.next()
                        fw.dma("sp", lambda e: e.dma_start(out=acc.ap, in_=x_d[tok0:tok0 + P, :]), acc.res,
                               writes=[acc.res])
                        xbs.append(make_xb(acc))
                    return f
                steps = [ld(j) for j in range(TT)]
            else:
                steps, xbs = phase_c_steps(c, False)

            def fin():
                for j in range(TT):
                    xT_from_xb(xbs[j], j)
            return steps, fin

        for l in range(NL):
            layer_setup(l)
            ws = WStream(ringA, NRA, 3)
            bls = [reg_blocks(l, ws) for c in range(NCH)]
            deferred = []
            steps, fin = prep_steps(l, 0)
            for f in steps:
                f()
            fin()
            for c in range(NCH):
                if c + 1 < NCH:
                    bg, bg_fin = prep_steps(l, c + 1)
                else:
                    bg, bg_fin = [], None
                phase_a(l, c, ws, bls[c], bg, bg_fin, deferred)
            for f in deferred:
                f()
            fw.barrier(allres)
            phase_b(l)
            fw.barrier(allres)
        for i in (2, 3):
            fw.dma("sp", lambda e, i=i: e.dma_start(out=LNP.ap[:, i, :], in_=lnp_d[NL - 1, i, :].partition_broadcast(P)),
                   LNP.res, writes=[LNP.res])
        for c in range(NCH):
            steps, _ = phase_c_steps(c, True)
            for f in steps:
                f()
        fw.barrier(allres)
        build_program.stats = dict(nins=fw.nins, nwaits=fw.nwaits, ndsem=fw.ndsem, a_end=A_END, b_end=B_END)
    return nc


def host_prm(inp, l0, l1):
    NL = l1 - l0
    prm = np.zeros((NL, P, NPRM), np.float32)
    for i, l in enumerate(range(l0, l1)):
        def pc(v):
            return np.asarray(v, np.float32).reshape(-1, P).T
        prm[i, :, O_BIN:O_BIN + 72] = pc(inp["b_in"][l])
        ca = np.asarray(inp["conv_a"][l], np.float32).reshape(3, KC, P)
        prm[i, :, O_CA:O_CA + 24] = ca.transpose(2, 1, 0).reshape(P, 24)
        prm[i, :, O_SP:O_SP + 8] = pc(inp["scale_pool"][l])
        cc = np.asarray(inp["conv_c"][l], np.float32).reshape(31, KC, P)
        prm[i, :, O_CC:O_CC + 248] = cc.transpose(2, 1, 0).reshape(P, 248)
        prm[i, :, O_CCB:O_CCB + 8] = pc(inp["conv_c_b"][l])
        prm[i, :, O_LG:O_LG + 8] = pc(inp["ln_c_g"][l])
        prm[i, :, O_LB:O_LB + 8] = pc(inp["ln_c_b"][l])
        prm[i, :, O_BOC:O_BOC + 8] = pc(inp["b_out_c"][l])
        bg = np.asarray(inp["b_gu"][l], np.float32).reshape(E, KC, P, 2)
        prm[i, :, O_BGU:O_BGU + 512] = bg.transpose(2, 0, 3, 1).reshape(P, 512)
    return prm


def make_in_maps(inp, xs, l0, l1):
    sl = slice(l0, l1)
    f = lambda a: np.ascontiguousarray(np.asarray(a, np.float32))
    shared = {
        "w_in": f(inp["w_in"][sl]), "w_out_a": f(inp["w_out_a"][sl]), "w_pool": f(inp["w_pool"][sl]),
        "w_out_c": f(inp["w_out_c"][sl]), "w_o": f(inp["w_o"][sl]), "w_router": f(inp["w_router"][sl]),
        "w_gu": f(inp["w_gu"][sl]), "w_down": f(inp["w_down"][sl]),
        "prm": host_prm(inp, l0, l1),
        "lnp": f(np.stack([inp["ln1_g"][sl], inp["ln1_b"][sl], inp["ln2_g"][sl], inp["ln2_b"][sl]], axis=1)),
        "b_router": f(inp["b_router"][sl]), "b_down": f(inp["b_down"][sl]),
    }
    return [dict(shared, x=f(x)) for x in xs]


CAP_FULL = 768
_prog_cache = {}


def run_layers(inp, xs, l0, l1, S, CAP):
    key = (S, CAP, l1 - l0)
    if key not in _prog_cache:
        _prog_cache[key] = build_program(S, CAP, l1 - l0)
    nc = _prog_cache[key]
    in_maps = make_in_maps(inp, xs, l0, l1)
    res = run_bass_kernel_spmd(nc, in_maps, core_ids=list(range(len(xs))))
    return [np.asarray(r["out"]) for r in res.results]


def kernel(**inputs):
    x = np.asarray(inputs["x"], np.float32)
    Bn, S, _ = x.shape
    xs = [x[b] for b in range(Bn)]
    outs = run_layers(inputs, xs, 0, DEPTH, S, CAP_FULL)
    return np.stack(outs, axis=0).astype(np.float32)
```

```python
import numpy as np
import concourse.bass as bass
import concourse.mybir as mybir
from concourse.bass_utils import run_bass_kernel_spmd
from contextlib import ExitStack

F32 = mybir.dt.float32
BF16 = mybir.dt.bfloat16
I32 = mybir.dt.int32
AF = mybir.ActivationFunctionType
ALU = mybir.AluOpType

P = 128
D = 1024
KC = 8
E = 32
T = 512
TT = 4
DEPTH = 4
ALPHA = (2.0 * DEPTH) ** 0.25
EPS = 1e-5
WB = 256
NPRM = 896
O_BIN, O_CA, O_SP, O_CC, O_CCB, O_LG, O_LB, O_BOC, O_BGU = 0, 72, 96, 104, 352, 360, 368, 376, 384


class Res:
    __slots__ = ("name", "lw", "rd", "dsem", "dcnt")

    def __init__(self, name):
        self.name = name
        self.lw = {}
        self.rd = {}
        self.dsem = None
        self.dcnt = 0


class FW:
    def __init__(self, nc, es):
        self.nc = nc
        self.es = es
        self.sems = {}
        self.eng = {}
        for name, h in (("pe", nc.tensor), ("act", nc.scalar), ("dve", nc.vector),
                        ("pool", nc.gpsimd), ("sp", nc.sync)):
            key = "S_" + name
            self.sems[key] = es.enter_context(nc.semaphore(key))
            self.eng[name] = dict(h=h, key=key, tick=0, known={})
        self.ndsem = 0
        self.nins = 0
        self.nwaits = 0

    def _emit_waits(self, ename, reads, writes, cwrites):
        E_ = self.eng[ename]
        own = E_["key"]
        waits = {}

        def need(k, c):
            if waits.get(k, 0) < c:
                waits[k] = c
        raw_own = 0
        for r in reads:
            for k, c in r.lw.items():
                if k == own:
                    raw_own = max(raw_own, c)
                else:
                    need(k, c)
        for w in writes:
            for k, c in w.lw.items():
                if k != own:
                    need(k, c)
            for k, c in w.rd.items():
                if k != own:
                    need(k, c)
        for w in cwrites:
            for k, c in w.rd.items():
                if k != own:
                    need(k, c)
        if raw_own and ename != "pe":
            need(own, raw_own)
        for k, c in waits.items():
            if E_["known"].get(k, 0) < c:
                E_["h"].wait_ge(self.sems[k], c)
                E_["known"][k] = c
                self.nwaits += 1

    def _mark(self, tok, reads, writes, cwrites):
        k, c = tok
        for r in reads:
            if r.rd.get(k, 0) < c:
                r.rd[k] = c
        for w in writes:
            w.lw = {k: c}
            w.rd = {}
        for w in cwrites:
            if w.lw.get(k, 0) < c:
                w.lw[k] = c

    def op(self, ename, fn, reads=(), writes=(), inc=True):
        E_ = self.eng[ename]
        self._emit_waits(ename, reads, writes, ())
        ins = fn(E_["h"])
        self.nins += 1
        if inc:
            E_["tick"] += 1
            ins.then_inc(self.sems[E_["key"]], 1)
            tok = (E_["key"], E_["tick"])
        else:
            tok = (E_["key"], E_["tick"] + 1)
        self._mark(tok, reads, writes, ())
        return ins

    def dma(self, ename, fn, sbres, reads=(), writes=(), cwrites=()):
        E_ = self.eng[ename]
        if sbres.dsem is None:
            key = "D%d" % self.ndsem
            self.ndsem += 1
            self.sems[key] = self.es.enter_context(self.nc.semaphore(key))
            sbres.dsem = key
        self._emit_waits(ename, reads, writes, cwrites)
        ins = fn(E_["h"])
        self.nins += 1
        sbres.dcnt += 16
        ins.then_inc(self.sems[sbres.dsem], 16)
        self._mark((sbres.dsem, sbres.dcnt), reads, writes, cwrites)
        return ins

    def barrier(self, all_res):
        waits = {}
        for nm, E_ in self.eng.items():
            if E_["tick"]:
                waits[E_["key"]] = E_["tick"]
        for r in all_res:
            for k, c in list(r.lw.items()) + list(r.rd.items()):
                if waits.get(k, 0) < c:
                    waits[k] = c
        for nm, E_ in self.eng.items():
            for k, c in waits.items():
                if k == E_["key"]:
                    continue
                if E_["known"].get(k, 0) < c:
                    E_["h"].wait_ge(self.sems[k], c)
                    E_["known"][k] = c
                    self.nwaits += 1


class Tile:
    __slots__ = ("ap", "res")

    def __init__(self, ap, name):
        self.ap = ap
        self.res = Res(name)


class Carver:
    def __init__(self, arena, base, limit, allres):
        self.arena = arena
        self.off = base
        self.limit = limit
        self.allres = allres

    def get(self, name, shape, dtype):
        esz = 4 if dtype in (F32, I32) else 2
        n = 1
        for s in shape[1:]:
            n *= s
        nb = (n * esz + 31) // 32 * 32
        o = self.off
        self.off += nb
        assert self.off <= self.limit, ("SBUF arena overflow", name, self.off, self.limit)
        ap = self.arena[:, o // 2:(o + n * esz) // 2]
        if dtype != BF16:
            ap = ap.bitcast(dtype)
        if len(shape) == 3:
            ap = ap.rearrange("p (a b) -> p a b", b=shape[2])
        elif len(shape) == 4:
            ap = ap.rearrange("p (a b c) -> p a b c", b=shape[2], c=shape[3])
        t = Tile(ap, name)
        self.allres.append(t.res)
        return t

    def ring(self, name, n, shape, dtype):
        return Ring([self.get("%s%d" % (name, i), shape, dtype) for i in range(n)])


class Ring:
    def __init__(self, tiles):
        self.tiles = tiles
        self.i = 0

    def next(self):
        t = self.tiles[self.i % len(self.tiles)]
        self.i += 1
        return t


class PsumAlloc:
    def __init__(self, psum_f32):
        self.f = psum_f32
        self.b = psum_f32.bitcast(BF16)
        self.res = [Res("psum%d" % i) for i in range(8)]
        self.free = list(range(8))

    def alloc(self):
        assert self.free, "PSUM banks exhausted"
        return self.free.pop(0)

    def release(self, i):
        self.free.append(i)


def build_program(S, CAP, NL, first_is_input=True):
    NCH = S // T
    NT = S // P
    NSLOT = E * CAP
    TRASH = NSLOT
    CT = CAP // P
    NH = 2 if CAP > 512 else 1
    CH = CAP // NH
    nc = bass.Bass("TRN2", target_bir_lowering=False)

    def din(name, shape, dt=F32):
        return nc.dram_tensor(name, list(shape), dt, kind="ExternalInput").ap()
    x_d = din("x", [S, D])
    w_in_d = din("w_in", [NL, D, 9 * D])
    w_oa_d = din("w_out_a", [NL, D, D])
    w_pool_d = din("w_pool", [NL, 4, 256, 256])
    w_oc_d = din("w_out_c", [NL, D, D])
    w_o_d = din("w_o", [NL, D, D])
    w_r_d = din("w_router", [NL, D, E])
    w_gu_d = din("w_gu", [NL, E, D, 2 * D])
    w_dn_d = din("w_down", [NL, E, D, D])
    prm_d = din("prm", [NL, P, NPRM])
    lnp_d = din("lnp", [NL, 4, D])
    brt_d = din("b_router", [NL, E])
    bdn_d = din("b_down", [NL, E, D])
    out_d = nc.dram_tensor("out", [S, D], F32, kind="ExternalOutput").ap()
    X1_d = nc.dram_tensor("X1s", [S, D], F32, kind="Internal").ap()
    X2_d = nc.dram_tensor("X2s", [S, D], F32, kind="Internal").ap()
    XS_d = nc.dram_tensor("XSs", [NSLOT + P, D], BF16, kind="Internal").ap()
    YS_d = nc.dram_tensor("YSs", [NSLOT + P, D], F32, kind="Internal").ap()
    r_X1 = [Res("X1c%d" % c) for c in range(NCH)]
    r_X2 = [Res("X2c%d" % c) for c in range(NCH)]
    r_XS = Res("XS")
    r_YS = Res("YS")

    with ExitStack() as es:
        fw = FW(nc, es)
        ARENA_BYTES = 206 * 1024
        arena = es.enter_context(nc.sbuf_tensor("arena", [P, ARENA_BYTES // 2], BF16))
        psum = es.enter_context(nc.psum_tensor("psum", [P, 8, 512], F32))
        PS = PsumAlloc(psum)
        allres = list(PS.res) + r_X1 + r_X2 + [r_XS, r_YS]
        G = Carver(arena, 0, ARENA_BYTES, allres)

        def act(fn, reads, writes):
            return fw.op("act", fn, reads, writes)

        def dve(fn, reads, writes):
            return fw.op("dve", fn, reads, writes)

        def pool(fn, reads, writes):
            return fw.op("pool", fn, reads, writes)

        def mm(out, lhsT, rhs, start, stop, reads, writes):
            return fw.op("pe", lambda e: e.matmul(out, lhsT=lhsT, rhs=rhs, start=start, stop=stop),
                         reads, writes, inc=stop)

        ident_f = G.get("ident_f", [P, P], F32)
        ident_b = G.get("ident_b", [P, P], BF16)
        triu_b = G.get("triu_b", [P, P], BF16)
        ones_b = G.get("ones_b", [P, P], BF16)
        iota_cap = G.get("iota_cap", [P, E], F32)
        rcnt = G.get("rcnt", [P, 4, 16], F32)
        io_t = G.get("io_t", [P, P], F32)
        PRM = G.get("PRM", [P, NPRM], F32)
        LNP = G.get("LNP", [P, 4, D], F32)
        WR = G.get("WR", [P, KC, E], F32)
        BRT = G.get("BRT", [P, E], F32)
        DG3 = G.get("DG3", [P, KC, 3, P], BF16)
        GATES = G.get("GATES", [P, NT, 4], F32)
        DESTI = G.get("DESTI", [P, NT, 4], I32)
        BASE = G.get("BASE", [P, E], F32)
        HA = G.get("HA", [P, KC, 2], BF16)
        HP = G.get("HP", [P, KC, 16], F32)
        HC = G.get("HC", [P, KC, 32], BF16)
        GBASE = G.off

        pool(lambda e: e.iota(io_t.ap, pattern=[[1, P]], base=0, channel_multiplier=-1,
                              allow_small_or_imprecise_dtypes=True), [], [io_t.res])
        dve(lambda e: e.tensor_single_scalar(out=ident_f.ap, in_=io_t.ap, scalar=0.0, op=ALU.is_equal),
            [io_t.res], [ident_f.res])
        dve(lambda e: e.tensor_single_scalar(out=ident_b.ap, in_=io_t.ap, scalar=0.0, op=ALU.is_equal),
            [io_t.res], [ident_b.res])
        dve(lambda e: e.tensor_single_scalar(out=triu_b.ap, in_=io_t.ap, scalar=0.0, op=ALU.is_gt),
            [io_t.res], [triu_b.res])
        dve(lambda e: e.memset(ones_b.ap, 1.0), [], [ones_b.res])
        pool(lambda e: e.iota(iota_cap.ap, pattern=[[CAP, E]], base=0, channel_multiplier=0,
                              allow_small_or_imprecise_dtypes=True), [], [iota_cap.res])
        for g in range(4):
            w = 2 << g
            pool(lambda e, g=g: e.iota(rcnt.ap[:, g, :], pattern=[[1, 16]], base=1, channel_multiplier=0,
                                       allow_small_or_imprecise_dtypes=True), [], [rcnt.res])
            dve(lambda e, g=g, w=w: e.tensor_scalar_min(out=rcnt.ap[:, g, :], in0=rcnt.ap[:, g, :], scalar1=float(w)),
                [rcnt.res], [rcnt.res])
        dve(lambda e: e.reciprocal(out=rcnt.ap, in_=rcnt.ap), [rcnt.res], [rcnt.res])
        A = Carver(arena, GBASE, ARENA_BYTES, allres)
        XT = A.get("XT", [P, KC, T], BF16)
        PL1 = A.get("PL1", [P, KC, T], BF16)
        CVB = A.get("CVB", [P, KC, T], BF16)
        MACC = A.get("MACC", [P, KC, T], BF16)
        DGa = A.get("DGa", [P, 16, P], BF16)
        DGb = A.get("DGb", [P, 15, P], BF16)
        NRA = 9
        ringA = A.ring("wA", NRA, [P, KC, WB], BF16)
        r_bf = A.ring("sbf", 6, [P, T], BF16)
        r_uh = A.ring("uh", 2, [P, T + 2], BF16)
        r_vh = A.ring("vh", 2, [P, T + 30], BF16)
        r_cv = A.ring("cv", 2, [P, T], F32)
        GBASE_PT = A.off
        Pt = A.get("Pt", [P, 2, T + 15], F32)
        Sa = A.get("Sa", [P, 2, T + 15], F32)
        Sb = A.get("Sb", [P, 2, T + 15], F32)
        r_pooled = A.ring("pooled", 4, [P, 2, T], BF16)
        st_mean = A.get("st_mean", [P, T], F32)
        st_var = A.get("st_var", [P, T], F32)
        st_mr = A.get("st_mr", [P, T], F32)
        r_R = A.ring("R", 4, [P, D], F32)
        r_x1b = A.ring("x1b", 4, [P, D], BF16)
        X1T = A.get("X1T", [P, KC, P], F32)
        r_small = A.ring("rt", 4, [P, 288], F32)
        r_lnst = A.ring("lnst", 8, [P, 16], F32)
        r_yg = A.ring("yg", 2, [P, D], F32)
        r_acc = Ring(r_R.tiles)
        YAb = A.get("YA", [P, KC, T], BF16)
        r_xb = A.ring("xb", 4, [P, D], BF16)
        A_END = A.off
        gt8_off = None
        GT8_ap = arena[:, (GBASE_PT) // 2:(GBASE_PT + KC * T * 2) // 2].rearrange("p (a b) -> p a b", b=T)
        GT8_res = [Pt.res, Sa.res]

        B = Carver(arena, GBASE, ARENA_BYTES, allres)
        r_xe = B.ring("xe", 2, [P, CT, D], BF16)
        r_xet = B.ring("xet", 2, [P, KC, CAP], BF16)
        r_actt = B.ring("actt", 2, [P, KC, CAP], BF16)
        NRB = 14
        ringB = B.ring("wB", NRB, [P, KC, WB], BF16)
        r_g = B.ring("bg", 2, [P, CAP], F32)
        r_sg = B.ring("bsg", 2, [P, CAP], F32)
        r_u = B.ring("bu", 2, [P, CAP], F32)
        r_a1 = B.ring("ba1", 2, [P, CAP], F32)
        r_yo = B.ring("yo", 3, [P, D], F32)
        r_bd = B.ring("bd", 2, [P, D], F32)
        B_END = B.off

        zrow = r_R.tiles[0]
        dve(lambda e: e.memset(zrow.ap, 0.0), [], [zrow.res])
        XSz = XS_d.rearrange("(n p) d -> n p d", p=P)
        YSz = YS_d.rearrange("(n p) d -> n p d", p=P)
        zb = zrow.ap.bitcast(BF16)
        for n in range(0, (NSLOT + P) // P, 2):
            nn = min(2, (NSLOT + P) // P - n)
            fw.dma("sp", lambda e, n=n, nn=nn: e.dma_start(
                out=XSz[n:n + nn].rearrange("n p d -> p n d"),
                in_=zb[:, 0:nn * D].rearrange("p (n d) -> p n d", d=D)), zrow.res,
                reads=[zrow.res], cwrites=[r_XS])
        fw.dma("sp", lambda e: e.dma_start(out=YSz[NSLOT // P], in_=zrow.ap), zrow.res,
               reads=[zrow.res], cwrites=[r_YS])

        class WStream:
            def __init__(self, ring, nring, la):
                self.ring = ring
                self.n = nring
                self.la = la
                self.blocks = []
                self.emitted = 0
                self.tiles = {}

            def add(self, src, nk=KC):
                self.blocks.append((src, nk))
                return len(self.blocks) - 1

            def get(self, i):
                lim = min(len(self.blocks), i + 1 + self.la)
                while self.emitted < lim:
                    j = self.emitted
                    t = self.ring.next()
                    src, nk = self.blocks[j]
                    fw.dma("pool", lambda e, t=t, src=src, nk=nk: e.dma_start(out=t.ap[:, 0:nk, :], in_=src), t.res,
                           writes=[t.res])
                    self.tiles[j] = t
                    self.emitted += 1
                return self.tiles[i]

        def wcols(w2d, c0, ncol=WB):
            return w2d.rearrange("(k p) c -> p k c", p=P)[:, :, c0:c0 + ncol]

        def layer_setup(l):
            fw.dma("sp", lambda e: e.dma_start(out=PRM.ap, in_=prm_d[l]), PRM.res, writes=[PRM.res])
            for i in range(4):
                ll = l if i < 2 else l - 1
                if ll < 0:
                    continue
                fw.dma("sp", lambda e, i=i, ll=ll: e.dma_start(out=LNP.ap[:, i, :], in_=lnp_d[ll, i, :].partition_broadcast(P)),
                       LNP.res, writes=[LNP.res])
            fw.dma("sp", lambda e: e.dma_start(out=WR.ap, in_=w_r_d[l].rearrange("(k p) e -> p k e", p=P)),
                   WR.res, writes=[WR.res])
            fw.dma("sp", lambda e: e.dma_start(out=BRT.ap, in_=brt_d[l, :].partition_broadcast(P)),
                   BRT.res, writes=[BRT.res])
            for oc in range(KC):
                dve(lambda e, oc=oc: e.tensor_tensor(
                    out=DG3.ap[:, oc], in0=ident_f.ap.unsqueeze(1).to_broadcast([P, 3, P]),
                    in1=PRM.ap[:, O_CA + oc * 3:O_CA + oc * 3 + 3].unsqueeze(2).to_broadcast([P, 3, P]),
                    op=ALU.mult), [ident_f.res, PRM.res], [DG3.res])
            dve(lambda e: e.memset(BASE.ap, 0.0), [], [BASE.res])
            dve(lambda e: e.memset(HA.ap, 0.0), [], [HA.res])
            dve(lambda e: e.memset(HP.ap, 0.0), [], [HP.res])
            dve(lambda e: e.memset(HC.ap, 0.0), [], [HC.res])

        def make_xb(src_tile):
            xb = r_xb.next()
            act(lambda e: e.copy(out=xb.ap, in_=src_tile.ap), [src_tile.res], [xb.res])
            return xb

        def xT_from_xb(xb, j):
            b = PS.alloc()
            for kc in range(KC):
                fw.op("pe", lambda e, kc=kc: e.transpose(out=PS.b[:, b, kc * P:(kc + 1) * P],
                                                         in_=xb.ap[:, kc * P:(kc + 1) * P], identity=ident_b.ap),
                      [xb.res, ident_b.res], [PS.res[b]], inc=(kc == KC - 1))
            act(lambda e: e.copy(out=XT.ap[:, :, j * P:(j + 1) * P],
                                 in_=PS.b[:, b, :].rearrange("p (k t) -> p k t", t=P)),
                [PS.res[b]], [XT.res])
            PS.release(b)

        def ln_stages(R, gi, bi, eng_gb):
            sm = r_lnst.next()
            st = sm.ap[:, 0:12].rearrange("p (a b) -> p a b", b=6)
            ag = sm.ap[:, 12:14]
            sd = sm.ap[:, 14:15]
            rs = sm.ap[:, 15:16]

            def s_stats():
                for h in range(2):
                    dve(lambda e, h=h: e.bn_stats(out=st[:, h, :], in_=R.ap[:, h * 512:(h + 1) * 512]),
                        [R.res], [sm.res])
                dve(lambda e: e.bn_aggr(out=ag, in_=st), [sm.res], [sm.res])
            return [
                s_stats,
                lambda: act(lambda e: e.activation(out=sd, in_=ag[:, 1:2], func=AF.Sqrt, bias=EPS, scale=1.0),
                            [sm.res], [sm.res]),
                lambda: dve(lambda e: e.reciprocal(out=rs, in_=sd), [sm.res], [sm.res]),
                lambda: dve(lambda e: e.tensor_scalar(out=R.ap, in0=R.ap, scalar1=ag[:, 0:1], scalar2=rs,
                                                      op0=ALU.subtract, op1=ALU.mult), [R.res, sm.res], [R.res]),
                lambda: fw.op(eng_gb, lambda e: e.tensor_tensor(out=R.ap, in0=R.ap, in1=LNP.ap[:, gi, :], op=ALU.mult),
                              [R.res, LNP.res], [R.res]),
                lambda: fw.op(eng_gb, lambda e: e.tensor_tensor(out=R.ap, in0=R.ap, in1=LNP.ap[:, bi, :], op=ALU.add),
                              [R.res, LNP.res], [R.res]),
            ]

        def ln_tokmajor(R, gi, bi, eng_gb):
            for f in ln_stages(R, gi, bi, eng_gb):
                f()

        def phase_c_steps(c, is_last):
            steps = []
            xbs = [None] * TT
            accs = [r_acc.next() for _ in range(TT)]
            lns = []

            def mk(j):
                tg = c * TT + j
                tok0 = tg * P
                acc = accs[j]
                ygs = {}

                def gather(k):
                    yg = r_yg.next()
                    ygs[k] = yg
                    fw.dma("pool", lambda e: e.indirect_dma_start(
                        out=yg.ap, out_offset=None, in_=YS_d,
                        in_offset=bass.IndirectOffsetOnAxis(ap=DESTI.ap[:, tg, k:k + 1], axis=0)),
                        yg.res, reads=[r_YS, DESTI.res], writes=[yg.res])

                def combine(k):
                    yg = ygs[k]
                    dve(lambda e: e.scalar_tensor_tensor(
                        out=acc.ap, in0=yg.ap, scalar=GATES.ap[:, tg, k:k + 1], in1=acc.ap,
                        op0=ALU.mult, op1=ALU.add), [yg.res, GATES.res, acc.res], [acc.res])

                def s0():
                    fw.dma("sp", lambda e: e.dma_start(out=acc.ap, in_=X1_d[tok0:tok0 + P, :]), acc.res,
                           reads=[r_X1[c]], writes=[acc.res])
                    gather(0)
                    gather(1)
                    act(lambda e: e.activation(out=acc.ap, in_=acc.ap, func=AF.Copy, scale=ALPHA), [acc.res], [acc.res])

                def s1():
                    combine(0)
                    gather(2)
                    combine(1)
                    gather(3)

                def s2():
                    combine(2)
                    combine(3)

                def s_out():
                    if is_last:
                        fw.dma("sp", lambda e: e.dma_start(out=out_d[tok0:tok0 + P, :], in_=acc.ap), acc.res,
                               reads=[acc.res], cwrites=[r_out])
                    else:
                        fw.dma("sp", lambda e: e.dma_start(out=X2_d[tok0:tok0 + P, :], in_=acc.ap), acc.res,
                               reads=[acc.res], cwrites=[r_X2[c]])
                        xbs[j] = make_xb(acc)
                return [s0, s1, s2], s_out
            outs = []
            for j in range(TT):
                pre, so = mk(j)
                steps.extend(pre)
                outs.append(so)
                lns.append(ln_stages(accs[j], 2, 3, "dve"))
                if j % 2 == 1:
                    for fs in zip(lns[j - 1], lns[j]):
                        steps.extend(fs)
                    steps.append(outs[j - 1])
                    steps.append(outs[j])
            return steps, xbs

        r_out = Res("out")
        allres.append(r_out)

        def stage1_steps(ws, bl):
            st = {"pend": None}

            def inproj1(s_, oc):
                wt = ws.get(bl[("in", s_, oc // 2)])
                b = PS.alloc()
                for k in range(KC):
                    mm(PS.f[:, b, :], wt.ap[:, k, (oc % 2) * P:(oc % 2 + 1) * P], XT.ap[:, k, :],
                       k == 0, k == KC - 1, [wt.res, XT.res], [PS.res[b]])
                return b

            def bias(s_, oc):
                col = O_BIN + s_ * 8 + oc
                return PRM.ap[:, col:col + 1]

            def it(oc):
                def f():
                    if oc < KC:
                        b_gc = inproj1(1, oc)
                        gct = r_bf.next()
                        act(lambda e: e.activation(out=gct.ap, in_=PS.f[:, b_gc, :], func=AF.Identity,
                                                   bias=bias(1, oc), scale=1.0),
                            [PS.res[b_gc], PRM.res], [gct.res])
                        PS.release(b_gc)
                        b_h = inproj1(2, oc)
                        uh = r_uh.next()
                        pool(lambda e: e.tensor_copy(out=uh.ap[:, 0:2], in_=HA.ap[:, oc, :]), [HA.res], [uh.res])
                        dve(lambda e: e.scalar_tensor_tensor(
                            out=uh.ap[:, 2:T + 2], in0=PS.f[:, b_h, :], scalar=bias(2, oc), in1=gct.ap,
                            op0=ALU.add, op1=ALU.mult), [PS.res[b_h], PRM.res, gct.res, uh.res], [uh.res])
                        PS.release(b_h)
                        pool(lambda e: e.tensor_copy(out=HA.ap[:, oc, :], in_=uh.ap[:, T:T + 2]), [uh.res], [HA.res])
                        b_gb = inproj1(0, oc)
                        cur = (oc, uh, b_gb)
                    else:
                        cur = None
                    if st["pend"] is not None:
                        poc, puh, pb_gb = st["pend"]
                        b_cv = PS.alloc()
                        for k in range(3):
                            mm(PS.f[:, b_cv, :], DG3.ap[:, poc, k, :], puh.ap[:, k:k + T], k == 0, k == 2,
                               [DG3.res, puh.res], [PS.res[b_cv]])
                        cvt = r_cv.next()
                        act(lambda e: e.copy(out=cvt.ap, in_=PS.f[:, b_cv, :]), [PS.res[b_cv]], [cvt.res])
                        PS.release(b_cv)
                        dve(lambda e: e.scalar_tensor_tensor(
                            out=YAb.ap[:, poc, :], in0=PS.f[:, pb_gb, :], scalar=bias(0, poc), in1=cvt.ap,
                            op0=ALU.add, op1=ALU.mult), [PS.res[pb_gb], PRM.res, cvt.res], [YAb.res])
                        PS.release(pb_gb)
                    st["pend"] = cur
                return f
            return [it(oc) for oc in range(KC + 1)]

        def reg_blocks(l, ws, c):
            w_in = w_in_d[l]
            bl = {}
            nb = {}

            def s1(d, qs):
                for q in qs:
                    for s_ in (1, 2, 0):
                        d[("in", s_, q)] = ws.add(wcols(w_in, s_ * D + q * WB))
            if c == 0:
                s1(bl, range(4))
            for q in range(4):
                bl[("oa", q)] = ws.add(wcols(w_oa_d[l], q * WB))
                bl[("in", 6, q)] = ws.add(wcols(w_in, 6 * D + q * WB))
            for g in range(4):
                bl[("in", 3, g)] = ws.add(wcols(w_in, 3 * D + g * WB))
            for g in range(4):
                bl[("in", 7, g)] = ws.add(wcols(w_in, 7 * D + g * WB))
                bl[("wp", g)] = ws.add(w_pool_d[l, g].rearrange("(k p) e -> p k e", p=P), 2)
            for q in range(4):
                bl[("in", 8, q)] = ws.add(wcols(w_in, 8 * D + q * WB))
            for q in range(4):
                bl[("in", 5, q)] = ws.add(wcols(w_in, 5 * D + q * WB))
                bl[("in", 4, q)] = ws.add(wcols(w_in, 4 * D + q * WB))
            if c + 1 < NCH:
                s1(nb, (0, 1))
            for q in range(4):
                bl[("oc", q)] = ws.add(wcols(w_oc_d[l], q * WB))
            for q in range(4):
                bl[("wo", q)] = ws.add(wcols(w_o_d[l], q * WB))
            if c + 1 < NCH:
                s1(nb, (2, 3))
            bl["next"] = nb
            return bl

        def phase_a(l, c, ws, bl, bg, bg_fin, deferred, a1_cur, a1_next):
            t0 = c * T
            def bg_step():
                if bg:
                    bg.pop(0)()

            def flush_deferred():
                n = len(deferred)
                for f in deferred[:n - (TT if False else 0)]:
                    f()
                del deferred[:]

            def inproj(s, oc):
                wt = ws.get(bl[("in", s, oc // 2)])
                b = PS.alloc()
                for k in range(KC):
                    mm(PS.f[:, b, :], wt.ap[:, k, (oc % 2) * P:(oc % 2 + 1) * P], XT.ap[:, k, :],
                       k == 0, k == KC - 1, [wt.res, XT.res], [PS.res[b]])
                return b

            def bias(s, oc):
                col = O_BIN + s * 8 + oc
                return PRM.ap[:, col:col + 1]

            YA = YAb
            for f in a1_cur:
                f()
            del a1_cur[:]

            def out_and_gate(wkey, yin, gs, mode, post_scalar_col, first):
                for oc in range(KC):
                    bg_step()
                    wt = ws.get(bl[(wkey, oc // 2)])
                    b_y = PS.alloc()
                    for k in range(KC):
                        mm(PS.f[:, b_y, :], wt.ap[:, k, (oc % 2) * P:(oc % 2 + 1) * P], yin.ap[:, k, :],
                           k == 0, k == KC - 1, [wt.res, yin.res], [PS.res[b_y]])
                    b_g = inproj(gs, oc)
                    gt = r_bf.next()
                    act(lambda e, b=b_g, gt=gt, oc=oc: e.activation(out=gt.ap, in_=PS.f[:, b, :], func=AF.Sigmoid,
                                                                    bias=bias(gs, oc), scale=1.0),
                        [PS.res[b_g], PRM.res], [gt.res])
                    PS.release(b_g)
                    merge(b_y, gt, oc, mode, post_scalar_col, first)
                    PS.release(b_y)

            def merge(b_y, gt, oc, mode, col, first):
                sc = PRM.ap[:, col + oc:col + oc + 1] if col is not None else None
                if first:
                    dve(lambda e: e.tensor_tensor(out=MACC.ap[:, oc, :], in0=PS.f[:, b_y, :], in1=gt.ap, op=ALU.mult),
                        [PS.res[b_y], gt.res], [MACC.res])
                else:
                    mt = r_bf.next()
                    dve(lambda e: e.scalar_tensor_tensor(out=mt.ap, in0=PS.f[:, b_y, :], scalar=sc, in1=gt.ap,
                                                         op0=(ALU.mult if mode == "scale" else ALU.add), op1=ALU.mult),
                        [PS.res[b_y], PRM.res, gt.res], [mt.res])
                    pool(lambda e: e.tensor_tensor(out=MACC.ap[:, oc, :], in0=MACC.ap[:, oc, :], in1=mt.ap, op=ALU.add),
                         [MACC.res, mt.res], [MACC.res])

            out_and_gate("oa", YA, 6, None, None, True)

            flush_deferred()
            pooled = []
            for g in range(4):
                bg_step()
                w = 2 << g
                for h in range(2):
                    oc = 2 * g + h
                    b_p = inproj(3, oc)
                    pool(lambda e, h=h, oc=oc: e.tensor_copy(out=Pt.ap[:, h, 0:15], in_=HP.ap[:, oc, 0:15]),
                         [HP.res], [Pt.res])
                    act(lambda e, b=b_p, h=h, oc=oc: e.activation(out=Pt.ap[:, h, 15:15 + T], in_=PS.f[:, b, :],
                                                                  func=AF.Identity, bias=bias(3, oc), scale=1.0),
                        [PS.res[b_p], PRM.res, Pt.res], [Pt.res])
                    PS.release(b_p)
                    pool(lambda e, h=h, oc=oc: e.tensor_copy(out=HP.ap[:, oc, 0:15], in_=Pt.ap[:, h, T:T + 15]),
                         [Pt.res], [HP.res])
                src = Pt
                lo = 0
                bufs = [Sa, Sb]
                for i in range(g + 1):
                    sh = 1 << i
                    dst = bufs[i % 2]
                    lo2 = lo + sh
                    dve(lambda e, src=src, dst=dst, lo2=lo2, sh=sh: e.tensor_tensor(
                        out=dst.ap[:, :, lo2:T + 15], in0=src.ap[:, :, lo2:T + 15], in1=src.ap[:, :, lo2 - sh:T + 15 - sh],
                        op=ALU.add), [src.res], [dst.res])
                    src = dst
                    lo = lo2
                pl = r_pooled.next()
                dve(lambda e, src=src, pl=pl, w=w: e.scalar_tensor_tensor(
                    out=pl.ap, in0=src.ap[:, :, 15:15 + T], scalar=1.0 / w, in1=Pt.ap[:, :, 15:15 + T],
                    op0=ALU.mult, op1=ALU.subtract), [src.res, Pt.res], [pl.res])
                if c == 0:
                    nfx = w - 1
                    tmpf = r_cv.next()
                    for h in range(2):
                        dve(lambda e, src=src, h=h, g=g, nfx=nfx: e.tensor_tensor(
                            out=tmpf.ap[:, h * 16:h * 16 + nfx], in0=src.ap[:, h, 15:15 + nfx], in1=rcnt.ap[:, g, 0:nfx],
                            op=ALU.mult), [src.res, rcnt.res, tmpf.res], [tmpf.res])
                        dve(lambda e, pl=pl, h=h, nfx=nfx: e.tensor_tensor(
                            out=pl.ap[:, h, 0:nfx], in0=tmpf.ap[:, h * 16:h * 16 + nfx], in1=Pt.ap[:, h, 15:15 + nfx],
                            op=ALU.subtract), [tmpf.res, Pt.res, pl.res], [pl.res])
                pooled.append(pl)
            for oc in range(KC):
                bg_step()
                g, h = oc // 2, oc % 2
                b_g = inproj(7, oc)
                gt = r_bf.next()
                act(lambda e, b=b_g, gt=gt, oc=oc: e.activation(out=gt.ap, in_=PS.f[:, b, :], func=AF.Sigmoid,
                                                                bias=bias(7, oc), scale=1.0),
                    [PS.res[b_g], PRM.res], [gt.res])
                PS.release(b_g)
                wt = ws.get(bl[("wp", g)])
                pl = pooled[g]
                b_y = PS.alloc()
                for k in range(2):
                    mm(PS.f[:, b_y, :], wt.ap[:, k, h * P:(h + 1) * P], pl.ap[:, k, :], k == 0, k == 1,
                       [wt.res, pl.res], [PS.res[b_y]])
                merge(b_y, gt, oc, "scale", O_SP, False)
                PS.release(b_y)

            for oc in range(KC):
                bg_step()
                b_g = inproj(8, oc)
                act(lambda e, b=b_g, oc=oc: e.activation(out=GT8_ap[:, oc, :], in_=PS.f[:, b, :], func=AF.Sigmoid,
                                                         bias=bias(8, oc), scale=1.0),
                    [PS.res[b_g], PRM.res] + GT8_res, GT8_res)
                PS.release(b_g)
            b_s1 = PS.alloc()
            b_s2 = PS.alloc()
            pend = None
            pend2 = None
            for oc in range(KC + 2):
                bg_step()
                if oc < KC:
                    b_gbb = inproj(5, oc)
                    sgt = r_bf.next()
                    act(lambda e, b=b_gbb, sgt=sgt, oc=oc: e.activation(out=sgt.ap, in_=PS.f[:, b, :], func=AF.Sigmoid,
                                                                        bias=bias(5, oc), scale=1.0),
                        [PS.res[b_gbb], PRM.res], [sgt.res])
                    PS.release(b_gbb)
                    b_ga = inproj(4, oc)
                    vh = r_vh.next()
                    pool(lambda e, vh=vh, oc=oc: e.tensor_copy(out=vh.ap[:, 0:30], in_=HC.ap[:, oc, 0:30]),
                         [HC.res], [vh.res])
                    dve(lambda e, b=b_ga, vh=vh, sgt=sgt, oc=oc: e.scalar_tensor_tensor(
                        out=vh.ap[:, 30:T + 30], in0=PS.f[:, b, :], scalar=bias(4, oc), in1=sgt.ap,
                        op0=ALU.add, op1=ALU.mult), [PS.res[b_ga], PRM.res, sgt.res, vh.res], [vh.res])
                    PS.release(b_ga)
                    pool(lambda e, vh=vh, oc=oc: e.tensor_copy(out=HC.ap[:, oc, 0:30], in_=vh.ap[:, T:T + 30]),
                         [vh.res], [HC.res])
                    cur = (oc, vh)
                else:
                    cur = None
                if pend2 is not None:
                    poc = pend2
                    sq = r_bf.next()
                    dve(lambda e, poc=poc, sq=sq: e.tensor_tensor(out=sq.ap, in0=CVB.ap[:, poc, :], in1=CVB.ap[:, poc, :],
                                                                  op=ALU.mult), [CVB.res], [sq.res])
                    fw.op("pe", lambda e, poc=poc: e.matmul(PS.f[:, b_s1, :], lhsT=ones_b.ap, rhs=CVB.ap[:, poc, :],
                                                            start=(poc == 0), stop=(poc == KC - 1)),
                          [ones_b.res, CVB.res], [PS.res[b_s1]], inc=True)
                    fw.op("pe", lambda e, poc=poc, sq=sq: e.matmul(PS.f[:, b_s2, :], lhsT=ones_b.ap, rhs=sq.ap,
                                                                   start=(poc == 0), stop=(poc == KC - 1)),
                          [ones_b.res, sq.res], [PS.res[b_s2]], inc=True)
                    pend2 = None
                if pend is not None:
                    poc, pvh = pend
                    cbase = O_CC + poc * 31
                    dve(lambda e: e.tensor_tensor(
                        out=DGa.ap, in0=ident_f.ap.unsqueeze(1).to_broadcast([P, 16, P]),
                        in1=PRM.ap[:, cbase:cbase + 16].unsqueeze(2).to_broadcast([P, 16, P]),
                        op=ALU.mult), [ident_f.res, PRM.res], [DGa.res])
                    dve(lambda e: e.tensor_tensor(
                        out=DGb.ap, in0=ident_f.ap.unsqueeze(1).to_broadcast([P, 15, P]),
                        in1=PRM.ap[:, cbase + 16:cbase + 31].unsqueeze(2).to_broadcast([P, 15, P]),
                        op=ALU.mult), [ident_f.res, PRM.res], [DGb.res])
                    b_cv = PS.alloc()
                    for k in range(31):
                        dg = DGa if k < 16 else DGb
                        mm(PS.f[:, b_cv, :], dg.ap[:, k % 16, :], pvh.ap[:, k:k + T], k == 0, k == 30,
                           [dg.res, pvh.res], [PS.res[b_cv]])
                    act(lambda e, b=b_cv, poc=poc: e.activation(out=CVB.ap[:, poc, :], in_=PS.f[:, b, :], func=AF.Identity,
                                                                bias=PRM.ap[:, O_CCB + poc:O_CCB + poc + 1], scale=1.0),
                        [PS.res[b_cv], PRM.res], [CVB.res])
                    PS.release(b_cv)
                    pend2 = poc
                pend = cur
            act(lambda e: e.activation(out=st_mean.ap, in_=PS.f[:, b_s1, :], func=AF.Copy, scale=1.0 / D),
                [PS.res[b_s1]], [st_mean.res])
            PS.release(b_s1)
            dve(lambda e: e.tensor_tensor(out=st_mr.ap, in0=st_mean.ap, in1=st_mean.ap, op=ALU.mult),
                [st_mean.res], [st_mr.res])
            dve(lambda e: e.scalar_tensor_tensor(out=st_var.ap, in0=PS.f[:, b_s2, :], scalar=1.0 / D, in1=st_mr.ap,
                                                 op0=ALU.mult, op1=ALU.subtract), [PS.res[b_s2], st_mr.res], [st_var.res])
            PS.release(b_s2)
            act(lambda e: e.activation(out=st_var.ap, in_=st_var.ap, func=AF.Sqrt, bias=EPS, scale=1.0),
                [st_var.res], [st_var.res])
            dve(lambda e: e.reciprocal(out=st_var.ap, in_=st_var.ap), [st_var.res], [st_var.res])
            dve(lambda e: e.tensor_tensor(out=st_mr.ap, in0=st_mean.ap, in1=st_var.ap, op=ALU.mult),
                [st_mean.res, st_var.res], [st_mr.res])
            VN = PL1
            for oc in range(KC):
                t1 = r_cv.next()
                dve(lambda e, oc=oc, t1=t1: e.tensor_tensor(out=t1.ap, in0=CVB.ap[:, oc, :], in1=st_var.ap, op=ALU.mult),
                    [CVB.res, st_var.res], [t1.res])
                pool(lambda e, t1=t1: e.tensor_tensor(out=t1.ap, in0=t1.ap, in1=st_mr.ap, op=ALU.subtract),
                     [t1.res, st_mr.res], [t1.res])
                act(lambda e, oc=oc, t1=t1: e.activation(out=VN.ap[:, oc, :], in_=t1.ap, func=AF.Silu,
                                                         bias=PRM.ap[:, O_LB + oc:O_LB + oc + 1],
                                                         scale=PRM.ap[:, O_LG + oc:O_LG + oc + 1]),
                    [t1.res, PRM.res], [VN.res])
            while bg:
                bg_step()
            if bg_fin is not None:
                bg_fin()
                for f in a1_next[:4]:
                    f()
                del a1_next[:4]
            for oc in range(KC):
                wt = ws.get(bl[("oc", oc // 2)])
                b_y = PS.alloc()
                for k in range(KC):
                    mm(PS.f[:, b_y, :], wt.ap[:, k, (oc % 2) * P:(oc % 2 + 1) * P], VN.ap[:, k, :],
                       k == 0, k == KC - 1, [wt.res, VN.res], [PS.res[b_y]])
                mt = r_bf.next()
                dve(lambda e, oc=oc, mt=mt, b_y=b_y: e.scalar_tensor_tensor(
                    out=mt.ap, in0=PS.f[:, b_y, :], scalar=PRM.ap[:, O_BOC + oc:O_BOC + oc + 1], in1=GT8_ap[:, oc, :],
                    op0=ALU.add, op1=ALU.mult), [PS.res[b_y], PRM.res, mt.res] + GT8_res, [mt.res])
                PS.release(b_y)
                pool(lambda e, oc=oc, mt=mt: e.tensor_tensor(out=MACC.ap[:, oc, :], in0=MACC.ap[:, oc, :], in1=mt.ap, op=ALU.add),
                     [MACC.res, mt.res], [MACC.res])

            wo = [ws.get(bl[("wo", q)]) for q in range(4)]
            src_d = x_d if l == 0 else X2_d
            rd = [] if l == 0 else [r_X2[c]]
            Rs = [r_R.next() for _ in range(TT)]
            for j in range(TT):
                tok0 = (c * TT + j) * P
                fw.dma("sp", lambda e, j=j, tok0=tok0: e.dma_start(out=Rs[j].ap, in_=src_d[tok0:tok0 + P, :]), Rs[j].res,
                       reads=rd, writes=[Rs[j].res])
            for j in range(TT):
                R = Rs[j]
                for hf in range(2):
                    b_h = PS.alloc()
                    for qq in range(2):
                        q = hf * 2 + qq
                        for k in range(KC):
                            mm(PS.f[:, b_h, qq * WB:(qq + 1) * WB], MACC.ap[:, k, j * P:(j + 1) * P], wo[q].ap[:, k, :],
                               k == 0, k == KC - 1, [MACC.res, wo[q].res], [PS.res[b_h]])
                    dve(lambda e, b=b_h, hf=hf, R=R: e.scalar_tensor_tensor(
                        out=R.ap[:, hf * 512:(hf + 1) * 512], in0=R.ap[:, hf * 512:(hf + 1) * 512], scalar=ALPHA,
                        in1=PS.f[:, b, :], op0=ALU.mult, op1=ALU.add), [PS.res[b_h], R.res], [R.res])
                    PS.release(b_h)
            stages = [ln_stages(Rs[j], 0, 1, "dve") for j in range(TT)]
            for fs in zip(*stages):
                for f in fs:
                    f()
                if a1_next:
                    a1_next.pop(0)()
            while a1_next:
                a1_next.pop(0)()
            x1bs = []
            sms = []
            banks = []
            for j in range(TT):
                R = Rs[j]
                tok0 = (c * TT + j) * P
                fw.dma("sp", lambda e, R=R, tok0=tok0: e.dma_start(out=X1_d[tok0:tok0 + P, :], in_=R.ap), R.res,
                       reads=[R.res], cwrites=[r_X1[c]])
                x1b = r_x1b.next()
                x1bs.append(x1b)
                act(lambda e, R=R, x1b=x1b: e.copy(out=x1b.ap, in_=R.ap), [R.res], [x1b.res])
                for half in range(2):
                    b_t = PS.alloc()
                    for kk in range(4):
                        kc = half * 4 + kk
                        fw.op("pe", lambda e, kc=kc, kk=kk, R=R, b_t=b_t: e.transpose(
                            out=PS.f[:, b_t, kk * P:(kk + 1) * P], in_=R.ap[:, kc * P:(kc + 1) * P], identity=ident_f.ap),
                              [R.res, ident_f.res], [PS.res[b_t]], inc=(kk == 3))
                    act(lambda e, b=b_t, half=half: e.copy(out=X1T.ap[:, half * 4:(half + 1) * 4, :],
                                                           in_=PS.f[:, b, :].rearrange("p (k t) -> p k t", t=P)),
                        [PS.res[b_t]], [X1T.res])
                    PS.release(b_t)
                b_l = PS.alloc()
                banks.append(b_l)
                for kc in range(KC):
                    mm(PS.f[:, b_l, 0:E], X1T.ap[:, kc, :], WR.ap[:, kc, :], kc == 0, kc == KC - 1,
                       [X1T.res, WR.res], [PS.res[b_l]])
                sms.append(r_small.next())

            def route_ops(j):
                tg = c * TT + j
                sm = sms[j]
                b_l = banks[j]
                x1b = x1bs[j]
                LG = sm.ap[:, 0:32]
                MX8 = sm.ap[:, 32:40]
                RANKF = sm.ap[:, 64:96]
                VAL = sm.ap[:, 96:128]
                DA = sm.ap[:, 128:160]
                JUNK = sm.ap[:, 160:192]
                DEST4 = sm.ap[:, 192:196]
                VAL4 = sm.ap[:, 196:200]
                EX4 = sm.ap[:, 200:204]
                SUM = sm.ap[:, 204:205]
                NEGMX = sm.ap[:, 205:206]
                RSM = sm.ap[:, 206:207]
                MSKb = sm.ap[:, 256:272].bitcast(BF16)
                sr = [sm.res]
                ops = []
                ops.append(lambda: dve(lambda e: e.tensor_tensor(out=LG, in0=PS.f[:, b_l, 0:E], in1=BRT.ap, op=ALU.add),
                                       [PS.res[b_l], BRT.res], sr))
                ops.append(lambda: dve(lambda e: e.max(out=MX8, in_=LG), sr, sr))
                ops.append(lambda: dve(lambda e: e.tensor_scalar(out=MSKb, in0=LG, scalar1=MX8[:, 3:4], scalar2=None,
                                                                 op0=ALU.is_ge), sr, sr))

                def mms():
                    mm(PS.f[:, b_l, 64:64 + E], triu_b.ap, MSKb, True, True, [triu_b.res, sm.res], [PS.res[b_l]])
                    mm(PS.f[:, b_l, 128:128 + E], ones_b.ap, MSKb, True, True, [ones_b.res, sm.res], [PS.res[b_l]])
                ops.append(mms)
                ops.append(lambda: dve(lambda e: e.tensor_scalar_mul(out=NEGMX, in0=MX8[:, 0:1], scalar1=-1.0), sr, sr))
                ops.append(lambda: act(lambda e: e.activation(out=EX4, in_=MX8[:, 0:4], func=AF.Exp, bias=NEGMX, scale=1.0,
                                                              accum_out=SUM), sr, sr))
                ops.append(lambda: dve(lambda e: e.reciprocal(out=RSM, in_=SUM), sr, sr))

                def rank():
                    dve(lambda e: e.tensor_tensor(out=RANKF, in0=PS.f[:, b_l, 64:64 + E], in1=BASE.ap, op=ALU.add),
                        [PS.res[b_l], BASE.res] + sr, sr)
                    dve(lambda e: e.tensor_tensor(out=BASE.ap, in0=PS.f[:, b_l, 128:128 + E], in1=BASE.ap, op=ALU.add),
                        [PS.res[b_l], BASE.res], [BASE.res])
                    PS.release(b_l)
                ops.append(rank)
                ops.append(lambda: dve(lambda e: e.tensor_single_scalar(out=VAL, in_=RANKF, scalar=float(CAP), op=ALU.is_lt), sr, sr))
                ops.append(lambda: dve(lambda e: e.tensor_tensor(out=DA, in0=RANKF, in1=iota_cap.ap, op=ALU.add),
                                       sr + [iota_cap.res], sr))
                ops.append(lambda: dve(lambda e: e.scalar_tensor_tensor(out=DA, in0=DA, scalar=-float(TRASH), in1=VAL,
                                                                        op0=ALU.add, op1=ALU.mult), sr, sr))
                ops.append(lambda: dve(lambda e: e.tensor_scalar_add(out=DA, in0=DA, scalar1=float(TRASH)), sr, sr))
                for k in range(4):
                    ops.append(lambda k=k: dve(lambda e: e.scalar_tensor_tensor(
                        out=JUNK, in0=LG, scalar=MX8[:, k:k + 1], in1=DA, op0=ALU.is_equal, op1=ALU.mult,
                        accum_out=DEST4[:, k:k + 1]), sr, sr))
                ops.append(lambda: dve(lambda e: e.tensor_single_scalar(out=VAL4, in_=DEST4, scalar=float(TRASH), op=ALU.is_lt), sr, sr))
                ops.append(lambda: dve(lambda e: e.scalar_tensor_tensor(out=GATES.ap[:, tg, :], in0=EX4, scalar=RSM, in1=VAL4,
                                                                        op0=ALU.mult, op1=ALU.mult), sr + [GATES.res], [GATES.res]))
                ops.append(lambda: dve(lambda e: e.tensor_copy(out=DESTI.ap[:, tg, :], in_=DEST4), sr + [DESTI.res], [DESTI.res]))

                def scat():
                    for k in range(4):
                        fw.dma("pool", lambda e, k=k: e.indirect_dma_start(
                            out=XS_d, out_offset=bass.IndirectOffsetOnAxis(ap=DESTI.ap[:, tg, k:k + 1], axis=0),
                            in_=x1b.ap, in_offset=None), x1b.res, reads=[x1b.res, DESTI.res], cwrites=[r_XS])
                deferred.append(scat)
                return ops
            allops = [route_ops(j) for j in range(TT)]
            for fs in zip(*allops):
                for f in fs:
                    f()

        def phase_b(l):
            ws = WStream(ringB, NRB, 8)
            bl = {}
            for e_ in range(E):
                for fc in range(KC):
                    bl[("gu", e_, fc)] = ws.add(wcols(w_gu_d[l, e_], fc * WB))
                if e_ >= 1:
                    for q in range(4):
                        bl[("dn", e_ - 1, q)] = ws.add(wcols(w_dn_d[l, e_ - 1], q * WB))
            for q in range(4):
                bl[("dn", E - 1, q)] = ws.add(wcols(w_dn_d[l, E - 1], q * WB))
            XSv = XS_d[0:NSLOT, :].rearrange("(e i p) d -> e p i d", e=E, p=P)
            YSv = YS_d[0:NSLOT, :].rearrange("(e i p) d -> e i p d", e=E, p=P)
            xes = {}
            bds = {}
            xets = {}
            actts = {}

            def load_x(e_):
                xe = r_xe.next()
                fw.dma("sp", lambda e: e.dma_start(out=xe.ap, in_=XSv[e_]), xe.res, reads=[r_XS], writes=[xe.res])
                xes[e_] = xe

            def load_bd(e_):
                bd = r_bd.next()
                fw.dma("sp", lambda e: e.dma_start(out=bd.ap, in_=bdn_d[l, e_, :].partition_broadcast(P)), bd.res,
                       writes=[bd.res])
                bds[e_] = bd

            def TR(e_):
                xe = xes[e_]
                xet = r_xet.next()
                xets[e_] = xet
                for kc in range(KC):
                    b = PS.alloc()
                    for i in range(CT):
                        fw.op("pe", lambda e, i=i, kc=kc, b=b: e.transpose(out=PS.b[:, b, i * P:(i + 1) * P],
                                                                           in_=xe.ap[:, i, kc * P:(kc + 1) * P],
                                                                           identity=ident_b.ap),
                              [xe.res, ident_b.res], [PS.res[b]], inc=(i == CT - 1))
                    if kc % 2 == 0:
                        act(lambda e, b=b, kc=kc: e.copy(out=xet.ap[:, kc, :], in_=PS.b[:, b, 0:CAP]), [PS.res[b]], [xet.res])
                    else:
                        dve(lambda e, b=b, kc=kc: e.tensor_copy(out=xet.ap[:, kc, :], in_=PS.b[:, b, 0:CAP]), [PS.res[b]], [xet.res])
                    PS.release(b)

            def GU(e_):
                xet = xets[e_]
                actt = r_actt.next()
                actts[e_] = actt
                for fc in range(KC):
                    wt = ws.get(bl[("gu", e_, fc)])
                    wv = wt.ap.rearrange("p k (f two) -> p k two f", two=2)
                    bg = [PS.alloc() for _ in range(NH)]
                    bu = [PS.alloc() for _ in range(NH)]
                    for gu, banks in ((0, bg), (1, bu)):
                        for nh in range(NH):
                            for k in range(KC):
                                mm(PS.f[:, banks[nh], 0:CH], wv[:, k, gu, :], xet.ap[:, k, nh * CH:(nh + 1) * CH],
                                   k == 0, k == KC - 1, [wt.res, xet.res], [PS.res[banks[nh]]])
                    cg = O_BGU + e_ * 16 + fc
                    cu = O_BGU + e_ * 16 + 8 + fc
                    gt = r_g.next()
                    sg = r_sg.next()
                    ut = r_u.next()
                    a1 = r_a1.next()
                    for nh in range(NH):
                        sl = slice(nh * CH, (nh + 1) * CH)
                        dve(lambda e, nh=nh, sl=sl: e.tensor_scalar(out=gt.ap[:, sl], in0=PS.f[:, bg[nh], 0:CH],
                                                                    scalar1=PRM.ap[:, cg:cg + 1], scalar2=7.0,
                                                                    op0=ALU.add, op1=ALU.min),
                            [PS.res[bg[nh]], PRM.res, gt.res], [gt.res])
                        act(lambda e, nh=nh, sl=sl: e.activation(out=ut.ap[:, sl], in_=PS.f[:, bu[nh], 0:CH],
                                                                 func=AF.Identity, bias=PRM.ap[:, cu:cu + 1], scale=1.0),
                            [PS.res[bu[nh]], PRM.res, ut.res], [ut.res])
                    for b in bg + bu:
                        PS.release(b)
                    act(lambda e: e.activation(out=sg.ap, in_=gt.ap, func=AF.Sigmoid, scale=1.702), [gt.res], [sg.res])
                    pool(lambda e: e.tensor_scalar(out=ut.ap, in0=ut.ap, scalar1=7.0, scalar2=-7.0,
                                                   op0=ALU.min, op1=ALU.max), [ut.res], [ut.res])
                    pool(lambda e: e.tensor_tensor(out=a1.ap, in0=gt.ap, in1=sg.ap, op=ALU.mult),
                         [gt.res, sg.res], [a1.res])
                    dve(lambda e, fc=fc: e.scalar_tensor_tensor(out=actt.ap[:, fc, :], in0=ut.ap, scalar=1.0, in1=a1.ap,
                                                                op0=ALU.add, op1=ALU.mult),
                        [ut.res, a1.res], [actt.res])

            def DN(e_):
                actt = actts.pop(e_)
                bd = bds.pop(e_)
                wd = [ws.get(bl[("dn", e_, q)]) for q in range(4)]
                for i in range(CT):
                    yo = r_yo.next()
                    for hf in range(2):
                        b = PS.alloc()
                        for qq in range(2):
                            q = hf * 2 + qq
                            for k in range(KC):
                                mm(PS.f[:, b, qq * WB:(qq + 1) * WB], actt.ap[:, k, i * P:(i + 1) * P], wd[q].ap[:, k, :],
                                   k == 0, k == KC - 1, [actt.res, wd[q].res], [PS.res[b]])
                        dve(lambda e, b=b, hf=hf: e.tensor_tensor(out=yo.ap[:, hf * 512:(hf + 1) * 512], in0=PS.f[:, b, :],
                                                                  in1=bd.ap[:, hf * 512:(hf + 1) * 512], op=ALU.add),
                            [PS.res[b], bd.res, yo.res], [yo.res])
                        PS.release(b)
                    fw.dma("sp", lambda e, i=i: e.dma_start(out=YSv[e_, i], in_=yo.ap), yo.res,
                           reads=[yo.res], cwrites=[r_YS])

            load_x(0)
            load_x(1)
            TR(0)
            for e_ in range(E):
                load_bd(e_)
                GU(e_)
                if e_ + 2 < E:
                    load_x(e_ + 2)
                if e_ + 1 < E:
                    TR(e_ + 1)
                if e_ >= 1:
                    DN(e_ - 1)
            DN(E - 1)

        def prep_steps(l, c):
            if l == 0:
                xbs = []

                def ld(j):
                    def f():
                        tok0 = (c * TT + j) * P
                        acc = r_acc.next()
                        fw.dma("sp", lambda e: e.dma_start(out=acc.ap, in_=x_d[tok0:tok0 + P, :]), acc.res,
                               writes=[acc.res])
                        xbs.append(make_xb(acc))
                    return f
                steps = [ld(j) for j in range(TT)]
            else:
                steps, xbs = phase_c_steps(c, False)

            def fin():
                for j in range(TT):
                    xT_from_xb(xbs[j], j)
            return steps, fin

        for l in range(NL):
            layer_setup(l)
            ws = WStream(ringA, NRA, 4)
            bls = [reg_blocks(l, ws, c) for c in range(NCH)]
            a1 = stage1_steps(ws, bls[0])
            deferred = []
            steps, fin = prep_steps(l, 0)
            for f in steps:
                f()
            fin()
            for c in range(NCH):
                if c + 1 < NCH:
                    bg, bg_fin = prep_steps(l, c + 1)
                else:
                    bg, bg_fin = [], None
                a1n = stage1_steps(ws, bls[c]["next"]) if c + 1 < NCH else []
                phase_a(l, c, ws, bls[c], bg, bg_fin, deferred, a1, a1n)
                a1 = a1n
            for f in deferred:
                f()
            fw.barrier(allres)
            phase_b(l)
            fw.barrier(allres)
        for i in (2, 3):
            fw.dma("sp", lambda e, i=i: e.dma_start(out=LNP.ap[:, i, :], in_=lnp_d[NL - 1, i, :].partition_broadcast(P)),
                   LNP.res, writes=[LNP.res])
        for c in range(NCH):
            steps, _ = phase_c_steps(c, True)
            for f in steps:
                f()
        fw.barrier(allres)
        build_program.stats = dict(nins=fw.nins, nwaits=fw.nwaits, ndsem=fw.ndsem, a_end=A_END, b_end=B_END)
    return nc


def host_prm(inp, l0, l1):
    NL = l1 - l0
    prm = np.zeros((NL, P, NPRM), np.float32)
    for i, l in enumerate(range(l0, l1)):
        def pc(v):
            return np.asarray(v, np.float32).reshape(-1, P).T
        prm[i, :, O_BIN:O_BIN + 72] = pc(inp["b_in"][l])
        ca = np.asarray(inp["conv_a"][l], np.float32).reshape(3, KC, P)
        prm[i, :, O_CA:O_CA + 24] = ca.transpose(2, 1, 0).reshape(P, 24)
        prm[i, :, O_SP:O_SP + 8] = pc(inp["scale_pool"][l])
        cc = np.asarray(inp["conv_c"][l], np.float32).reshape(31, KC, P)
        prm[i, :, O_CC:O_CC + 248] = cc.transpose(2, 1, 0).reshape(P, 248)
        prm[i, :, O_CCB:O_CCB + 8] = pc(inp["conv_c_b"][l])
        prm[i, :, O_LG:O_LG + 8] = pc(inp["ln_c_g"][l])
        prm[i, :, O_LB:O_LB + 8] = pc(inp["ln_c_b"][l])
        prm[i, :, O_BOC:O_BOC + 8] = pc(inp["b_out_c"][l])
        bg = np.asarray(inp["b_gu"][l], np.float32).reshape(E, KC, P, 2)
        prm[i, :, O_BGU:O_BGU + 512] = bg.transpose(2, 0, 3, 1).reshape(P, 512)
    return prm


def make_in_maps(inp, xs, l0, l1):
    sl = slice(l0, l1)
    f = lambda a: np.ascontiguousarray(np.asarray(a, np.float32))
    shared = {
        "w_in": f(inp["w_in"][sl]), "w_out_a": f(inp["w_out_a"][sl]), "w_pool": f(inp["w_pool"][sl]),
        "w_out_c": f(inp["w_out_c"][sl]), "w_o": f(inp["w_o"][sl]), "w_router": f(inp["w_router"][sl]),
        "w_gu": f(inp["w_gu"][sl]), "w_down": f(inp["w_down"][sl]),
        "prm": host_prm(inp, l0, l1),
        "lnp": f(np.stack([inp["ln1_g"][sl], inp["ln1_b"][sl], inp["ln2_g"][sl], inp["ln2_b"][sl]], axis=1)),
        "b_router": f(inp["b_router"][sl]), "b_down": f(inp["b_down"][sl]),
    }
    return [dict(shared, x=f(x)) for x in xs]


CAP_FULL = 768
_prog_cache = {}


def run_layers(inp, xs, l0, l1, S, CAP):
    key = (S, CAP, l1 - l0)
    if key not in _prog_cache:
        _prog_cache[key] = build_program(S, CAP, l1 - l0)
    nc = _prog_cache[key]
    in_maps = make_in_maps(inp, xs, l0, l1)
    res = run_bass_kernel_spmd(nc, in_maps, core_ids=list(range(len(xs))))
    return [np.asarray(r["out"]) for r in res.results]


def kernel(**inputs):
    x = np.asarray(inputs["x"], np.float32)
    Bn, S, _ = x.shape
    xs = [x[b] for b in range(Bn)]
    outs = run_layers(inputs, xs, 0, DEPTH, S, CAP_FULL)
    return np.stack(outs, axis=0).astype(np.float32)
```

```python
import numpy as np
import concourse.bass as bass
import concourse.mybir as mybir
from concourse.bass_utils import run_bass_kernel_spmd
from contextlib import ExitStack

F32 = mybir.dt.float32
BF16 = mybir.dt.bfloat16
I32 = mybir.dt.int32
AF = mybir.ActivationFunctionType
ALU = mybir.AluOpType

P = 128
D = 1024
KC = 8
E = 32
T = 512
TT = 4
DEPTH = 4
ALPHA = (2.0 * DEPTH) ** 0.25
EPS = 1e-5
WB = 256
NPRM = 896
O_BIN, O_CA, O_SP, O_CC, O_CCB, O_LG, O_LB, O_BOC, O_BGU = 0, 72, 96, 104, 352, 360, 368, 376, 384


class Res:
    __slots__ = ("name", "lw", "rd", "dsem", "dcnt")

    def __init__(self, name):
        self.name = name
        self.lw = {}
        self.rd = {}
        self.dsem = None
        self.dcnt = 0


class FW:
    def __init__(self, nc, es):
        self.nc = nc
        self.es = es
        self.sems = {}
        self.eng = {}
        for name, h in (("pe", nc.tensor), ("act", nc.scalar), ("dve", nc.vector),
                        ("pool", nc.gpsimd), ("sp", nc.sync)):
            key = "S_" + name
            self.sems[key] = es.enter_context(nc.semaphore(key))
            self.eng[name] = dict(h=h, key=key, tick=0, known={})
        self.ndsem = 0
        self.nins = 0
        self.nwaits = 0

    def _emit_waits(self, ename, reads, writes, cwrites):
        E_ = self.eng[ename]
        own = E_["key"]
        waits = {}

        def need(k, c):
            if waits.get(k, 0) < c:
                waits[k] = c
        raw_own = 0
        for r in reads:
            for k, c in r.lw.items():
                if k == own:
                    raw_own = max(raw_own, c)
                else:
                    need(k, c)
        for w in writes:
            for k, c in w.lw.items():
                if k != own:
                    need(k, c)
            for k, c in w.rd.items():
                if k != own:
                    need(k, c)
        for w in cwrites:
            for k, c in w.rd.items():
                if k != own:
                    need(k, c)
        if raw_own and ename != "pe":
            need(own, raw_own)
        for k, c in waits.items():
            if E_["known"].get(k, 0) < c:
                E_["h"].wait_ge(self.sems[k], c)
                E_["known"][k] = c
                self.nwaits += 1

    def _mark(self, tok, reads, writes, cwrites):
        k, c = tok
        for r in reads:
            if r.rd.get(k, 0) < c:
                r.rd[k] = c
        for w in writes:
            w.lw = {k: c}
            w.rd = {}
        for w in cwrites:
            if w.lw.get(k, 0) < c:
                w.lw[k] = c

    def op(self, ename, fn, reads=(), writes=(), inc=True):
        E_ = self.eng[ename]
        self._emit_waits(ename, reads, writes, ())
        ins = fn(E_["h"])
        self.nins += 1
        if inc:
            E_["tick"] += 1
            ins.then_inc(self.sems[E_["key"]], 1)
            tok = (E_["key"], E_["tick"])
        else:
            tok = (E_["key"], E_["tick"] + 1)
        self._mark(tok, reads, writes, ())
        return ins

    def dma(self, ename, fn, sbres, reads=(), writes=(), cwrites=()):
        E_ = self.eng[ename]
        if sbres.dsem is None:
            key = "D%d" % self.ndsem
            self.ndsem += 1
            self.sems[key] = self.es.enter_context(self.nc.semaphore(key))
            sbres.dsem = key
        self._emit_waits(ename, reads, writes, cwrites)
        ins = fn(E_["h"])
        self.nins += 1
        sbres.dcnt += 16
        ins.then_inc(self.sems[sbres.dsem], 16)
        self._mark((sbres.dsem, sbres.dcnt), reads, writes, cwrites)
        return ins

    def barrier(self, all_res):
        waits = {}
        for nm, E_ in self.eng.items():
            if E_["tick"]:
                waits[E_["key"]] = E_["tick"]
        for r in all_res:
            for k, c in list(r.lw.items()) + list(r.rd.items()):
                if waits.get(k, 0) < c:
                    waits[k] = c
        for nm, E_ in self.eng.items():
            for k, c in waits.items():
                if k == E_["key"]:
                    continue
                if E_["known"].get(k, 0) < c:
                    E_["h"].wait_ge(self.sems[k], c)
                    E_["known"][k] = c
                    self.nwaits += 1


class Tile:
    __slots__ = ("ap", "res")

    def __init__(self, ap, name):
        self.ap = ap
        self.res = Res(name)


class Carver:
    def __init__(self, arena, base, limit, allres):
        self.arena = arena
        self.off = base
        self.limit = limit
        self.allres = allres

    def get(self, name, shape, dtype):
        esz = 4 if dtype in (F32, I32) else 2
        n = 1
        for s in shape[1:]:
            n *= s
        nb = (n * esz + 31) // 32 * 32
        o = self.off
        self.off += nb
        assert self.off <= self.limit, ("SBUF arena overflow", name, self.off, self.limit)
        ap = self.arena[:, o // 2:(o + n * esz) // 2]
        if dtype != BF16:
            ap = ap.bitcast(dtype)
        if len(shape) == 3:
            ap = ap.rearrange("p (a b) -> p a b", b=shape[2])
        elif len(shape) == 4:
            ap = ap.rearrange("p (a b c) -> p a b c", b=shape[2], c=shape[3])
        t = Tile(ap, name)
        self.allres.append(t.res)
        return t

    def ring(self, name, n, shape, dtype):
        return Ring([self.get("%s%d" % (name, i), shape, dtype) for i in range(n)])


class Ring:
    def __init__(self, tiles):
        self.tiles = tiles
        self.i = 0

    def next(self):
        t = self.tiles[self.i % len(self.tiles)]
        self.i += 1
        return t


class PsumAlloc:
    def __init__(self, psum_f32):
        self.f = psum_f32
        self.b = psum_f32.bitcast(BF16)
        self.res = [Res("psum%d" % i) for i in range(8)]
        self.free = list(range(8))

    def alloc(self):
        assert self.free, "PSUM banks exhausted"
        return self.free.pop(0)

    def release(self, i):
        self.free.append(i)


def build_program(S, CAP, NL, first_is_input=True):
    NCH = S // T
    NT = S // P
    NSLOT = E * CAP
    TRASH = NSLOT
    CT = CAP // P
    NH = 2 if CAP > 512 else 1
    CH = CAP // NH
    nc = bass.Bass("TRN2", target_bir_lowering=False)

    def din(name, shape, dt=F32):
        return nc.dram_tensor(name, list(shape), dt, kind="ExternalInput").ap()
    x_d = din("x", [S, D])
    w_in_d = din("w_in", [NL, D, 9 * D])
    w_oa_d = din("w_out_a", [NL, D, D])
    w_pool_d = din("w_pool", [NL, 4, 256, 256])
    w_oc_d = din("w_out_c", [NL, D, D])
    w_o_d = din("w_o", [NL, D, D])
    w_r_d = din("w_router", [NL, D, E])
    w_gu_d = din("w_gu", [NL, E, D, 2 * D])
    w_dn_d = din("w_down", [NL, E, D, D])
    prm_d = din("prm", [NL, P, NPRM])
    lnp_d = din("lnp", [NL, 4, D])
    brt_d = din("b_router", [NL, E])
    bdn_d = din("b_down", [NL, E, D])
    out_d = nc.dram_tensor("out", [S, D], F32, kind="ExternalOutput").ap()
    X1_d = nc.dram_tensor("X1s", [S, D], F32, kind="Internal").ap()
    X2_d = nc.dram_tensor("X2s", [S, D], F32, kind="Internal").ap()
    XS_d = nc.dram_tensor("XSs", [NSLOT + P, D], BF16, kind="Internal").ap()
    YS_d = nc.dram_tensor("YSs", [NSLOT + P, D], F32, kind="Internal").ap()
    r_X1 = [Res("X1c%d" % c) for c in range(NCH)]
    r_X2 = [Res("X2c%d" % c) for c in range(NCH)]
    r_XS = Res("XS")
    r_YS = Res("YS")

    with ExitStack() as es:
        fw = FW(nc, es)
        ARENA_BYTES = 206 * 1024
        arena = es.enter_context(nc.sbuf_tensor("arena", [P, ARENA_BYTES // 2], BF16))
        psum = es.enter_context(nc.psum_tensor("psum", [P, 8, 512], F32))
        PS = PsumAlloc(psum)
        allres = list(PS.res) + r_X1 + r_X2 + [r_XS, r_YS]
        G = Carver(arena, 0, ARENA_BYTES, allres)

        def act(fn, reads, writes):
            return fw.op("act", fn, reads, writes)

        def dve(fn, reads, writes):
            return fw.op("dve", fn, reads, writes)

        def pool(fn, reads, writes):
            return fw.op("pool", fn, reads, writes)

        def mm(out, lhsT, rhs, start, stop, reads, writes):
            return fw.op("pe", lambda e: e.matmul(out, lhsT=lhsT, rhs=rhs, start=start, stop=stop),
                         reads, writes, inc=stop)

        ident_f = G.get("ident_f", [P, P], F32)
        ident_b = G.get("ident_b", [P, P], BF16)
        triu_b = G.get("triu_b", [P, P], BF16)
        ones_b = G.get("ones_b", [P, P], BF16)
        iota_cap = G.get("iota_cap", [P, E], F32)
        rcnt = G.get("rcnt", [P, 4, 16], F32)
        io_t = G.get("io_t", [P, P], F32)
        PRM = G.get("PRM", [P, NPRM], F32)
        LNP = G.get("LNP", [P, 4, D], F32)
        WR = G.get("WR", [P, KC, E], F32)
        BRT = G.get("BRT", [P, E], F32)
        DG3 = G.get("DG3", [P, KC, 3, P], BF16)
        GATES = G.get("GATES", [P, NT, 4], F32)
        DESTI = G.get("DESTI", [P, NT, 4], I32)
        BASE = G.get("BASE", [P, E], F32)
        HA = G.get("HA", [P, KC, 2], BF16)
        HP = G.get("HP", [P, KC, 16], F32)
        HC = G.get("HC", [P, KC, 32], BF16)
        GBASE = G.off

        pool(lambda e: e.iota(io_t.ap, pattern=[[1, P]], base=0, channel_multiplier=-1,
                              allow_small_or_imprecise_dtypes=True), [], [io_t.res])
        dve(lambda e: e.tensor_single_scalar(out=ident_f.ap, in_=io_t.ap, scalar=0.0, op=ALU.is_equal),
            [io_t.res], [ident_f.res])
        dve(lambda e: e.tensor_single_scalar(out=ident_b.ap, in_=io_t.ap, scalar=0.0, op=ALU.is_equal),
            [io_t.res], [ident_b.res])
        dve(lambda e: e.tensor_single_scalar(out=triu_b.ap, in_=io_t.ap, scalar=0.0, op=ALU.is_gt),
            [io_t.res], [triu_b.res])
        dve(lambda e: e.memset(ones_b.ap, 1.0), [], [ones_b.res])
        pool(lambda e: e.iota(iota_cap.ap, pattern=[[CAP, E]], base=0, channel_multiplier=0,
                              allow_small_or_imprecise_dtypes=True), [], [iota_cap.res])
        for g in range(4):
            w = 2 << g
            pool(lambda e, g=g: e.iota(rcnt.ap[:, g, :], pattern=[[1, 16]], base=1, channel_multiplier=0,
                                       allow_small_or_imprecise_dtypes=True), [], [rcnt.res])
            dve(lambda e, g=g, w=w: e.tensor_scalar_min(out=rcnt.ap[:, g, :], in0=rcnt.ap[:, g, :], scalar1=float(w)),
                [rcnt.res], [rcnt.res])
        dve(lambda e: e.reciprocal(out=rcnt.ap, in_=rcnt.ap), [rcnt.res], [rcnt.res])
        A = Carver(arena, GBASE, ARENA_BYTES, allres)
        XT = A.get("XT", [P, KC, T], BF16)
        PL1 = A.get("PL1", [P, KC, T], BF16)
        CVB = A.get("CVB", [P, KC, T], BF16)
        MACC = A.get("MACC", [P, KC, T], BF16)
        DGa = A.get("DGa", [P, 16, P], BF16)
        DGb = A.get("DGb", [P, 15, P], BF16)
        NRA = 9
        ringA = A.ring("wA", NRA, [P, KC, WB], BF16)
        r_bf = A.ring("sbf", 6, [P, T], BF16)
        r_uh = A.ring("uh", 2, [P, T + 2], BF16)
        r_vh = A.ring("vh", 2, [P, T + 30], BF16)
        r_cv = A.ring("cv", 2, [P, T], F32)
        GBASE_PT = A.off
        Pt = A.get("Pt", [P, 2, T + 15], F32)
        Sa = A.get("Sa", [P, 2, T + 15], F32)
        Sb = A.get("Sb", [P, 2, T + 15], F32)
        r_pooled = A.ring("pooled", 4, [P, 2, T], BF16)
        st_mean = A.get("st_mean", [P, T], F32)
        st_var = A.get("st_var", [P, T], F32)
        st_mr = A.get("st_mr", [P, T], F32)
        r_R = A.ring("R", 4, [P, D], F32)
        r_x1b = A.ring("x1b", 4, [P, D], BF16)
        X1T = A.get("X1T", [P, KC, P], F32)
        r_small = A.ring("rt", 4, [P, 288], F32)
        r_lnst = A.ring("lnst", 8, [P, 16], F32)
        r_yg = A.ring("yg", 2, [P, D], F32)
        r_acc = Ring(r_R.tiles)
        YAb = A.get("YA", [P, KC, T], BF16)
        r_xb = A.ring("xb", 4, [P, D], BF16)
        A_END = A.off
        gt8_off = None
        GT8_ap = arena[:, (GBASE_PT) // 2:(GBASE_PT + KC * T * 2) // 2].rearrange("p (a b) -> p a b", b=T)
        GT8_res = [Pt.res, Sa.res]

        B = Carver(arena, GBASE, ARENA_BYTES, allres)
        r_xe = B.ring("xe", 2, [P, CT, D], BF16)
        r_xet = B.ring("xet", 2, [P, KC, CAP], BF16)
        r_actt = B.ring("actt", 2, [P, KC, CAP], BF16)
        NRB = 14
        ringB = B.ring("wB", NRB, [P, KC, WB], BF16)
        r_g = B.ring("bg", 2, [P, CAP], F32)
        r_sg = B.ring("bsg", 2, [P, CAP], F32)
        r_u = B.ring("bu", 2, [P, CAP], F32)
        r_a1 = B.ring("ba1", 2, [P, CAP], F32)
        r_yo = B.ring("yo", 3, [P, D], F32)
        r_bd = B.ring("bd", 2, [P, D], F32)
        B_END = B.off

        zrow = r_R.tiles[0]
        dve(lambda e: e.memset(zrow.ap, 0.0), [], [zrow.res])
        XSz = XS_d.rearrange("(n p) d -> n p d", p=P)
        YSz = YS_d.rearrange("(n p) d -> n p d", p=P)
        zb = zrow.ap.bitcast(BF16)
        for n in range(0, (NSLOT + P) // P, 2):
            nn = min(2, (NSLOT + P) // P - n)
            fw.dma("sp", lambda e, n=n, nn=nn: e.dma_start(
                out=XSz[n:n + nn].rearrange("n p d -> p n d"),
                in_=zb[:, 0:nn * D].rearrange("p (n d) -> p n d", d=D)), zrow.res,
                reads=[zrow.res], cwrites=[r_XS])
        fw.dma("sp", lambda e: e.dma_start(out=YSz[NSLOT // P], in_=zrow.ap), zrow.res,
               reads=[zrow.res], cwrites=[r_YS])

        class WStream:
            def __init__(self, ring, nring, la):
                self.ring = ring
                self.n = nring
                self.la = la
                self.blocks = []
                self.emitted = 0
                self.tiles = {}

            def add(self, src, nk=KC):
                self.blocks.append((src, nk))
                return len(self.blocks) - 1

            def get(self, i):
                lim = min(len(self.blocks), i + 1 + self.la)
                while self.emitted < lim:
                    j = self.emitted
                    t = self.ring.next()
                    src, nk = self.blocks[j]
                    fw.dma("pool", lambda e, t=t, src=src, nk=nk: e.dma_start(out=t.ap[:, 0:nk, :], in_=src), t.res,
                           writes=[t.res])
                    self.tiles[j] = t
                    self.emitted += 1
                return self.tiles[i]

        def wcols(w2d, c0, ncol=WB):
            return w2d.rearrange("(k p) c -> p k c", p=P)[:, :, c0:c0 + ncol]

        def layer_setup(l):
            fw.dma("sp", lambda e: e.dma_start(out=PRM.ap, in_=prm_d[l]), PRM.res, writes=[PRM.res])
            for i in range(4):
                ll = l if i < 2 else l - 1
                if ll < 0:
                    continue
                fw.dma("sp", lambda e, i=i, ll=ll: e.dma_start(out=LNP.ap[:, i, :], in_=lnp_d[ll, i, :].partition_broadcast(P)),
                       LNP.res, writes=[LNP.res])
            fw.dma("sp", lambda e: e.dma_start(out=WR.ap, in_=w_r_d[l].rearrange("(k p) e -> p k e", p=P)),
                   WR.res, writes=[WR.res])
            fw.dma("sp", lambda e: e.dma_start(out=BRT.ap, in_=brt_d[l, :].partition_broadcast(P)),
                   BRT.res, writes=[BRT.res])
            for oc in range(KC):
                dve(lambda e, oc=oc: e.tensor_tensor(
                    out=DG3.ap[:, oc], in0=ident_f.ap.unsqueeze(1).to_broadcast([P, 3, P]),
                    in1=PRM.ap[:, O_CA + oc * 3:O_CA + oc * 3 + 3].unsqueeze(2).to_broadcast([P, 3, P]),
                    op=ALU.mult), [ident_f.res, PRM.res], [DG3.res])
            dve(lambda e: e.memset(BASE.ap, 0.0), [], [BASE.res])
            dve(lambda e: e.memset(HA.ap, 0.0), [], [HA.res])
            dve(lambda e: e.memset(HP.ap, 0.0), [], [HP.res])
            dve(lambda e: e.memset(HC.ap, 0.0), [], [HC.res])

        def make_xb(src_tile):
            xb = r_xb.next()
            act(lambda e: e.copy(out=xb.ap, in_=src_tile.ap), [src_tile.res], [xb.res])
            return xb

        def xT_from_xb(xb, j):
            b = PS.alloc()
            for kc in range(KC):
                fw.op("pe", lambda e, kc=kc: e.transpose(out=PS.b[:, b, kc * P:(kc + 1) * P],
                                                         in_=xb.ap[:, kc * P:(kc + 1) * P], identity=ident_b.ap),
                      [xb.res, ident_b.res], [PS.res[b]], inc=(kc == KC - 1))
            act(lambda e: e.copy(out=XT.ap[:, :, j * P:(j + 1) * P],
                                 in_=PS.b[:, b, :].rearrange("p (k t) -> p k t", t=P)),
                [PS.res[b]], [XT.res])
            PS.release(b)

        def ln_stages(R, gi, bi, eng_gb):
            sm = r_lnst.next()
            st = sm.ap[:, 0:12].rearrange("p (a b) -> p a b", b=6)
            ag = sm.ap[:, 12:14]
            sd = sm.ap[:, 14:15]
            rs = sm.ap[:, 15:16]

            def s_stats():
                for h in range(2):
                    dve(lambda e, h=h: e.bn_stats(out=st[:, h, :], in_=R.ap[:, h * 512:(h + 1) * 512]),
                        [R.res], [sm.res])
                dve(lambda e: e.bn_aggr(out=ag, in_=st), [sm.res], [sm.res])
            return [
                s_stats,
                lambda: act(lambda e: e.activation(out=sd, in_=ag[:, 1:2], func=AF.Sqrt, bias=EPS, scale=1.0),
                            [sm.res], [sm.res]),
                lambda: dve(lambda e: e.reciprocal(out=rs, in_=sd), [sm.res], [sm.res]),
                lambda: dve(lambda e: e.tensor_scalar(out=R.ap, in0=R.ap, scalar1=ag[:, 0:1], scalar2=rs,
                                                      op0=ALU.subtract, op1=ALU.mult), [R.res, sm.res], [R.res]),
                lambda: fw.op(eng_gb, lambda e: e.tensor_tensor(out=R.ap, in0=R.ap, in1=LNP.ap[:, gi, :], op=ALU.mult),
                              [R.res, LNP.res], [R.res]),
                lambda: fw.op(eng_gb, lambda e: e.tensor_tensor(out=R.ap, in0=R.ap, in1=LNP.ap[:, bi, :], op=ALU.add),
                              [R.res, LNP.res], [R.res]),
            ]

        def ln_tokmajor(R, gi, bi, eng_gb):
            for f in ln_stages(R, gi, bi, eng_gb):
                f()

        def phase_c_steps(c, is_last):
            steps = []
            xbs = [None] * TT
            accs = [r_acc.next() for _ in range(TT)]
            lns = []

            def mk(j):
                tg = c * TT + j
                tok0 = tg * P
                acc = accs[j]
                ygs = {}

                def gather(k):
                    yg = r_yg.next()
                    ygs[k] = yg
                    fw.dma("pool", lambda e: e.indirect_dma_start(
                        out=yg.ap, out_offset=None, in_=YS_d,
                        in_offset=bass.IndirectOffsetOnAxis(ap=DESTI.ap[:, tg, k:k + 1], axis=0)),
                        yg.res, reads=[r_YS, DESTI.res], writes=[yg.res])

                def combine(k):
                    yg = ygs[k]
                    dve(lambda e: e.scalar_tensor_tensor(
                        out=acc.ap, in0=yg.ap, scalar=GATES.ap[:, tg, k:k + 1], in1=acc.ap,
                        op0=ALU.mult, op1=ALU.add), [yg.res, GATES.res, acc.res], [acc.res])

                def s0():
                    fw.dma("sp", lambda e: e.dma_start(out=acc.ap, in_=X1_d[tok0:tok0 + P, :]), acc.res,
                           reads=[r_X1[c]], writes=[acc.res])
                    gather(0)
                    gather(1)
                    act(lambda e: e.activation(out=acc.ap, in_=acc.ap, func=AF.Copy, scale=ALPHA), [acc.res], [acc.res])

                def s1():
                    combine(0)
                    gather(2)
                    combine(1)
                    gather(3)

                def s2():
                    combine(2)
                    combine(3)

                def s_out():
                    if is_last:
                        fw.dma("sp", lambda e: e.dma_start(out=out_d[tok0:tok0 + P, :], in_=acc.ap), acc.res,
                               reads=[acc.res], cwrites=[r_out])
                    else:
                        fw.dma("sp", lambda e: e.dma_start(out=X2_d[tok0:tok0 + P, :], in_=acc.ap), acc.res,
                               reads=[acc.res], cwrites=[r_X2[c]])
                        xbs[j] = make_xb(acc)
                return [s0, s1, s2], s_out
            outs = []
            for j in range(TT):
                pre, so = mk(j)
                steps.extend(pre)
                outs.append(so)
                lns.append(ln_stages(accs[j], 2, 3, "dve"))
                if j % 2 == 1:
                    for fs in zip(lns[j - 1], lns[j]):
                        steps.extend(fs)
                    steps.append(outs[j - 1])
                    steps.append(outs[j])
            return steps, xbs

        r_out = Res("out")
        allres.append(r_out)

        def stage1_steps(ws, bl):
            st = {"pend": None}

            def inproj1(s_, oc):
                wt = ws.get(bl[("in", s_, oc // 2)])
                b = PS.alloc()
                for k in range(KC):
                    mm(PS.f[:, b, :], wt.ap[:, k, (oc % 2) * P:(oc % 2 + 1) * P], XT.ap[:, k, :],
                       k == 0, k == KC - 1, [wt.res, XT.res], [PS.res[b]])
                return b

            def bias(s_, oc):
                col = O_BIN + s_ * 8 + oc
                return PRM.ap[:, col:col + 1]

            def it(oc):
                def f():
                    if oc < KC:
                        b_gc = inproj1(1, oc)
                        gct = r_bf.next()
                        act(lambda e: e.activation(out=gct.ap, in_=PS.f[:, b_gc, :], func=AF.Identity,
                                                   bias=bias(1, oc), scale=1.0),
                            [PS.res[b_gc], PRM.res], [gct.res])
                        PS.release(b_gc)
                        b_h = inproj1(2, oc)
                        uh = r_uh.next()
                        pool(lambda e: e.tensor_copy(out=uh.ap[:, 0:2], in_=HA.ap[:, oc, :]), [HA.res], [uh.res])
                        dve(lambda e: e.scalar_tensor_tensor(
                            out=uh.ap[:, 2:T + 2], in0=PS.f[:, b_h, :], scalar=bias(2, oc), in1=gct.ap,
                            op0=ALU.add, op1=ALU.mult), [PS.res[b_h], PRM.res, gct.res, uh.res], [uh.res])
                        PS.release(b_h)
                        pool(lambda e: e.tensor_copy(out=HA.ap[:, oc, :], in_=uh.ap[:, T:T + 2]), [uh.res], [HA.res])
                        b_gb = inproj1(0, oc)
                        cur = (oc, uh, b_gb)
                    else:
                        cur = None
                    if st["pend"] is not None:
                        poc, puh, pb_gb = st["pend"]
                        b_cv = PS.alloc()
                        for k in range(3):
                            mm(PS.f[:, b_cv, :], DG3.ap[:, poc, k, :], puh.ap[:, k:k + T], k == 0, k == 2,
                               [DG3.res, puh.res], [PS.res[b_cv]])
                        cvt = r_cv.next()
                        act(lambda e: e.copy(out=cvt.ap, in_=PS.f[:, b_cv, :]), [PS.res[b_cv]], [cvt.res])
                        PS.release(b_cv)
                        dve(lambda e: e.scalar_tensor_tensor(
                            out=YAb.ap[:, poc, :], in0=PS.f[:, pb_gb, :], scalar=bias(0, poc), in1=cvt.ap,
                            op0=ALU.add, op1=ALU.mult), [PS.res[pb_gb], PRM.res, cvt.res], [YAb.res])
                        PS.release(pb_gb)
                    st["pend"] = cur
                return f
            return [it(oc) for oc in range(KC + 1)]

        def reg_blocks(l, ws, c):
            w_in = w_in_d[l]
            bl = {}
            nb = {}

            def s1(d, qs):
                for q in qs:
                    for s_ in (1, 2, 0):
                        d[("in", s_, q)] = ws.add(wcols(w_in, s_ * D + q * WB))
            if c == 0:
                s1(bl, range(4))
            for q in range(4):
                bl[("oa", q)] = ws.add(wcols(w_oa_d[l], q * WB))
                bl[("in", 6, q)] = ws.add(wcols(w_in, 6 * D + q * WB))
            for g in range(4):
                bl[("in", 3, g)] = ws.add(wcols(w_in, 3 * D + g * WB))
            for g in range(4):
                bl[("in", 7, g)] = ws.add(wcols(w_in, 7 * D + g * WB))
                bl[("wp", g)] = ws.add(w_pool_d[l, g].rearrange("(k p) e -> p k e", p=P), 2)
            for q in range(4):
                bl[("in", 5, q)] = ws.add(wcols(w_in, 5 * D + q * WB))
                bl[("in", 4, q)] = ws.add(wcols(w_in, 4 * D + q * WB))
                bl[("in", 8, q)] = ws.add(wcols(w_in, 8 * D + q * WB))
            if c + 1 < NCH:
                s1(nb, (0, 1))
            for q in range(4):
                bl[("oc", q)] = ws.add(wcols(w_oc_d[l], q * WB))
            for q in range(4):
                bl[("wo", q)] = ws.add(wcols(w_o_d[l], q * WB))
            if c + 1 < NCH:
                s1(nb, (2, 3))
            bl["next"] = nb
            return bl

        def phase_a(l, c, ws, bl, bg, bg_fin, deferred, a1_cur, a1_next):
            t0 = c * T
            def bg_step():
                for _ in range(2):
                    if bg:
                        bg.pop(0)()

            def flush_deferred():
                n = len(deferred)
                for f in deferred[:n - (TT if False else 0)]:
                    f()
                del deferred[:]

            def inproj(s, oc):
                wt = ws.get(bl[("in", s, oc // 2)])
                b = PS.alloc()
                for k in range(KC):
                    mm(PS.f[:, b, :], wt.ap[:, k, (oc % 2) * P:(oc % 2 + 1) * P], XT.ap[:, k, :],
                       k == 0, k == KC - 1, [wt.res, XT.res], [PS.res[b]])
                return b

            def bias(s, oc):
                col = O_BIN + s * 8 + oc
                return PRM.ap[:, col:col + 1]

            YA = YAb
            for f in a1_cur:
                f()
            del a1_cur[:]

            def out_and_gate(wkey, yin, gs, mode, post_scalar_col, first):
                for oc in range(KC):
                    bg_step()
                    wt = ws.get(bl[(wkey, oc // 2)])
                    b_y = PS.alloc()
                    for k in range(KC):
                        mm(PS.f[:, b_y, :], wt.ap[:, k, (oc % 2) * P:(oc % 2 + 1) * P], yin.ap[:, k, :],
                           k == 0, k == KC - 1, [wt.res, yin.res], [PS.res[b_y]])
                    b_g = inproj(gs, oc)
                    gt = r_bf.next()
                    act(lambda e, b=b_g, gt=gt, oc=oc: e.activation(out=gt.ap, in_=PS.f[:, b, :], func=AF.Sigmoid,
                                                                    bias=bias(gs, oc), scale=1.0),
                        [PS.res[b_g], PRM.res], [gt.res])
                    PS.release(b_g)
                    merge(b_y, gt, oc, mode, post_scalar_col, first)
                    PS.release(b_y)

            def merge(b_y, gt, oc, mode, col, first):
                sc = PRM.ap[:, col + oc:col + oc + 1] if col is not None else None
                if first:
                    dve(lambda e: e.tensor_tensor(out=MACC.ap[:, oc, :], in0=PS.f[:, b_y, :], in1=gt.ap, op=ALU.mult),
                        [PS.res[b_y], gt.res], [MACC.res])
                else:
                    mt = r_bf.next()
                    dve(lambda e: e.scalar_tensor_tensor(out=mt.ap, in0=PS.f[:, b_y, :], scalar=sc, in1=gt.ap,
                                                         op0=(ALU.mult if mode == "scale" else ALU.add), op1=ALU.mult),
                        [PS.res[b_y], PRM.res, gt.res], [mt.res])
                    pool(lambda e: e.tensor_tensor(out=MACC.ap[:, oc, :], in0=MACC.ap[:, oc, :], in1=mt.ap, op=ALU.add),
                         [MACC.res, mt.res], [MACC.res])

            out_and_gate("oa", YA, 6, None, None, True)

            flush_deferred()
            pooled = []
            for g in range(4):
                bg_step()
                w = 2 << g
                for h in range(2):
                    oc = 2 * g + h
                    b_p = inproj(3, oc)
                    pool(lambda e, h=h, oc=oc: e.tensor_copy(out=Pt.ap[:, h, 0:15], in_=HP.ap[:, oc, 0:15]),
                         [HP.res], [Pt.res])
                    act(lambda e, b=b_p, h=h, oc=oc: e.activation(out=Pt.ap[:, h, 15:15 + T], in_=PS.f[:, b, :],
                                                                  func=AF.Identity, bias=bias(3, oc), scale=1.0),
                        [PS.res[b_p], PRM.res, Pt.res], [Pt.res])
                    PS.release(b_p)
                    pool(lambda e, h=h, oc=oc: e.tensor_copy(out=HP.ap[:, oc, 0:15], in_=Pt.ap[:, h, T:T + 15]),
                         [Pt.res], [HP.res])
                src = Pt
                lo = 0
                bufs = [Sa, Sb]
                for i in range(g + 1):
                    sh = 1 << i
                    dst = bufs[i % 2]
                    lo2 = lo + sh
                    dve(lambda e, src=src, dst=dst, lo2=lo2, sh=sh: e.tensor_tensor(
                        out=dst.ap[:, :, lo2:T + 15], in0=src.ap[:, :, lo2:T + 15], in1=src.ap[:, :, lo2 - sh:T + 15 - sh],
                        op=ALU.add), [src.res], [dst.res])
                    src = dst
                    lo = lo2
                pl = r_pooled.next()
                dve(lambda e, src=src, pl=pl, w=w: e.scalar_tensor_tensor(
                    out=pl.ap, in0=src.ap[:, :, 15:15 + T], scalar=1.0 / w, in1=Pt.ap[:, :, 15:15 + T],
                    op0=ALU.mult, op1=ALU.subtract), [src.res, Pt.res], [pl.res])
                if c == 0:
                    nfx = w - 1
                    tmpf = r_cv.next()
                    for h in range(2):
                        dve(lambda e, src=src, h=h, g=g, nfx=nfx: e.tensor_tensor(
                            out=tmpf.ap[:, h * 16:h * 16 + nfx], in0=src.ap[:, h, 15:15 + nfx], in1=rcnt.ap[:, g, 0:nfx],
                            op=ALU.mult), [src.res, rcnt.res, tmpf.res], [tmpf.res])
                        dve(lambda e, pl=pl, h=h, nfx=nfx: e.tensor_tensor(
                            out=pl.ap[:, h, 0:nfx], in0=tmpf.ap[:, h * 16:h * 16 + nfx], in1=Pt.ap[:, h, 15:15 + nfx],
                            op=ALU.subtract), [tmpf.res, Pt.res, pl.res], [pl.res])
                pooled.append(pl)
            for oc in range(KC):
                bg_step()
                g, h = oc // 2, oc % 2
                b_g = inproj(7, oc)
                gt = r_bf.next()
                act(lambda e, b=b_g, gt=gt, oc=oc: e.activation(out=gt.ap, in_=PS.f[:, b, :], func=AF.Sigmoid,
                                                                bias=bias(7, oc), scale=1.0),
                    [PS.res[b_g], PRM.res], [gt.res])
                PS.release(b_g)
                wt = ws.get(bl[("wp", g)])
                pl = pooled[g]
                b_y = PS.alloc()
                for k in range(2):
                    mm(PS.f[:, b_y, :], wt.ap[:, k, h * P:(h + 1) * P], pl.ap[:, k, :], k == 0, k == 1,
                       [wt.res, pl.res], [PS.res[b_y]])
                merge(b_y, gt, oc, "scale", O_SP, False)
                PS.release(b_y)

            b_s1 = PS.alloc()
            b_s2 = PS.alloc()
            pend = None
            pend2 = None
            for oc in range(KC + 2):
                bg_step()
                if oc < KC:
                    b_gbb = inproj(5, oc)
                    sgt = r_bf.next()
                    act(lambda e, b=b_gbb, sgt=sgt, oc=oc: e.activation(out=sgt.ap, in_=PS.f[:, b, :], func=AF.Sigmoid,
                                                                        bias=bias(5, oc), scale=1.0),
                        [PS.res[b_gbb], PRM.res], [sgt.res])
                    PS.release(b_gbb)
                    b_ga = inproj(4, oc)
                    vh = r_vh.next()
                    pool(lambda e, vh=vh, oc=oc: e.tensor_copy(out=vh.ap[:, 0:30], in_=HC.ap[:, oc, 0:30]),
                         [HC.res], [vh.res])
                    dve(lambda e, b=b_ga, vh=vh, sgt=sgt, oc=oc: e.scalar_tensor_tensor(
                        out=vh.ap[:, 30:T + 30], in0=PS.f[:, b, :], scalar=bias(4, oc), in1=sgt.ap,
                        op0=ALU.add, op1=ALU.mult), [PS.res[b_ga], PRM.res, sgt.res, vh.res], [vh.res])
                    PS.release(b_ga)
                    pool(lambda e, vh=vh, oc=oc: e.tensor_copy(out=HC.ap[:, oc, 0:30], in_=vh.ap[:, T:T + 30]),
                         [vh.res], [HC.res])
                    b_g = inproj(8, oc)
                    act(lambda e, b=b_g, oc=oc: e.activation(out=GT8_ap[:, oc, :], in_=PS.f[:, b, :], func=AF.Sigmoid,
                                                             bias=bias(8, oc), scale=1.0),
                        [PS.res[b_g], PRM.res] + GT8_res, GT8_res)
                    PS.release(b_g)
                    cur = (oc, vh)
                else:
                    cur = None
                if pend2 is not None:
                    poc = pend2
                    sq = r_bf.next()
                    dve(lambda e, poc=poc, sq=sq: e.tensor_tensor(out=sq.ap, in0=CVB.ap[:, poc, :], in1=CVB.ap[:, poc, :],
                                                                  op=ALU.mult), [CVB.res], [sq.res])
                    fw.op("pe", lambda e, poc=poc: e.matmul(PS.f[:, b_s1, :], lhsT=ones_b.ap, rhs=CVB.ap[:, poc, :],
                                                            start=(poc == 0), stop=(poc == KC - 1)),
                          [ones_b.res, CVB.res], [PS.res[b_s1]], inc=True)
                    fw.op("pe", lambda e, poc=poc, sq=sq: e.matmul(PS.f[:, b_s2, :], lhsT=ones_b.ap, rhs=sq.ap,
                                                                   start=(poc == 0), stop=(poc == KC - 1)),
                          [ones_b.res, sq.res], [PS.res[b_s2]], inc=True)
                    pend2 = None
                if pend is not None:
                    poc, pvh = pend
                    cbase = O_CC + poc * 31
                    dve(lambda e: e.tensor_tensor(
                        out=DGa.ap, in0=ident_f.ap.unsqueeze(1).to_broadcast([P, 16, P]),
                        in1=PRM.ap[:, cbase:cbase + 16].unsqueeze(2).to_broadcast([P, 16, P]),
                        op=ALU.mult), [ident_f.res, PRM.res], [DGa.res])
                    dve(lambda e: e.tensor_tensor(
                        out=DGb.ap, in0=ident_f.ap.unsqueeze(1).to_broadcast([P, 15, P]),
                        in1=PRM.ap[:, cbase + 16:cbase + 31].unsqueeze(2).to_broadcast([P, 15, P]),
                        op=ALU.mult), [ident_f.res, PRM.res], [DGb.res])
                    b_cv = PS.alloc()
                    for k in range(31):
                        dg = DGa if k < 16 else DGb
                        mm(PS.f[:, b_cv, :], dg.ap[:, k % 16, :], pvh.ap[:, k:k + T], k == 0, k == 30,
                           [dg.res, pvh.res], [PS.res[b_cv]])
                    act(lambda e, b=b_cv, poc=poc: e.activation(out=CVB.ap[:, poc, :], in_=PS.f[:, b, :], func=AF.Identity,
                                                                bias=PRM.ap[:, O_CCB + poc:O_CCB + poc + 1], scale=1.0),
                        [PS.res[b_cv], PRM.res], [CVB.res])
                    PS.release(b_cv)
                    pend2 = poc
                pend = cur
            act(lambda e: e.activation(out=st_mean.ap, in_=PS.f[:, b_s1, :], func=AF.Copy, scale=1.0 / D),
                [PS.res[b_s1]], [st_mean.res])
            PS.release(b_s1)
            dve(lambda e: e.tensor_tensor(out=st_mr.ap, in0=st_mean.ap, in1=st_mean.ap, op=ALU.mult),
                [st_mean.res], [st_mr.res])
            dve(lambda e: e.scalar_tensor_tensor(out=st_var.ap, in0=PS.f[:, b_s2, :], scalar=1.0 / D, in1=st_mr.ap,
                                                 op0=ALU.mult, op1=ALU.subtract), [PS.res[b_s2], st_mr.res], [st_var.res])
            PS.release(b_s2)
            act(lambda e: e.activation(out=st_var.ap, in_=st_var.ap, func=AF.Sqrt, bias=EPS, scale=1.0),
                [st_var.res], [st_var.res])
            dve(lambda e: e.reciprocal(out=st_var.ap, in_=st_var.ap), [st_var.res], [st_var.res])
            dve(lambda e: e.tensor_tensor(out=st_mr.ap, in0=st_mean.ap, in1=st_var.ap, op=ALU.mult),
                [st_mean.res, st_var.res], [st_mr.res])
            VN = PL1
            for oc in range(KC):
                t1 = r_cv.next()
                dve(lambda e, oc=oc, t1=t1: e.tensor_tensor(out=t1.ap, in0=CVB.ap[:, oc, :], in1=st_var.ap, op=ALU.mult),
                    [CVB.res, st_var.res], [t1.res])
                pool(lambda e, t1=t1: e.tensor_tensor(out=t1.ap, in0=t1.ap, in1=st_mr.ap, op=ALU.subtract),
                     [t1.res, st_mr.res], [t1.res])
                act(lambda e, oc=oc, t1=t1: e.activation(out=VN.ap[:, oc, :], in_=t1.ap, func=AF.Silu,
                                                         bias=PRM.ap[:, O_LB + oc:O_LB + oc + 1],
                                                         scale=PRM.ap[:, O_LG + oc:O_LG + oc + 1]),
                    [t1.res, PRM.res], [VN.res])
            while bg:
                bg_step()
            if bg_fin is not None:
                bg_fin()
                for f in a1_next[:4]:
                    f()
                del a1_next[:4]
            for oc in range(KC):
                wt = ws.get(bl[("oc", oc // 2)])
                b_y = PS.alloc()
                for k in range(KC):
                    mm(PS.f[:, b_y, :], wt.ap[:, k, (oc % 2) * P:(oc % 2 + 1) * P], VN.ap[:, k, :],
                       k == 0, k == KC - 1, [wt.res, VN.res], [PS.res[b_y]])
                mt = r_bf.next()
                dve(lambda e, oc=oc, mt=mt, b_y=b_y: e.scalar_tensor_tensor(
                    out=mt.ap, in0=PS.f[:, b_y, :], scalar=PRM.ap[:, O_BOC + oc:O_BOC + oc + 1], in1=GT8_ap[:, oc, :],
                    op0=ALU.add, op1=ALU.mult), [PS.res[b_y], PRM.res, mt.res] + GT8_res, [mt.res])
                PS.release(b_y)
                pool(lambda e, oc=oc, mt=mt: e.tensor_tensor(out=MACC.ap[:, oc, :], in0=MACC.ap[:, oc, :], in1=mt.ap, op=ALU.add),
                     [MACC.res, mt.res], [MACC.res])

            wo = [ws.get(bl[("wo", q)]) for q in range(4)]
            src_d = x_d if l == 0 else X2_d
            rd = [] if l == 0 else [r_X2[c]]
            Rs = [r_R.next() for _ in range(TT)]
            for j in range(TT):
                tok0 = (c * TT + j) * P
                fw.dma("sp", lambda e, j=j, tok0=tok0: e.dma_start(out=Rs[j].ap, in_=src_d[tok0:tok0 + P, :]), Rs[j].res,
                       reads=rd, writes=[Rs[j].res])
            for j in range(TT):
                R = Rs[j]
                for hf in range(2):
                    b_h = PS.alloc()
                    for qq in range(2):
                        q = hf * 2 + qq
                        for k in range(KC):
                            mm(PS.f[:, b_h, qq * WB:(qq + 1) * WB], MACC.ap[:, k, j * P:(j + 1) * P], wo[q].ap[:, k, :],
                               k == 0, k == KC - 1, [MACC.res, wo[q].res], [PS.res[b_h]])
                    dve(lambda e, b=b_h, hf=hf, R=R: e.scalar_tensor_tensor(
                        out=R.ap[:, hf * 512:(hf + 1) * 512], in0=R.ap[:, hf * 512:(hf + 1) * 512], scalar=ALPHA,
                        in1=PS.f[:, b, :], op0=ALU.mult, op1=ALU.add), [PS.res[b_h], R.res], [R.res])
                    PS.release(b_h)
            stages = [ln_stages(Rs[j], 0, 1, "dve") for j in range(TT)]
            for fs in zip(*stages):
                for f in fs:
                    f()
                if a1_next:
                    a1_next.pop(0)()
            while a1_next:
                a1_next.pop(0)()
            x1bs = []
            sms = []
            banks = []
            for j in range(TT):
                R = Rs[j]
                tok0 = (c * TT + j) * P
                fw.dma("sp", lambda e, R=R, tok0=tok0: e.dma_start(out=X1_d[tok0:tok0 + P, :], in_=R.ap), R.res,
                       reads=[R.res], cwrites=[r_X1[c]])
                x1b = r_x1b.next()
                x1bs.append(x1b)
                act(lambda e, R=R, x1b=x1b: e.copy(out=x1b.ap, in_=R.ap), [R.res], [x1b.res])
                for half in range(2):
                    b_t = PS.alloc()
                    for kk in range(4):
                        kc = half * 4 + kk
                        fw.op("pe", lambda e, kc=kc, kk=kk, R=R, b_t=b_t: e.transpose(
                            out=PS.f[:, b_t, kk * P:(kk + 1) * P], in_=R.ap[:, kc * P:(kc + 1) * P], identity=ident_f.ap),
                              [R.res, ident_f.res], [PS.res[b_t]], inc=(kk == 3))
                    act(lambda e, b=b_t, half=half: e.copy(out=X1T.ap[:, half * 4:(half + 1) * 4, :],
                                                           in_=PS.f[:, b, :].rearrange("p (k t) -> p k t", t=P)),
                        [PS.res[b_t]], [X1T.res])
                    PS.release(b_t)
                b_l = PS.alloc()
                banks.append(b_l)
                for kc in range(KC):
                    mm(PS.f[:, b_l, 0:E], X1T.ap[:, kc, :], WR.ap[:, kc, :], kc == 0, kc == KC - 1,
                       [X1T.res, WR.res], [PS.res[b_l]])
                sms.append(r_small.next())

            def route_ops(j):
                tg = c * TT + j
                sm = sms[j]
                b_l = banks[j]
                x1b = x1bs[j]
                LG = sm.ap[:, 0:32]
                MX8 = sm.ap[:, 32:40]
                RANKF = sm.ap[:, 64:96]
                VAL = sm.ap[:, 96:128]
                DA = sm.ap[:, 128:160]
                JUNK = sm.ap[:, 160:192]
                DEST4 = sm.ap[:, 192:196]
                VAL4 = sm.ap[:, 196:200]
                EX4 = sm.ap[:, 200:204]
                SUM = sm.ap[:, 204:205]
                NEGMX = sm.ap[:, 205:206]
                RSM = sm.ap[:, 206:207]
                MSKb = sm.ap[:, 256:272].bitcast(BF16)
                sr = [sm.res]
                ops = []
                ops.append(lambda: dve(lambda e: e.tensor_tensor(out=LG, in0=PS.f[:, b_l, 0:E], in1=BRT.ap, op=ALU.add),
                                       [PS.res[b_l], BRT.res], sr))
                ops.append(lambda: dve(lambda e: e.max(out=MX8, in_=LG), sr, sr))
                ops.append(lambda: dve(lambda e: e.tensor_scalar(out=MSKb, in0=LG, scalar1=MX8[:, 3:4], scalar2=None,
                                                                 op0=ALU.is_ge), sr, sr))

                def mms():
                    mm(PS.f[:, b_l, 64:64 + E], triu_b.ap, MSKb, True, True, [triu_b.res, sm.res], [PS.res[b_l]])
                    mm(PS.f[:, b_l, 128:128 + E], ones_b.ap, MSKb, True, True, [ones_b.res, sm.res], [PS.res[b_l]])
                ops.append(mms)
                ops.append(lambda: dve(lambda e: e.tensor_scalar_mul(out=NEGMX, in0=MX8[:, 0:1], scalar1=-1.0), sr, sr))
                ops.append(lambda: act(lambda e: e.activation(out=EX4, in_=MX8[:, 0:4], func=AF.Exp, bias=NEGMX, scale=1.0,
                                                              accum_out=SUM), sr, sr))
                ops.append(lambda: dve(lambda e: e.reciprocal(out=RSM, in_=SUM), sr, sr))

                def rank():
                    dve(lambda e: e.tensor_tensor(out=RANKF, in0=PS.f[:, b_l, 64:64 + E], in1=BASE.ap, op=ALU.add),
                        [PS.res[b_l], BASE.res] + sr, sr)
                    dve(lambda e: e.tensor_tensor(out=BASE.ap, in0=PS.f[:, b_l, 128:128 + E], in1=BASE.ap, op=ALU.add),
                        [PS.res[b_l], BASE.res], [BASE.res])
                    PS.release(b_l)
                ops.append(rank)
                ops.append(lambda: dve(lambda e: e.tensor_single_scalar(out=VAL, in_=RANKF, scalar=float(CAP), op=ALU.is_lt), sr, sr))
                ops.append(lambda: dve(lambda e: e.tensor_tensor(out=DA, in0=RANKF, in1=iota_cap.ap, op=ALU.add),
                                       sr + [iota_cap.res], sr))
                ops.append(lambda: dve(lambda e: e.scalar_tensor_tensor(out=DA, in0=DA, scalar=-float(TRASH), in1=VAL,
                                                                        op0=ALU.add, op1=ALU.mult), sr, sr))
                ops.append(lambda: dve(lambda e: e.tensor_scalar_add(out=DA, in0=DA, scalar1=float(TRASH)), sr, sr))
                for k in range(4):
                    ops.append(lambda k=k: dve(lambda e: e.scalar_tensor_tensor(
                        out=JUNK, in0=LG, scalar=MX8[:, k:k + 1], in1=DA, op0=ALU.is_equal, op1=ALU.mult,
                        accum_out=DEST4[:, k:k + 1]), sr, sr))
                ops.append(lambda: dve(lambda e: e.tensor_single_scalar(out=VAL4, in_=DEST4, scalar=float(TRASH), op=ALU.is_lt), sr, sr))
                ops.append(lambda: dve(lambda e: e.scalar_tensor_tensor(out=GATES.ap[:, tg, :], in0=EX4, scalar=RSM, in1=VAL4,
                                                                        op0=ALU.mult, op1=ALU.mult), sr + [GATES.res], [GATES.res]))
                ops.append(lambda: dve(lambda e: e.tensor_copy(out=DESTI.ap[:, tg, :], in_=DEST4), sr + [DESTI.res], [DESTI.res]))

                def scat():
                    for k in range(4):
                        fw.dma("pool", lambda e, k=k: e.indirect_dma_start(
                            out=XS_d, out_offset=bass.IndirectOffsetOnAxis(ap=DESTI.ap[:, tg, k:k + 1], axis=0),
                            in_=x1b.ap, in_offset=None), x1b.res, reads=[x1b.res, DESTI.res], cwrites=[r_XS])
                deferred.append(scat)
                return ops
            allops = [route_ops(j) for j in range(TT)]
            for fs in zip(*allops):
                for f in fs:
                    f()

        def phase_b(l):
            ws = WStream(ringB, NRB, 8)
            bl = {}
            for e_ in range(E):
                for fc in range(KC):
                    bl[("gu", e_, fc)] = ws.add(wcols(w_gu_d[l, e_], fc * WB))
                if e_ >= 1:
                    for q in range(4):
                        bl[("dn", e_ - 1, q)] = ws.add(wcols(w_dn_d[l, e_ - 1], q * WB))
            for q in range(4):
                bl[("dn", E - 1, q)] = ws.add(wcols(w_dn_d[l, E - 1], q * WB))
            XSv = XS_d[0:NSLOT, :].rearrange("(e i p) d -> e p i d", e=E, p=P)
            YSv = YS_d[0:NSLOT, :].rearrange("(e i p) d -> e i p d", e=E, p=P)
            xes = {}
            bds = {}
            xets = {}
            actts = {}

            def load_x(e_):
                xe = r_xe.next()
                fw.dma("sp", lambda e: e.dma_start(out=xe.ap, in_=XSv[e_]), xe.res, reads=[r_XS], writes=[xe.res])
                xes[e_] = xe

            def load_bd(e_):
                bd = r_bd.next()
                fw.dma("sp", lambda e: e.dma_start(out=bd.ap, in_=bdn_d[l, e_, :].partition_broadcast(P)), bd.res,
                       writes=[bd.res])
                bds[e_] = bd

            def TR(e_):
                xe = xes[e_]
                xet = r_xet.next()
                xets[e_] = xet
                for kc in range(KC):
                    b = PS.alloc()
                    for i in range(CT):
                        fw.op("pe", lambda e, i=i, kc=kc, b=b: e.transpose(out=PS.b[:, b, i * P:(i + 1) * P],
                                                                           in_=xe.ap[:, i, kc * P:(kc + 1) * P],
                                                                           identity=ident_b.ap),
                              [xe.res, ident_b.res], [PS.res[b]], inc=(i == CT - 1))
                    if kc % 2 == 0:
                        act(lambda e, b=b, kc=kc: e.copy(out=xet.ap[:, kc, :], in_=PS.b[:, b, 0:CAP]), [PS.res[b]], [xet.res])
                    else:
                        dve(lambda e, b=b, kc=kc: e.tensor_copy(out=xet.ap[:, kc, :], in_=PS.b[:, b, 0:CAP]), [PS.res[b]], [xet.res])
                    PS.release(b)

            def GU(e_):
                xet = xets[e_]
                actt = r_actt.next()
                actts[e_] = actt
                for fc in range(KC):
                    wt = ws.get(bl[("gu", e_, fc)])
                    wv = wt.ap.rearrange("p k (f two) -> p k two f", two=2)
                    bg = [PS.alloc() for _ in range(NH)]
                    bu = [PS.alloc() for _ in range(NH)]
                    for gu, banks in ((0, bg), (1, bu)):
                        for nh in range(NH):
                            for k in range(KC):
                                mm(PS.f[:, banks[nh], 0:CH], wv[:, k, gu, :], xet.ap[:, k, nh * CH:(nh + 1) * CH],
                                   k == 0, k == KC - 1, [wt.res, xet.res], [PS.res[banks[nh]]])
                    cg = O_BGU + e_ * 16 + fc
                    cu = O_BGU + e_ * 16 + 8 + fc
                    gt = r_g.next()
                    sg = r_sg.next()
                    ut = r_u.next()
                    a1 = r_a1.next()
                    for nh in range(NH):
                        sl = slice(nh * CH, (nh + 1) * CH)
                        dve(lambda e, nh=nh, sl=sl: e.tensor_scalar(out=gt.ap[:, sl], in0=PS.f[:, bg[nh], 0:CH],
                                                                    scalar1=PRM.ap[:, cg:cg + 1], scalar2=7.0,
                                                                    op0=ALU.add, op1=ALU.min),
                            [PS.res[bg[nh]], PRM.res, gt.res], [gt.res])
                        act(lambda e, nh=nh, sl=sl: e.activation(out=ut.ap[:, sl], in_=PS.f[:, bu[nh], 0:CH],
                                                                 func=AF.Identity, bias=PRM.ap[:, cu:cu + 1], scale=1.0),
                            [PS.res[bu[nh]], PRM.res, ut.res], [ut.res])
                    for b in bg + bu:
                        PS.release(b)
                    act(lambda e: e.activation(out=sg.ap, in_=gt.ap, func=AF.Sigmoid, scale=1.702), [gt.res], [sg.res])
                    pool(lambda e: e.tensor_scalar(out=ut.ap, in0=ut.ap, scalar1=7.0, scalar2=-7.0,
                                                   op0=ALU.min, op1=ALU.max), [ut.res], [ut.res])
                    pool(lambda e: e.tensor_tensor(out=a1.ap, in0=gt.ap, in1=sg.ap, op=ALU.mult),
                         [gt.res, sg.res], [a1.res])
                    dve(lambda e, fc=fc: e.scalar_tensor_tensor(out=actt.ap[:, fc, :], in0=ut.ap, scalar=1.0, in1=a1.ap,
                                                                op0=ALU.add, op1=ALU.mult),
                        [ut.res, a1.res], [actt.res])

            def DN(e_):
                actt = actts.pop(e_)
                bd = bds.pop(e_)
                wd = [ws.get(bl[("dn", e_, q)]) for q in range(4)]
                for i in range(CT):
                    yo = r_yo.next()
                    for hf in range(2):
                        b = PS.alloc()
                        for qq in range(2):
                            q = hf * 2 + qq
                            for k in range(KC):
                                mm(PS.f[:, b, qq * WB:(qq + 1) * WB], actt.ap[:, k, i * P:(i + 1) * P], wd[q].ap[:, k, :],
                                   k == 0, k == KC - 1, [actt.res, wd[q].res], [PS.res[b]])
                        dve(lambda e, b=b, hf=hf: e.tensor_tensor(out=yo.ap[:, hf * 512:(hf + 1) * 512], in0=PS.f[:, b, :],
                                                                  in1=bd.ap[:, hf * 512:(hf + 1) * 512], op=ALU.add),
                            [PS.res[b], bd.res, yo.res], [yo.res])
                        PS.release(b)
                    fw.dma("sp", lambda e, i=i: e.dma_start(out=YSv[e_, i], in_=yo.ap), yo.res,
                           reads=[yo.res], cwrites=[r_YS])

            load_x(0)
            load_x(1)
            TR(0)
            for e_ in range(E):
                load_bd(e_)
                GU(e_)
                if e_ + 2 < E:
                    load_x(e_ + 2)
                if e_ + 1 < E:
                    TR(e_ + 1)
                if e_ >= 1:
                    DN(e_ - 1)
            DN(E - 1)

        def prep_steps(l, c):
            if l == 0:
                xbs = []

                def ld(j):
                    def f():
                        tok0 = (c * TT + j) * P
                        acc = r_acc.next()
                        fw.dma("sp", lambda e: e.dma_start(out=acc.ap, in_=x_d[tok0:tok0 + P, :]), acc.res,
                               writes=[acc.res])
                        xbs.append(make_xb(acc))
                    return f
                steps = [ld(j) for j in range(TT)]
            else:
                steps, xbs = phase_c_steps(c, False)

            def fin():
                for j in range(TT):
                    xT_from_xb(xbs[j], j)
            return steps, fin

        for l in range(NL):
            layer_setup(l)
            ws = WStream(ringA, NRA, 4)
            bls = [reg_blocks(l, ws, c) for c in range(NCH)]
            a1 = stage1_steps(ws, bls[0])
            deferred = []
            steps, fin = prep_steps(l, 0)
            for f in steps:
                f()
            fin()
            for c in range(NCH):
                if c + 1 < NCH:
                    bg, bg_fin = prep_steps(l, c + 1)
                else:
                    bg, bg_fin = [], None
                a1n = stage1_steps(ws, bls[c]["next"]) if c + 1 < NCH else []
                phase_a(l, c, ws, bls[c], bg, bg_fin, deferred, a1, a1n)
                a1 = a1n
            for f in deferred:
                f()
            fw.barrier(allres)
            phase_b(l)
            fw.barrier(allres)
        for i in (2, 3):
            fw.dma("sp", lambda e, i=i: e.dma_start(out=LNP.ap[:, i, :], in_=lnp_d[NL - 1, i, :].partition_broadcast(P)),
                   LNP.res, writes=[LNP.res])
        for c in range(NCH):
            steps, _ = phase_c_steps(c, True)
            for f in steps:
                f()
        fw.barrier(allres)
        build_program.stats = dict(nins=fw.nins, nwaits=fw.nwaits, ndsem=fw.ndsem, a_end=A_END, b_end=B_END)
    return nc


def host_prm(inp, l0, l1):
    NL = l1 - l0
    prm = np.zeros((NL, P, NPRM), np.float32)
    for i, l in enumerate(range(l0, l1)):
        def pc(v):
            return np.asarray(v, np.float32).reshape(-1, P).T
        prm[i, :, O_BIN:O_BIN + 72] = pc(inp["b_in"][l])
        ca = np.asarray(inp["conv_a"][l], np.float32).reshape(3, KC, P)
        prm[i, :, O_CA:O_CA + 24] = ca.transpose(2, 1, 0).reshape(P, 24)
        prm[i, :, O_SP:O_SP + 8] = pc(inp["scale_pool"][l])
        cc = np.asarray(inp["conv_c"][l], np.float32).reshape(31, KC, P)
        prm[i, :, O_CC:O_CC + 248] = cc.transpose(2, 1, 0).reshape(P, 248)
        prm[i, :, O_CCB:O_CCB + 8] = pc(inp["conv_c_b"][l])
        prm[i, :, O_LG:O_LG + 8] = pc(inp["ln_c_g"][l])
        prm[i, :, O_LB:O_LB + 8] = pc(inp["ln_c_b"][l])
        prm[i, :, O_BOC:O_BOC + 8] = pc(inp["b_out_c"][l])
        bg = np.asarray(inp["b_gu"][l], np.float32).reshape(E, KC, P, 2)
        prm[i, :, O_BGU:O_BGU + 512] = bg.transpose(2, 0, 3, 1).reshape(P, 512)
    return prm


def make_in_maps(inp, xs, l0, l1):
    sl = slice(l0, l1)
    f = lambda a: np.ascontiguousarray(np.asarray(a, np.float32))
    shared = {
        "w_in": f(inp["w_in"][sl]), "w_out_a": f(inp["w_out_a"][sl]), "w_pool": f(inp["w_pool"][sl]),
        "w_out_c": f(inp["w_out_c"][sl]), "w_o": f(inp["w_o"][sl]), "w_router": f(inp["w_router"][sl]),
        "w_gu": f(inp["w_gu"][sl]), "w_down": f(inp["w_down"][sl]),
        "prm": host_prm(inp, l0, l1),
        "lnp": f(np.stack([inp["ln1_g"][sl], inp["ln1_b"][sl], inp["ln2_g"][sl], inp["ln2_b"][sl]], axis=1)),
        "b_router": f(inp["b_router"][sl]), "b_down": f(inp["b_down"][sl]),
    }
    return [dict(shared, x=f(x)) for x in xs]


CAP_FULL = 768
_prog_cache = {}


def run_layers(inp, xs, l0, l1, S, CAP):
    key = (S, CAP, l1 - l0)
    if key not in _prog_cache:
        _prog_cache[key] = build_program(S, CAP, l1 - l0)
    nc = _prog_cache[key]
    in_maps = make_in_maps(inp, xs, l0, l1)
    res = run_bass_kernel_spmd(nc, in_maps, core_ids=list(range(len(xs))))
    return [np.asarray(r["out"]) for r in res.results]


def kernel(**inputs):
    x = np.asarray(inputs["x"], np.float32)
    Bn, S, _ = x.shape
    xs = [x[b] for b in range(Bn)]
    outs = run_layers(inputs, xs, 0, DEPTH, S, CAP_FULL)
    return np.stack(outs, axis=0).astype(np.float32)
```
